# Optimizing a Trainium2 kernel written in Bass

```python
import math
import jax, jax.numpy as jnp
from jax import lax
import numpy as np

D_MODEL = 1024
BATCH = 4
SEQ = 8192
DEPTH = 4

N_MIXERS = 3
N_NSA = (DEPTH + 2) // 3
N_MLSTM = (DEPTH + 1) // 3
N_RWKV = DEPTH // 3
NORM_EPS = 1e-6
ROPE_THETA = 500000.0

NSA_HEAD_DIM = 64
NSA_HEADS = D_MODEL // NSA_HEAD_DIM
NSA_KV_GROUPS = 4
NSA_ROT_DIM = NSA_HEAD_DIM // 4
CMP_BLOCK = 32
CMP_STRIDE = 16
CMP_HIDDEN = 256
SEL_BLOCK = 64
SEL_TOPK = 16
WINDOW = 512
NSA_Q_BLOCK = 64
NSA_IN = NSA_HEADS * NSA_HEAD_DIM + 6 * NSA_KV_GROUPS * NSA_HEAD_DIM + 3 * NSA_HEADS
FORCE_SCORE = 1e6

MLSTM_HEADS = 8
MLSTM_QK_DIM = D_MODEL // (2 * MLSTM_HEADS)
MLSTM_V_DIM = D_MODEL // MLSTM_HEADS
MLSTM_CHUNK = 64
MLSTM_CONV = 4
MLSTM_IN = 2 * MLSTM_HEADS * MLSTM_QK_DIM + 2 * MLSTM_HEADS * MLSTM_V_DIM + 2 * MLSTM_HEADS

RWKV_HEAD_DIM = 64
RWKV_HEADS = D_MODEL // RWKV_HEAD_DIM
RWKV_DECAY_LORA = 64
RWKV_AAA_LORA = 64
RWKV_GATE_LORA = 128
RWKV_GN_EPS = 64e-5

FFN_DIM = 2816
FFN_CONV = 3

kernel_name = 'hybrid_nsa_mlstm_rwkv7_convffn'


def rmsnorm(x, g):
    xf = x.astype(jnp.float32)
    y = xf * lax.rsqrt(jnp.mean(xf * xf, axis=-1, keepdims=True) + NORM_EPS)
    return (y * g.astype(jnp.float32)).astype(x.dtype)


def causal_dwconv(x, w, b):
    k, c = w.shape
    y = lax.conv_general_dilated(x, w[:, None, :].astype(x.dtype), (1,), [(k - 1, 0)],
                                 dimension_numbers=('NWC', 'WIO', 'NWC'), feature_group_count=c)
    return y + b.astype(x.dtype)


def rope_tables(seq_len):
    half = NSA_ROT_DIM // 2
    inv_freq = ROPE_THETA ** (-jnp.arange(half, dtype=jnp.float32) / half)
    ang = jnp.arange(seq_len, dtype=jnp.float32)[:, None] * inv_freq[None, :]
    return jnp.cos(ang), jnp.sin(ang)


def partial_rope(x, cos, sin):
    half = NSA_ROT_DIM // 2
    x1 = x[..., :half].astype(jnp.float32)
    x2 = x[..., half:NSA_ROT_DIM].astype(jnp.float32)
    c = cos[None, :, None, :]
    s = sin[None, :, None, :]
    rot = jnp.concatenate([x1 * c - x2 * s, x1 * s + x2 * c], axis=-1).astype(x.dtype)
    return jnp.concatenate([rot, x[..., NSA_ROT_DIM:]], axis=-1)


def masked_softmax(s, mask):
    s = jnp.where(mask, s.astype(jnp.float32), -jnp.inf)
    m = jnp.max(s, axis=-1, keepdims=True)
    m = jnp.where(jnp.isfinite(m), m, 0.0)
    e = jnp.exp(s - m)
    return e / jnp.maximum(jnp.sum(e, axis=-1, keepdims=True), 1e-30)


def nsa_mixer(h, cos, sin, w_in, pe_k, pe_v, cmp_k_w1, cmp_k_w2, cmp_v_w1, cmp_v_w2, b_gate, w_out):
    B, T, _ = h.shape
    H, G, dh = NSA_HEADS, NSA_KV_GROUPS, NSA_HEAD_DIM
    R = H // G
    proj = h @ w_in
    cuts = np.cumsum([H * dh] + [G * dh] * 6).tolist()
    q, k_cmp, v_cmp, k_sel, v_sel, k_win, v_win, gate = jnp.split(proj, cuts, axis=-1)
    gate = jax.nn.sigmoid((gate + b_gate).astype(jnp.float32)).reshape(B, T, H, 3)
    q = q.reshape(B, T, H, dh)
    kv = lambda z: z.reshape(B, T, G, dh)
    k_cmp, v_cmp, k_sel, v_sel, k_win, v_win = kv(k_cmp), kv(v_cmp), kv(k_sel), kv(v_sel), kv(k_win), kv(v_win)
    q_rot = partial_rope(q, cos, sin)
    k_sel = partial_rope(k_sel, cos, sin)
    k_win = partial_rope(k_win, cos, sin)

    heads_first = lambda z: z.reshape(B, T, G, R, -1).transpose(0, 2, 3, 1, 4)
    q_p, q_r, gates = heads_first(q), heads_first(q_rot), heads_first(gate)

    ratio = CMP_BLOCK // CMP_STRIDE
    n_cmp = T // CMP_STRIDE - ratio + 1

    def compress(z, pe, w1, w2):
        chunks = z.reshape(B, T // CMP_STRIDE, CMP_STRIDE, G, dh)
        blocks = jnp.concatenate([chunks[:, r:r + n_cmp] for r in range(ratio)], axis=2)
        blocks = blocks + pe[None, None, :, None, :]
        flat = blocks.transpose(0, 1, 3, 2, 4).reshape(B, n_cmp, G, CMP_BLOCK * dh)
        return (jax.nn.gelu(flat @ w1) @ w2).transpose(0, 2, 1, 3)

    kc = compress(k_cmp, pe_k, cmp_k_w1, cmp_k_w2)
    vc = compress(v_cmp, pe_v, cmp_v_w1, cmp_v_w2)
    cmp_start = jnp.arange(n_cmp) * CMP_STRIDE
    cmp_end = cmp_start + CMP_BLOCK - 1

    n_sel = T // SEL_BLOCK
    k_top = min(SEL_TOPK, n_sel)
    sel_blocks = lambda z: z.transpose(0, 2, 1, 3).reshape(B, G, n_sel, SEL_BLOCK, dh)
    ks_blk, vs_blk = sel_blocks(k_sel), sel_blocks(v_sel)
    blk_start = jnp.arange(n_sel) * SEL_BLOCK
    overlap = ((cmp_end[:, None] >= blk_start[None, :]) &
               (cmp_start[:, None] <= blk_start[None, :] + SEL_BLOCK - 1)).astype(jnp.float32)
    b_idx = jnp.arange(B)[:, None, None, None]
    g_idx = jnp.arange(G)[None, :, None, None]

    pad_win = lambda z: jnp.pad(z.transpose(0, 2, 1, 3), ((0, 0), (0, 0), (WINDOW, 0), (0, 0)))
    kw_pad, vw_pad = pad_win(k_win), pad_win(v_win)

    scale = dh ** -0.5
    QB = NSA_Q_BLOCK

    def block(qi):
        q0 = qi * QB
        t = q0 + jnp.arange(QB)
        qp = lax.dynamic_slice_in_dim(q_p, q0, QB, axis=3)
        qr = lax.dynamic_slice_in_dim(q_r, q0, QB, axis=3)
        gt = lax.dynamic_slice_in_dim(gates, q0, QB, axis=3)
        s_c = jnp.einsum('bgrqd,bgcd->bgrqc', qp, kc) * scale
        p_c = masked_softmax(s_c, cmp_end[None, :] <= t[:, None])
        o_c = jnp.einsum('bgrqc,bgcd->bgrqd', p_c, vc)
        imp = jnp.einsum('bgrqc,cs->bgqs', p_c, overlap)
        tb = (t // SEL_BLOCK)[:, None]
        sid = jnp.arange(n_sel)[None, :]
        forced = (sid == 0) | (sid == tb) | (sid == tb - 1)
        imp = jnp.where(forced, FORCE_SCORE, jnp.where(sid <= tb, imp, -1.0))
        _, idx = lax.top_k(imp, k_top)
        ks = ks_blk[b_idx, g_idx, idx]
        vs = vs_blk[b_idx, g_idx, idx]
        s_s = jnp.einsum('bgrqd,bgqnkd->bgrqnk', qr, ks) * scale
        pos = idx[..., None] * SEL_BLOCK + jnp.arange(SEL_BLOCK)
        vis = (pos <= t[:, None, None])[:, :, None]
        p_s = masked_softmax(s_s.reshape(B, G, R, QB, -1), vis.reshape(B, G, 1, QB, -1))
        o_s = jnp.einsum('bgrqm,bgqmd->bgrqd', p_s, vs.reshape(B, G, QB, -1, dh))
        kw = lax.dynamic_slice_in_dim(kw_pad, q0, WINDOW + QB, axis=2)
        vw = lax.dynamic_slice_in_dim(vw_pad, q0, WINDOW + QB, axis=2)
        pw = q0 - WINDOW + jnp.arange(WINDOW + QB)
        vis_w = (pw[None, :] <= t[:, None]) & (pw[None, :] > t[:, None] - WINDOW) & (pw[None, :] >= 0)
        s_w = jnp.einsum('bgrqd,bgkd->bgrqk', qr, kw) * scale
        o_w = jnp.einsum('bgrqk,bgkd->bgrqd', masked_softmax(s_w, vis_w), vw)
        return gt[..., 0:1] * o_c + gt[..., 1:2] * o_s + gt[..., 2:3] * o_w

    o = lax.map(block, jnp.arange(T // QB))
    o = o.transpose(1, 0, 4, 2, 3, 5).reshape(B, T, H * dh).astype(h.dtype)
    return o @ w_out


def mlstm_mixer(h, w_in, conv_w, conv_b, b_gates, norm_g, w_out):
    B, T, _ = h.shape
    H, dk, dv, L = MLSTM_HEADS, MLSTM_QK_DIM, MLSTM_V_DIM, MLSTM_CHUNK
    nc = T // L
    f32 = jnp.float32
    proj = h @ w_in
    cuts = np.cumsum([2 * H * dk, H * dv, H, H]).tolist()
    qk, v, i_pre, f_pre, o_pre = jnp.split(proj, cuts, axis=-1)
    qk = jax.nn.silu(causal_dwconv(qk, conv_w, conv_b))
    q, k = jnp.split(qk, 2, axis=-1)
    chunk = lambda z: z.reshape(B, nc, L, H, -1).transpose(0, 3, 1, 2, 4).astype(f32)
    q, k, v = chunk(q), chunk(k) * (dk ** -0.5), chunk(v)
    gate_chunk = lambda z: z.astype(f32).reshape(B, nc, L, H).transpose(0, 3, 1, 2)
    log_i = gate_chunk(i_pre + b_gates[:H])
    log_f = jax.nn.log_sigmoid(gate_chunk(f_pre + b_gates[H:]))
    b = jnp.cumsum(log_f, axis=-1)
    b_end = b[..., -1]
    g_end = b_end[..., None] - b + log_i
    g_max = jnp.max(g_end, axis=-1)
    w_end = jnp.exp(g_end - g_max[..., None])
    c_loc = jnp.einsum('bhcs,bhcsk,bhcsv->bhckv', w_end, k, v)
    n_loc = jnp.einsum('bhcs,bhcsk->bhck', w_end, k)

    def step(carry, xs):
        c_st, n_st, m_st = carry
        bl, gm, cl, nl = xs
        m_new = jnp.maximum(bl + m_st, gm)
        a = jnp.exp(bl + m_st - m_new)
        s = jnp.exp(gm - m_new)
        c_new = a[..., None, None] * c_st + s[..., None, None] * cl
        n_new = a[..., None] * n_st + s[..., None] * nl
        return (c_new, n_new, m_new), (c_st, n_st, m_st)

    init = (jnp.zeros((B, H, dk, dv), f32), jnp.zeros((B, H, dk), f32), jnp.zeros((B, H), f32))
    front = lambda z: jnp.moveaxis(z, 2, 0)
    _, (c_prev, n_prev, m_prev) = lax.scan(step, init, (front(b_end), front(g_max), front(c_loc), front(n_loc)))
    c_prev = jnp.moveaxis(c_prev, 0, 2)
    n_prev = jnp.moveaxis(n_prev, 0, 2)
    m_prev = jnp.moveaxis(m_prev, 0, 2)
    causal = jnp.tril(jnp.ones((L, L), dtype=bool))
    d = jnp.where(causal, b[..., :, None] - b[..., None, :] + log_i[..., None, :], -jnp.inf)
    m_inter = b + m_prev[..., None]
    m_t = jnp.maximum(m_inter, jnp.max(d, axis=-1))
    att = jnp.exp(d - m_t[..., None]) * jnp.einsum('bhctk,bhcsk->bhcts', q, k)
    inter = jnp.exp(m_inter - m_t)
    num = jnp.einsum('bhcts,bhcsv->bhctv', att, v) + inter[..., None] * jnp.einsum('bhctk,bhckv->bhctv', q, c_prev)
    den = jnp.sum(att, axis=-1) + inter * jnp.einsum('bhctk,bhck->bhct', q, n_prev)
    h_t = num / jnp.maximum(jnp.abs(den), jnp.exp(-m_t))[..., None]
    h_t = h_t.transpose(0, 2, 3, 1, 4).reshape(B, T, H, dv)
    h_t = h_t * lax.rsqrt(jnp.mean(h_t * h_t, axis=-1, keepdims=True) + NORM_EPS) * norm_g.reshape(H, dv)
    out = h_t.reshape(B, T, H * dv) * jax.nn.sigmoid(o_pre.astype(f32))
    return out.astype(h.dtype) @ w_out


def rwkv7_mixer(h, mu, w_r, w_k, w_v, w_o, w0, w_w1, w_w2, a0, a_w1, a_w2, g_w1, g_w2,
                k_k, k_a, r_k, ln_w, ln_b):
    B, T, D = h.shape
    H, N = RWKV_HEADS, RWKV_HEAD_DIM
    f32 = jnp.float32
    xx = jnp.pad(h, ((0, 0), (1, 0), (0, 0)))[:, :-1] - h
    xr, xw, xk, xv, xa, xg = (h + xx * mu[j] for j in range(6))
    r = (xr @ w_r).astype(f32)
    k = (xk @ w_k).astype(f32)
    v = (xv @ w_v).astype(f32)
    w_log = -jax.nn.softplus(-(w0 + jnp.tanh(xw @ w_w1) @ w_w2).astype(f32)) - 0.5
    decay = jnp.exp(-jnp.exp(w_log))
    a = jax.nn.sigmoid((a0 + (xa @ a_w1) @ a_w2).astype(f32))
    g = jax.nn.sigmoid(xg @ g_w1) @ g_w2
    heads = lambda z: z.reshape(B, T, H, N)
    kk = heads(k * k_k)
    kk = kk / jnp.maximum(jnp.sqrt(jnp.sum(kk * kk, axis=-1, keepdims=True)), 1e-12)
    k = k * (1.0 + (a - 1.0) * k_a)
    r, decay, k, v, a = heads(r), heads(decay), heads(k), heads(v), heads(a)

    def step(state, inp):
        r_t, w_t, k_t, v_t, kk_t, a_t = inp
        removed = jnp.einsum('bhij,bhj->bhi', state, kk_t)
        state = (state * w_t[:, :, None, :] - removed[..., None] * (kk_t * a_t)[:, :, None, :]
                 + v_t[..., None] * k_t[:, :, None, :])
        return state, jnp.einsum('bhij,bhj->bhi', state, r_t)

    tm = lambda z: jnp.moveaxis(z, 1, 0)
    _, y = lax.scan(step, jnp.zeros((B, H, N, N), f32), (tm(r), tm(decay), tm(k), tm(v), tm(kk), tm(a)))
    y = jnp.moveaxis(y, 0, 1)
    mean = jnp.mean(y, axis=-1, keepdims=True)
    var = jnp.mean(jnp.square(y - mean), axis=-1, keepdims=True)
    y = ((y - mean) * lax.rsqrt(var + RWKV_GN_EPS)).reshape(B, T, D) * ln_w + ln_b
    bonus = jnp.sum(r * k * r_k, axis=-1, keepdims=True) * v
    y = y + bonus.reshape(B, T, D)
    return (y * g).astype(h.dtype) @ w_o


def conv_ffn(h, w_up, conv_w, conv_b, w_down):
    gate, val = jnp.split(h @ w_up, 2, axis=-1)
    gate = causal_dwconv(gate, conv_w, conv_b)
    return (jax.nn.silu(gate) * val) @ w_down


def setup_inputs(seed: int = 0) -> dict:
    key = jax.random.key(seed)
    ks = iter(jax.random.split(key, 64))
    f32 = jnp.float32

    def dense(shape, fan_in):
        return jax.random.normal(next(ks), shape, f32) * fan_in ** -0.5

    def gain(shape):
        return 1.0 + 0.02 * jax.random.normal(next(ks), shape, f32)

    def small(shape, s=0.02):
        return s * jax.random.normal(next(ks), shape, f32)

    D, H, dh, G = D_MODEL, NSA_HEADS, NSA_HEAD_DIM, NSA_KV_GROUPS
    MH, dk, dv = MLSTM_HEADS, MLSTM_QK_DIM, MLSTM_V_DIM
    inp = {}
    inp['x'] = jax.random.normal(next(ks), (BATCH, SEQ, D), f32)
    inp['norm_mixer'] = gain((DEPTH, D))
    inp['norm_ffn'] = gain((DEPTH, D))
    inp['ffn_w_up'] = dense((DEPTH, D, 2 * FFN_DIM), D)
    inp['ffn_conv_w'] = dense((DEPTH, FFN_CONV, FFN_DIM), FFN_CONV)
    inp['ffn_conv_b'] = small((DEPTH, FFN_DIM))
    inp['ffn_w_down'] = dense((DEPTH, FFN_DIM, D), FFN_DIM)
    inp['nsa_w_in'] = dense((N_NSA, D, NSA_IN), D)
    inp['nsa_pe_k'] = small((N_NSA, CMP_BLOCK, dh), 0.1)
    inp['nsa_pe_v'] = small((N_NSA, CMP_BLOCK, dh), 0.1)
    inp['nsa_cmp_k_w1'] = dense((N_NSA, CMP_BLOCK * dh, CMP_HIDDEN), CMP_BLOCK * dh)
    inp['nsa_cmp_k_w2'] = dense((N_NSA, CMP_HIDDEN, dh), CMP_HIDDEN)
    inp['nsa_cmp_v_w1'] = dense((N_NSA, CMP_BLOCK * dh, CMP_HIDDEN), CMP_BLOCK * dh)
    inp['nsa_cmp_v_w2'] = dense((N_NSA, CMP_HIDDEN, dh), CMP_HIDDEN)
    inp['nsa_b_gate'] = small((N_NSA, 3 * H), 0.1)
    inp['nsa_w_out'] = dense((N_NSA, H * dh, D), H * dh)
    inp['mlstm_w_in'] = dense((N_MLSTM, D, MLSTM_IN), D)
    inp['mlstm_conv_w'] = dense((N_MLSTM, MLSTM_CONV, 2 * MH * dk), MLSTM_CONV)
    inp['mlstm_conv_b'] = small((N_MLSTM, 2 * MH * dk))
    b_i = small((N_MLSTM, MH), 0.1)
    b_f = jnp.linspace(3.0, 6.0, MH, dtype=f32)[None, :] + small((N_MLSTM, MH), 0.1)
    inp['mlstm_b_gates'] = jnp.concatenate([b_i, b_f], axis=-1)
    inp['mlstm_norm'] = gain((N_MLSTM, MH * dv))
    inp['mlstm_w_out'] = dense((N_MLSTM, MH * dv, D), MH * dv)
    inp['rwkv_mu'] = jax.random.uniform(next(ks), (N_RWKV, 6, D), f32)
    inp['rwkv_w_r'] = dense((N_RWKV, D, D), D)
    inp['rwkv_w_k'] = dense((N_RWKV, D, D), D)
    inp['rwkv_w_v'] = dense((N_RWKV, D, D), D)
    inp['rwkv_w_o'] = dense((N_RWKV, D, D), D)
    inp['rwkv_w0'] = jax.random.uniform(next(ks), (N_RWKV, D), f32, -6.0, -1.0)
    inp['rwkv_w_w1'] = dense((N_RWKV, D, RWKV_DECAY_LORA), D)
    inp['rwkv_w_w2'] = dense((N_RWKV, RWKV_DECAY_LORA, D), RWKV_DECAY_LORA)
    inp['rwkv_a0'] = small((N_RWKV, D), 0.1)
    inp['rwkv_a_w1'] = dense((N_RWKV, D, RWKV_AAA_LORA), D)
    inp['rwkv_a_w2'] = dense((N_RWKV, RWKV_AAA_LORA, D), RWKV_AAA_LORA)
    inp['rwkv_g_w1'] = dense((N_RWKV, D, RWKV_GATE_LORA), D)
    inp['rwkv_g_w2'] = dense((N_RWKV, RWKV_GATE_LORA, D), RWKV_GATE_LORA)
    inp['rwkv_k_k'] = 0.85 + small((N_RWKV, D))
    inp['rwkv_k_a'] = 1.0 + small((N_RWKV, D))
    inp['rwkv_r_k'] = small((N_RWKV, RWKV_HEADS, RWKV_HEAD_DIM), 0.1)
    inp['rwkv_ln_w'] = gain((N_RWKV, D))
    inp['rwkv_ln_b'] = small((N_RWKV, D))
    inp['final_norm'] = gain((D,))
    return inp


def reference(x, norm_mixer, norm_ffn, ffn_w_up, ffn_conv_w, ffn_conv_b, ffn_w_down,
              nsa_w_in, nsa_pe_k, nsa_pe_v, nsa_cmp_k_w1, nsa_cmp_k_w2, nsa_cmp_v_w1, nsa_cmp_v_w2,
              nsa_b_gate, nsa_w_out,
              mlstm_w_in, mlstm_conv_w, mlstm_conv_b, mlstm_b_gates, mlstm_norm, mlstm_w_out,
              rwkv_mu, rwkv_w_r, rwkv_w_k, rwkv_w_v, rwkv_w_o, rwkv_w0, rwkv_w_w1, rwkv_w_w2,
              rwkv_a0, rwkv_a_w1, rwkv_a_w2, rwkv_g_w1, rwkv_g_w2, rwkv_k_k, rwkv_k_a, rwkv_r_k,
              rwkv_ln_w, rwkv_ln_b, final_norm):
    T = x.shape[1]
    cos, sin = rope_tables(T)
    for i in range(DEPTH):
        kind, j = i % N_MIXERS, i // N_MIXERS
        h = rmsnorm(x, norm_mixer[i])
        if kind == 0:
            y = nsa_mixer(h, cos, sin, nsa_w_in[j], nsa_pe_k[j], nsa_pe_v[j], nsa_cmp_k_w1[j],
                          nsa_cmp_k_w2[j], nsa_cmp_v_w1[j], nsa_cmp_v_w2[j], nsa_b_gate[j], nsa_w_out[j])
        elif kind == 1:
            y = mlstm_mixer(h, mlstm_w_in[j], mlstm_conv_w[j], mlstm_conv_b[j], mlstm_b_gates[j],
                            mlstm_norm[j], mlstm_w_out[j])
        else:
            y = rwkv7_mixer(h, rwkv_mu[j], rwkv_w_r[j], rwkv_w_k[j], rwkv_w_v[j], rwkv_w_o[j],
                            rwkv_w0[j], rwkv_w_w1[j], rwkv_w_w2[j], rwkv_a0[j], rwkv_a_w1[j],
                            rwkv_a_w2[j], rwkv_g_w1[j], rwkv_g_w2[j], rwkv_k_k[j], rwkv_k_a[j],
                            rwkv_r_k[j], rwkv_ln_w[j], rwkv_ln_b[j])
        x = x + y.astype(x.dtype)
        h = rmsnorm(x, norm_ffn[i])
        x = x + conv_ffn(h, ffn_w_up[i], ffn_conv_w[i], ffn_conv_b[i], ffn_w_down[i]).astype(x.dtype)
    return rmsnorm(x, final_norm)
```

```python
import numpy as np
import contextlib
import concourse.bass as bass
import concourse.mybir as mybir
from concourse.bass_utils import run_bass_kernel_spmd


F32 = mybir.dt.float32
BF16 = mybir.dt.bfloat16
AF = mybir.ActivationFunctionType
ALU = mybir.AluOpType
AX = mybir.AxisListType

ENGS = ("pe", "act", "dve", "pool", "sp")
NDSEM = 12


class Prog:
    def __init__(self, nc):
        self.nc = nc
        self.streams = {e: [] for e in ENGS}
        self.cnt = {e: 0 for e in ENGS}
        self.waited = {e: {} for e in ENGS}
        self.lastw = {}
        self.readers = {}
        self.ndma = 0
        self.pe_same_engine_sync = False

    def _need(self, eng, tok, waits):
        if tok is None:
            return
        if tok[0] == "e":
            _, e2, idx = tok
            if e2 == eng and eng == "pe":
                return
            key = ("e", e2)
            val = idx
        else:
            did = tok[1]
            key = ("d", did % NDSEM)
            val = 16 * (did // NDSEM + 1)
        if self.waited[eng].get(key, 0) >= val:
            return
        if waits.get(key, 0) < val:
            waits[key] = val

    def _deps(self, eng, reads, writes):
        waits = {}
        for r in reads:
            self._need(eng, self.lastw.get(r), waits)
        for w in writes:
            self._need(eng, self.lastw.get(w), waits)
            for t in self.readers.get(w, ()):
                self._need(eng, t, waits)
        for key, val in waits.items():
            self.streams[eng].append(("wait", key, val))
            self.waited[eng][key] = val

    def _commit(self, tok, reads, writes):
        for r in reads:
            self.readers.setdefault(r, []).append(tok)
        for w in writes:
            self.lastw[w] = tok
            self.readers[w] = []

    def op(self, eng, fn, reads=(), writes=()):
        px = [r for r in reads if r.startswith("ps") and r not in writes]
        if px:
            writes = list(writes) + px
        self._deps(eng, reads, writes)
        self.cnt[eng] += 1
        tok = ("e", eng, self.cnt[eng])
        self.streams[eng].append(("op", fn, None))
        self._commit(tok, reads, writes)
        return tok

    def dma(self, out, in_, reads=(), writes=(), q="sp", **kw):
        did = self.ndma
        self.ndma += 1
        self._deps(q, reads, writes)
        if did >= NDSEM:
            w = {}
            self._need(q, ("d", did - NDSEM), w)
            for key, val in w.items():
                self.streams[q].append(("wait", key, val))
                self.waited[q][key] = val
        self.streams[q].append(("dma", (out, in_, kw), did % NDSEM))
        tok = ("d", did)
        self._commit(tok, reads, writes)
        return tok

    def emit(self, final_wait_all=True):
        nc = self.nc
        import contextlib
        with contextlib.ExitStack() as es:
            esem = {e: es.enter_context(nc.semaphore("s_" + e)) for e in ENGS}
            dsem = [es.enter_context(nc.semaphore("d_%d" % i)) for i in range(NDSEM)]
            block = es.enter_context(nc.Block())

            def semof(key):
                return esem[key[1]] if key[0] == "e" else dsem[key[1]]

            def run(engname, eng):
                for item in self.streams[engname]:
                    if item[0] == "wait":
                        eng.wait_ge(semof(item[1]), item[2])
                    elif item[0] == "op":
                        ins = item[1](eng)
                        ins.then_inc(esem[engname], 1)
                    else:
                        out, in_, kw = item[1]
                        eng.dma_start(out=out, in_=in_, **kw).then_inc(dsem[item[2]], 16)
                if engname == "sp" and final_wait_all:
                    for i in range(NDSEM):
                        n = (self.ndma - 1 - i) // NDSEM + 1 if self.ndma > i else 0
                        if n > 0:
                            eng.wait_ge(dsem[i], 16 * n)
                    for e in ENGS:
                        if e != "sp" and self.cnt[e] > 0:
                            eng.wait_ge(esem[e], self.cnt[e])

            @block.sync
            def _(sync):
                run("sp", sync)

            @block.tensor
            def _(tensor):
                run("pe", tensor)

            @block.scalar
            def _(scalar):
                run("act", scalar)

            @block.vector
            def _(vector):
                run("dve", vector)

            @block.gpsimd
            def _(gpsimd):
                run("pool", gpsimd)


D = 1024
EPS = 1e-6
NSA_IN = 2608


def load_cast_weight(P, es, nc, w_dram, K, N, name, stg, stgn, CW=512):
    KC = K // 128
    wt = es.enter_context(nc.sbuf_tensor("sbw_" + name, [128, KC, N], BF16))
    i = 0
    for kc in range(KC):
        for c0 in range(0, N, CW):
            cw = min(CW, N - c0)
            s = stg[i % len(stg)]
            sn = stgn[i % len(stg)]
            P.dma(s[:, 0:cw], w_dram[kc * 128:(kc + 1) * 128, c0:c0 + cw], writes=[sn])
            if i % 2 == 0:
                P.op("act", lambda e, o=wt[:, kc, c0:c0 + cw], a=s[:, 0:cw]: e.copy(out=o, in_=a),
                     reads=[sn], writes=[name])
            else:
                P.op("pool", lambda e, o=wt[:, kc, c0:c0 + cw], a=s[:, 0:cw]: e.tensor_copy(out=o, in_=a),
                     reads=[sn], writes=[name])
            i += 1
    return wt


class Front:
    def __init__(self, P, es, nc, gn_dram, TILE=512, nps=None):
        self.P, self.nc, self.TILE = P, nc, TILE
        T = lambda name, shape, dt: es.enter_context(nc.sbuf_tensor(name, shape, dt))
        self.x_t = T("x_t", [128, 8, TILE], F32)
        self.sq_t = T("sq_t", [128, 8, TILE], BF16)
        self.h_t = T("h_t", [128, 8, TILE], BF16)
        self.rs_t = T("rs_t", [128, TILE], F32)
        self.gn_t = T("gn_t", [128, 8], F32)
        self.ones = T("ones", [128, 128], BF16)
        P.dma(self.gn_t[:], gn_dram, writes=["gn_t"])
        P.op("pool", lambda e: e.memset(self.ones[:], 1.0), writes=["ones"])
        self.nps = nps

    def run(self, xT_cols, n):
        P = self.P
        x_t, sq_t, h_t, rs_t, gn_t, ones = self.x_t, self.sq_t, self.h_t, self.rs_t, self.gn_t, self.ones
        P.dma(x_t[:, :, 0:n], xT_cols.rearrange("(c p) t -> p c t", p=128), writes=["x_t"])
        P.op("act", lambda e: e.activation(out=sq_t[:, :, 0:n], in_=x_t[:, :, 0:n], func=AF.Square),
             reads=["x_t"], writes=["sq_t"])
        pt, pn = self.nps()
        for c in range(8):
            P.op("pe", lambda e, c=c, pt=pt: e.matmul(pt[:, 0:n], lhsT=ones[:], rhs=sq_t[:, c, 0:n],
                                                     start=(c == 0), stop=(c == 7)),
                 reads=["ones", "sq_t"], writes=[pn])
        P.op("dve", lambda e, pt=pt: e.tensor_scalar(out=rs_t[:, 0:n], in0=pt[:, 0:n], scalar1=1.0 / D,
                                                    scalar2=EPS, op0=ALU.mult, op1=ALU.add),
             reads=[pn], writes=["rs_t"])
        P.op("act", lambda e: e.sqrt(out=rs_t[:, 0:n], in_=rs_t[:, 0:n]), reads=["rs_t"], writes=["rs_t"])
        P.op("dve", lambda e: e.reciprocal(out=rs_t[:, 0:n], in_=rs_t[:, 0:n]), reads=["rs_t"], writes=["rs_t"])
        for c in range(8):
            P.op("dve", lambda e, c=c: e.scalar_tensor_tensor(
                out=h_t[:, c, 0:n], in0=x_t[:, c, 0:n], scalar=gn_t[:, c:c + 1], in1=rs_t[:, 0:n],
                op0=ALU.mult, op1=ALU.mult), reads=["x_t", "rs_t", "gn_t"], writes=["h_t"])


def make_ps(es, nc, n):
    ps = [es.enter_context(nc.psum_tensor("ps%d" % i, [128, 512], F32)) for i in range(n)]
    ctr = [0]

    def nextps():
        i = ctr[0] % n
        ctr[0] += 1
        return ps[i], "ps%d" % i
    return nextps


def rope_ops(P, src, dst, nh, cs, sn, t1, t2, rd, wr):
    cb = cs[:, :].unsqueeze(1).to_broadcast([128, nh, 8])
    sb = sn[:, :].unsqueeze(1).to_broadcast([128, nh, 8])
    x1 = src[:, :, 0:8]
    x2 = src[:, :, 8:16]
    a = t1[:, 0:nh, :]
    b = t2[:, 0:nh, :]
    P.op("dve", lambda e: e.tensor_tensor(out=a, in0=x1, in1=cb, op=ALU.mult), reads=rd, writes=["rt1"])
    P.op("dve", lambda e: e.tensor_tensor(out=b, in0=x2, in1=sb, op=ALU.mult), reads=rd, writes=["rt2"])
    P.op("dve", lambda e: e.tensor_tensor(out=dst[:, :, 0:8], in0=a, in1=b, op=ALU.subtract),
         reads=["rt1", "rt2"], writes=wr)
    P.op("dve", lambda e: e.tensor_tensor(out=a, in0=x1, in1=sb, op=ALU.mult), reads=rd, writes=["rt1"])
    P.op("dve", lambda e: e.tensor_tensor(out=b, in0=x2, in1=cb, op=ALU.mult), reads=rd, writes=["rt2"])
    P.op("dve", lambda e: e.tensor_tensor(out=dst[:, :, 8:16], in0=a, in1=b, op=ALU.add),
         reads=["rt1", "rt2"], writes=wr)


def build_nsa_a(NT):
    nc = bass.Bass("TRN2", target_bir_lowering=False)
    xT = nc.dram_tensor("xT", [D, NT], F32, kind="ExternalInput").ap()
    w_in = nc.dram_tensor("w_in", [D, NSA_IN], F32, kind="ExternalInput").ap()
    gn = nc.dram_tensor("gn", [128, 8], F32, kind="ExternalInput").ap()
    cosd = nc.dram_tensor("cos", [NT, 8], F32, kind="ExternalInput").ap()
    sind = nc.dram_tensor("sin", [NT, 8], F32, kind="ExternalInput").ap()
    bg = nc.dram_tensor("bg", [128, 48], F32, kind="ExternalInput").ap()
    pr = nc.dram_tensor("pr", [NT, 3584], BF16, kind="ExternalOutput").ap()
    gate = nc.dram_tensor("gate", [NT, 48], F32, kind="ExternalOutput").ap()
    P = Prog(nc)
    with contextlib.ExitStack() as es:
        T = lambda name, shape, dt: es.enter_context(nc.sbuf_tensor(name, shape, dt))
        nextps = make_ps(es, nc, 7)
        stg = [T("stg%d" % i, [128, 512], F32) for i in range(2)]
        w_t = load_cast_weight(P, es, nc, w_in, D, NSA_IN, "w_t", stg, ["stg0", "stg1"])
        fr = Front(P, es, nc, gn, nps=nextps)
        bg_t = T("bg_t", [128, 48], F32)
        P.dma(bg_t[:], bg, writes=["bg_t"])
        PR = T("PR", [128, NSA_IN], F32)
        OUT = T("OUT", [128, 3584], BF16)
        GT = T("GT", [128, 48], F32)
        cs = T("cs", [128, 8], F32)
        sn = T("sn", [128, 8], F32)
        rt1 = T("rt1", [128, 16, 8], F32)
        rt2 = T("rt2", [128, 16, 8], F32)
        blocks = [(c0, min(512, NSA_IN - c0)) for c0 in range(0, NSA_IN, 512)]
        for t0 in range(0, NT, 512):
            n = min(512, NT - t0)
            fr.run(xT[:, t0:t0 + n], n)
            for s0 in range(0, n, 128):
                tt = t0 + s0
                P.dma(cs[:], cosd[tt:tt + 128, :], writes=["cs"], q="pool")
                P.dma(sn[:], sind[tt:tt + 128, :], writes=["sn"], q="pool")
                for (c0, cwid) in blocks:
                    pt, pn = nextps()
                    for kc in range(8):
                        P.op("pe", lambda e, kc=kc, pt=pt, c0=c0, cwid=cwid, s0=s0: e.matmul(
                            pt[:, 0:cwid], lhsT=fr.h_t[:, kc, s0:s0 + 128], rhs=w_t[:, kc, c0:c0 + cwid],
                            start=(kc == 0), stop=(kc == 7)), reads=["h_t", "w_t"], writes=[pn])
                    P.op("act", lambda e, pt=pt, c0=c0, cwid=cwid: e.copy(out=PR[:, c0:c0 + cwid],
                                                                          in_=pt[:, 0:cwid]),
                         reads=[pn], writes=["PR"])
                P.op("act", lambda e: e.copy(out=OUT[:, 0:2560], in_=PR[:, 0:2560]), reads=["PR"], writes=["OUT"])
                P.op("pool", lambda e: e.tensor_copy(out=OUT[:, 2560:3584], in_=PR[:, 0:1024]),
                     reads=["PR"], writes=["OUT"])
                P.op("dve", lambda e: e.tensor_tensor(out=GT[:], in0=PR[:, 2560:2608], in1=bg_t[:], op=ALU.add),
                     reads=["PR", "bg_t"], writes=["GT"])
                P.op("act", lambda e: e.activation(out=GT[:], in_=GT[:], func=AF.Sigmoid), reads=["GT"], writes=["GT"])
                qv = PR[:, 0:1024].rearrange("p (h d) -> p h d", d=64)
                rope_ops(P, qv, OUT[:, 2560:3584].rearrange("p (h d) -> p h d", d=64), 16, cs, sn, rt1, rt2,
                         ["PR", "cs", "sn"], ["OUT"])
                for off in (1024 + 512, 1024 + 1024):
                    kv_ = PR[:, off:off + 256].rearrange("p (h d) -> p h d", d=64)
                    rope_ops(P, kv_, OUT[:, off:off + 256].rearrange("p (h d) -> p h d", d=64), 4, cs, sn,
                             rt1, rt2, ["PR", "cs", "sn"], ["OUT"])
                P.dma(pr[tt:tt + 128, :], OUT[:], reads=["OUT"])
                P.dma(gate[tt:tt + 128, :], GT[:], reads=["GT"], q="pool")
        P.emit()
    return nc


def pack_vec(v):
    return np.ascontiguousarray(v.reshape(-1, 128).T)


def rope_tables_np(T):
    half = 8
    inv = 500000.0 ** (-np.arange(half, dtype=np.float32) / half)
    ang = np.arange(T, dtype=np.float32)[:, None] * inv[None, :]
    return np.cos(ang).astype(np.float32), np.sin(ang).astype(np.float32)


D = 1024
FF = 2816
NFC = FF // 128
EPS = 1e-6


def load_cast_weight_ffn(P, es, nc, w_dram, K, N, name, stg, qs=("sp",)):
    KC = K // 128
    wt = es.enter_context(nc.sbuf_tensor(name, [128, KC, N], BF16))
    CW = 512
    i = 0
    for kc in range(KC):
        for c0 in range(0, N, CW):
            cw = min(CW, N - c0)
            s = stg[i % len(stg)]
            sn = 'stg%d' % (i % len(stg))
            P.dma(s[:, 0:cw], w_dram[kc * 128:(kc + 1) * 128, c0:c0 + cw],
                  writes=[sn], q=qs[i % len(qs)])
            eng = ("act", "pool")[i % 2]
            if eng == "act":
                P.op("act", lambda e, o=wt[:, kc, c0:c0 + cw], a=s[:, 0:cw]: e.copy(out=o, in_=a),
                     reads=[sn], writes=[name])
            else:
                P.op("pool", lambda e, o=wt[:, kc, c0:c0 + cw], a=s[:, 0:cw]: e.tensor_copy(out=o, in_=a),
                     reads=[sn], writes=[name])
            i += 1
    return wt


def build_ffn(NT, final_norm=False, TILE=512):
    nc = bass.Bass("TRN2", target_bir_lowering=False)
    xT = nc.dram_tensor("xT", [D, NT + 2], F32, kind="ExternalInput").ap()
    oT = nc.dram_tensor("oT", [D, NT + 2], BF16, kind="ExternalInput").ap()
    w_out = nc.dram_tensor("w_out", [D, D], F32, kind="ExternalInput").ap()
    w_up = nc.dram_tensor("w_up", [D, 2 * FF], F32, kind="ExternalInput").ap()
    w_down = nc.dram_tensor("w_down", [FF, D], F32, kind="ExternalInput").ap()
    gn = nc.dram_tensor("gn", [128, 8], F32, kind="ExternalInput").ap()
    gfin = nc.dram_tensor("gfin", [128, 8], F32, kind="ExternalInput").ap()
    cw = nc.dram_tensor("cw", [128, 3 * NFC], F32, kind="ExternalInput").ap()
    cb = nc.dram_tensor("cb", [128, NFC], F32, kind="ExternalInput").ap()
    yT = nc.dram_tensor("yT", [D, NT], F32, kind="ExternalOutput").ap()

    P = Prog(nc)
    with contextlib.ExitStack() as es:
        T = lambda name, shape, dt: es.enter_context(nc.sbuf_tensor(name, shape, dt))
        stg = [T("stg%d" % i, [128, 512], F32) for i in range(2)]
        gn_t = T("gn_t", [128, 8], F32)
        gf_t = T("gf_t", [128, 8], F32)
        cw_t = T("cw_t", [128, 3 * NFC], F32)
        cb_t = T("cb_t", [128, NFC], F32)
        ones = T("ones", [128, 128], BF16)
        P.dma(gn_t[:], gn, writes=["gn_t"])
        P.dma(gf_t[:], gfin, writes=["gf_t"])
        P.dma(cw_t[:], cw, writes=["cw_t"])
        P.dma(cb_t[:], cb, writes=["cb_t"])
        P.op("pool", lambda e: e.memset(ones[:], 1.0), writes=["ones"])
        wo_t = load_cast_weight_ffn(P, es, nc, w_out, D, D, "wo_t", stg)
        wu_t = load_cast_weight_ffn(P, es, nc, w_up, D, 2 * FF, "wu_t", stg)
        wd_t = load_cast_weight_ffn(P, es, nc, w_down, FF, D, "wd_t", stg)

        x_t = T("x_t", [128, 8, TILE], F32)
        o_t = T("o_t", [128, 8, TILE], BF16)
        h_t = o_t
        rs_t = T("rs_t", [128, TILE], F32)
        Gc = T("Gc", [128, NFC, 2], F32)
        Gw = [T("Gw%d" % i, [128, TILE + 2], F32) for i in range(1)]
        tt = [T("tt%d" % i, [128, TILE], F32) for i in range(1)]
        ss = [T("ss%d" % i, [128, TILE], BF16) for i in range(1)]
        A_t = T("A_t", [128, NFC, TILE], BF16)
        sq_t = A_t
        NPS = 6
        ps = [es.enter_context(nc.psum_tensor("ps%d" % i, [128, 512], F32)) for i in range(NPS)]
        psi = [0]

        def nextps():
            i = psi[0] % NPS
            psi[0] += 1
            return ps[i], "ps%d" % i

        def rmsnorm(n, g_t, gname, dst, dstname, dst_dt_bf16=True):
            P.op("act", lambda e: e.activation(out=sq_t[:, 0:8, 0:n], in_=x_t[:, :, 0:n], func=AF.Square),
                 reads=["x_t"], writes=["A_t"])
            pt, pn = nextps()
            for c in range(8):
                P.op("pe", lambda e, c=c, pt=pt: e.matmul(pt[:, 0:n], lhsT=ones[:], rhs=sq_t[:, c, 0:n],
                                                         start=(c == 0), stop=(c == 7)),
                     reads=["ones", "A_t"], writes=[pn])
            P.op("dve", lambda e, pt=pt: e.tensor_scalar(out=rs_t[:, 0:n], in0=pt[:, 0:n], scalar1=1.0 / D,
                                                        scalar2=EPS, op0=ALU.mult, op1=ALU.add),
                 reads=[pn], writes=["rs_t"])
            P.op("act", lambda e: e.sqrt(out=rs_t[:, 0:n], in_=rs_t[:, 0:n]),
                 reads=["rs_t"], writes=["rs_t"])
            P.op("dve", lambda e: e.reciprocal(out=rs_t[:, 0:n], in_=rs_t[:, 0:n]),
                 reads=["rs_t"], writes=["rs_t"])
            for c in range(8):
                P.op("dve", lambda e, c=c: e.scalar_tensor_tensor(
                    out=dst[:, c, 0:n], in0=x_t[:, c, 0:n], scalar=g_t[:, c:c + 1], in1=rs_t[:, 0:n],
                    op0=ALU.mult, op1=ALU.mult), reads=["x_t", "rs_t", gname], writes=[dstname])

        def do_tile(c0, n, halo):
            P.dma(x_t[:, :, 0:n], xT[:, c0:c0 + n].rearrange("(c p) t -> p c t", p=128), writes=["x_t"])
            P.dma(o_t[:, :, 0:n], oT[:, c0:c0 + n].rearrange("(c p) t -> p c t", p=128), writes=["o_t"],
                  q="pool")
            for dc in range(8):
                pt, pn = nextps()
                for kc in range(8):
                    P.op("pe", lambda e, dc=dc, kc=kc, pt=pt: e.matmul(
                        pt[:, 0:n], lhsT=wo_t[:, kc, dc * 128:(dc + 1) * 128], rhs=o_t[:, kc, 0:n],
                        start=(kc == 0), stop=(kc == 7)), reads=["wo_t", "o_t"], writes=[pn])
                P.op("dve", lambda e, dc=dc, pt=pt: e.tensor_tensor(
                    out=x_t[:, dc, 0:n], in0=pt[:, 0:n], in1=x_t[:, dc, 0:n], op=ALU.add),
                    reads=[pn, "x_t"], writes=["x_t"])
            rmsnorm(n, gn_t, "gn_t", h_t, "o_t")
            for fc in range(NFC):
                pt, pn = nextps()
                for kc in range(8):
                    P.op("pe", lambda e, fc=fc, kc=kc, pt=pt: e.matmul(
                        pt[:, 0:n], lhsT=wu_t[:, kc, fc * 128:(fc + 1) * 128], rhs=h_t[:, kc, 0:n],
                        start=(kc == 0), stop=(kc == 7)), reads=["wu_t", "o_t"], writes=[pn])
                gk = "Gw0"
                g = Gw[0]
                if halo:
                    P.op("act", lambda e, fc=fc, pt=pt: e.copy(out=Gc[:, fc, :], in_=pt[:, 0:2]),
                         reads=[pn], writes=["Gc"])
                    continue
                P.op("act", lambda e, fc=fc, g=g: e.copy(out=g[:, 0:2], in_=Gc[:, fc, :]),
                     reads=["Gc"], writes=[gk])
                P.op("act", lambda e, fc=fc, pt=pt, g=g: e.copy(out=g[:, 2:2 + n], in_=pt[:, 0:n]),
                     reads=[pn], writes=[gk])
                P.op("act", lambda e, fc=fc, g=g: e.copy(out=Gc[:, fc, :], in_=g[:, n:n + 2]),
                     reads=[gk], writes=["Gc"])
                pv, pvn = nextps()
                for kc in range(8):
                    P.op("pe", lambda e, fc=fc, kc=kc, pv=pv: e.matmul(
                        pv[:, 0:n], lhsT=wu_t[:, kc, FF + fc * 128:FF + (fc + 1) * 128], rhs=h_t[:, kc, 0:n],
                        start=(kc == 0), stop=(kc == 7)), reads=["wu_t", "o_t"], writes=[pvn])
                t = tt[0]
                tn = "tt0"
                s = ss[0]
                sn = "ss0"
                P.op("dve", lambda e, fc=fc, t=t, g=g: e.tensor_scalar(
                    out=t[:, 0:n], in0=g[:, 0:n], scalar1=cw_t[:, fc:fc + 1], scalar2=None, op0=ALU.mult),
                    reads=[gk, "cw_t"], writes=[tn])
                for k in (1, 2):
                    P.op("dve", lambda e, fc=fc, t=t, k=k, g=g: e.scalar_tensor_tensor(
                        out=t[:, 0:n], in0=g[:, k:k + n], scalar=cw_t[:, k * NFC + fc:k * NFC + fc + 1],
                        in1=t[:, 0:n], op0=ALU.mult, op1=ALU.add), reads=[gk, "cw_t", tn], writes=[tn])
                P.op("act", lambda e, fc=fc, t=t, s=s: e.activation(
                    out=s[:, 0:n], in_=t[:, 0:n], func=AF.Silu, bias=cb_t[:, fc:fc + 1]),
                    reads=[tn, "cb_t"], writes=[sn])
                P.op("dve", lambda e, fc=fc, s=s, pv=pv: e.tensor_tensor(
                    out=A_t[:, fc, 0:n], in0=pv[:, 0:n], in1=s[:, 0:n], op=ALU.mult),
                    reads=[pvn, sn], writes=["A_t"])
            if halo:
                return
            for dc in range(8):
                pt, pn = nextps()
                for fc in range(NFC):
                    P.op("pe", lambda e, dc=dc, fc=fc, pt=pt: e.matmul(
                        pt[:, 0:n], lhsT=wd_t[:, fc, dc * 128:(dc + 1) * 128], rhs=A_t[:, fc, 0:n],
                        start=(fc == 0), stop=(fc == NFC - 1)), reads=["wd_t", "A_t"], writes=[pn])
                P.op("dve", lambda e, dc=dc, pt=pt: e.tensor_tensor(
                    out=x_t[:, dc, 0:n], in0=pt[:, 0:n], in1=x_t[:, dc, 0:n], op=ALU.add),
                    reads=[pn, "x_t"], writes=["x_t"])
            src, srcn = x_t, "x_t"
            if final_norm:
                rmsnorm(n, gf_t, "gf_t", x_t, "x_t")
            P.dma(yT[:, c0 - 2:c0 - 2 + n].rearrange("(c p) t -> p c t", p=128), src[:, :, 0:n], reads=[srcn])

        es_fin = [None]
        do_tile(0, 2, True)
        for c0 in range(2, NT + 2, TILE):
            do_tile(c0, min(TILE, NT + 2 - c0), False)
        P.emit()
    return nc


def ffn_ref(x, o, w_out, gn, w_up, cw, cb, w_down, gfin=None):
    x1 = x + o @ w_out
    h = x1 / np.sqrt((x1 * x1).mean(-1, keepdims=True) + EPS) * gn
    u = h @ w_up
    gate, val = u[:, :FF], u[:, FF:]
    g = cw[0] * gate[:-2] + cw[1] * gate[1:-1] + cw[2] * gate[2:] + cb
    a = g / (1 + np.exp(-g)) * val[2:]
    y = x1[2:] + a @ w_down
    if gfin is not None:
        y = y / np.sqrt((y * y).mean(-1, keepdims=True) + EPS) * gfin
    return y


def pack_vec(v):
    return np.ascontiguousarray(v.reshape(-1, 128).T)


NEG = -30000.0


def nsa_consts(T):
    import ml_dtypes
    bf = ml_dtypes.bfloat16
    kl = np.arange(128)[:, None]
    tl = np.arange(512)[None, :]
    cm = np.zeros((128, 8, 512), np.float32)
    for di, d in enumerate(range(-4, 4)):
        rel = 128 * d + kl - tl
        cm[:, di, :] = np.where((rel <= 0) & (rel >= -511), 1.0, 0.0)
    cmpm = np.zeros((128, 5, 512), np.float32)
    for di, d in enumerate(range(-4, 1)):
        cmpm[:, di, :] = np.where(16 * kl + 31 + 512 * d <= tl, 0.0, NEG)
    nct = (T // 16 + 127) // 128
    ov = np.zeros((128, nct, 129), np.float32)
    for j in range(nct):
        c = 128 * j + np.arange(128)[:, None]
        s = np.arange(128)[None, :]
        ov[:, j, 0:128] = ((c >= 4 * s - 1) & (c <= 4 * s + 3)).astype(np.float32)
        ov[:, j, 128] = 1.0
    ebig = (np.arange(128)[:, None] == (np.arange(T)[None, :] // 64)).astype(np.float32)
    p = np.arange(128)[:, None]
    sp = np.arange(256)[None, :] - 128
    tb = (p >= 64).astype(np.int64)
    causal = sp <= tb
    forced = (sp == tb) | (sp == tb - 1)
    m01 = causal.astype(np.float32)
    add = np.where(forced, 1e6, np.where(causal, 0.0, -1.0)).astype(np.float32)
    m01 = np.where(forced, 0.0, m01).astype(np.float32)
    return {"CM": cm.reshape(128, -1).astype(bf), "CMPM": cmpm.reshape(128, -1).astype(bf),
            "OV": ov.reshape(128, -1).astype(bf), "EBIG": ebig.astype(bf),
            "M01": m01, "ADDM": add, "IDB": np.eye(128, dtype=np.float32).astype(bf),
            "IDF": np.eye(128, dtype=np.float32)}


def build_nsa_b(T):
    NTT = T // 512
    NKT = T // 128
    NCMP = T // 16 - 1
    NCT = (T // 16 + 127) // 128
    nc = bass.Bass("TRN2", target_bir_lowering=False)
    dt_in = lambda name, shape, dt: nc.dram_tensor(name, shape, dt, kind="ExternalInput").ap()
    Qd = dt_in("Q", [128, 4, T], BF16)
    QRd = dt_in("QR", [128, 4, T], BF16)
    KCd = dt_in("KC", [128, T], BF16)
    VCd = dt_in("VC", [128, T], BF16)
    KSd = dt_in("KS", [128, T], BF16)
    KWd = dt_in("KW", [128, T], BF16)
    VSd = dt_in("VS", [T, 2 * 65], BF16)
    VWd = dt_in("VW", [T, 2 * 65], BF16)
    GAd = dt_in("GA", [T, 24], F32)
    W1d = [dt_in("W1K", [128, 32 * 256], F32), dt_in("W1V", [128, 32 * 256], F32)]
    W2d = [dt_in("W2K", [128, 2 * 128], F32), dt_in("W2V", [128, 2 * 128], F32)]
    PEd = [dt_in("PEK", [128, 32], F32), dt_in("PEV", [128, 32], F32)]
    CMd = dt_in("CM", [128, 8 * 512], BF16)
    CMPMd = dt_in("CMPM", [128, 5 * 512], BF16)
    OVd = dt_in("OV", [128, NCT * 129], BF16)
    EBd = dt_in("EBIG", [128, T], BF16)
    M01d = dt_in("M01", [128, 256], F32)
    ADDd = dt_in("ADDM", [128, 256], F32)
    IDBd = dt_in("IDB", [128, 128], BF16)
    IDFd = dt_in("IDF", [128, 128], F32)
    Od = nc.dram_tensor("O", [T, 512], BF16, kind="ExternalOutput").ap()

    P = Prog(nc)
    with contextlib.ExitStack() as es:
        Tn = lambda name, shape, dt: es.enter_context(nc.sbuf_tensor("sb_" + name, shape, dt))

        def const_load(name, dram, shape, dt, q="sp"):
            t = Tn(name, shape, dt)
            if len(shape) == 2:
                P.dma(t[:], dram, writes=[name], q=q)
            elif len(shape) == 3:
                P.dma(t[:], dram.rearrange("p (a b) -> p a b", b=shape[2]), writes=[name], q=q)
            return t
        KVC = Tn("KVC", [128, T], BF16)
        KS = const_load("KS", KSd, [128, T], BF16)
        KW = const_load("KW", KWd, [128, T], BF16, q="pool")
        VS = Tn("VS", [128, NKT, 130], BF16)
        VW = Tn("VW", [128, NKT, 130], BF16)
        P.dma(VS[:], VSd.rearrange("(k p) c -> p k c", p=128), writes=["VS"])
        P.dma(VW[:], VWd.rearrange("(k p) c -> p k c", p=128), writes=["VW"], q="pool")
        CM = const_load("CM", CMd, [128, 8, 512], BF16)
        CMPM = const_load("CMPM", CMPMd, [128, 5, 512], BF16)
        OV = const_load("OV", OVd, [128, NCT, 129], BF16)
        EB = const_load("EBIG", EBd, [128, T], BF16)
        M01 = const_load("M01", M01d, [128, 256], F32)
        ADDM = const_load("ADDM", ADDd, [128, 256], F32)
        IDB = const_load("IDB", IDBd, [128, 128], BF16)
        IDF = const_load("IDF", IDFd, [128, 128], F32)

        psS = [es.enter_context(nc.psum_tensor("psS%d" % i, [128, 512], F32)) for i in range(3)]
        psO = [es.enter_context(nc.psum_tensor("psO%d" % i, [128, 512], F32)) for i in range(2)]
        psI = [es.enter_context(nc.psum_tensor("psI%d" % i, [128, 512], F32)) for i in range(1)]
        psT = [es.enter_context(nc.psum_tensor("psT%d" % i, [128, 512], F32)) for i in range(2)]
        ctr = {"S": 0, "O": 0, "I": 0, "T": 0}
        pools = {"S": psS, "O": psO, "I": psI, "T": psT}

        def nps(k):
            i = ctr[k] % len(pools[k])
            ctr[k] += 1
            return pools[k][i], "ps%s%d" % (k, i)

        stg = [Tn("stg%d" % i, [128, 1024], F32) for i in range(2)]
        W1b = Tn("W1b", [128, 32, 256], BF16)
        W1 = [W1b, W1b]
        W2 = [Tn("W2_%d" % k, [128, 2, 128], BF16) for k in range(2)]
        PEt = [Tn("PE_%d" % k, [128, 32], BF16) for k in range(2)]
        sictr = [0]

        def load_cmp_weights(k):
            for c0 in range(0, 32 * 256, 1024):
                s, sn = stg[sictr[0] % 2], "stg%d" % (sictr[0] % 2)
                sictr[0] += 1
                P.dma(s[:], W1d[k][:, c0:c0 + 1024], writes=[sn])
                P.op("act", lambda e, c0=c0, s=s: e.copy(
                    out=W1b[:].rearrange("p a b -> p (a b)")[:, c0:c0 + 1024], in_=s[:]),
                    reads=[sn], writes=["W1b"])
            s, sn = stg[sictr[0] % 2], "stg%d" % (sictr[0] % 2)
            sictr[0] += 1
            P.dma(s[:, 0:256], W2d[k], writes=[sn])
            P.dma(s[:, 256:288], PEd[k], writes=[sn])
            P.op("act", lambda e, k=k, s=s: e.copy(out=W2[k][:].rearrange("p a b -> p (a b)"), in_=s[:, 0:256]),
                 reads=[sn], writes=["W2_%d" % k])
            P.op("act", lambda e, k=k, s=s: e.copy(out=PEt[k][:], in_=s[:, 256:288]),
                 reads=[sn], writes=["PE_%d" % k])
            P.dma(KVC[:], (KCd, VCd)[k], writes=["KVC"], q="pool")
        KCMP = Tn("KCMP", [128, NCT * 128], BF16)
        VCMP = Tn("VCMP", [128, NCT, 130], BF16)
        P.op("pool", lambda e: e.memset(KCMP[:], 0.0), writes=["KCMP"])
        P.op("pool", lambda e: e.memset(VCMP[:], 0.0), writes=["VCMP"])
        for gl in range(2):
            P.op("pool", lambda e, gl=gl: e.memset(VCMP[:, :, gl * 65 + 64:gl * 65 + 65], 1.0), writes=["VCMP"])
        bias_t = Tn("bias_t", [128, 2], F32)
        xh = Tn("xh", [128, 512], F32)
        x2 = Tn("x2", [128, 512], F32)
        gT = Tn("gT", [128, 2, 512], BF16)
        srcs = [KVC, KVC]
        srcn = ["KVC", "KVC"]
        for k in range(2):
            load_cmp_weights(k)
            for gl in range(2):
                pr = slice(64 * gl, 64 * gl + 64)
                wn = "W1b"
                for hc in range(2):
                    pb, pbn = nps("I")
                    for pos in range(32):
                        P.op("pe", lambda e, k=k, hc=hc, pos=pos, pb=pb, pr=pr: e.matmul(
                            pb[:, 0:1], lhsT=W1[k][pr, pos, hc * 128:(hc + 1) * 128], rhs=PEt[k][pr, pos:pos + 1],
                            start=(pos == 0), stop=(pos == 31)), reads=[wn, "PE_%d" % k], writes=[pbn])
                    P.op("act", lambda e, hc=hc, pb=pb: e.copy(out=bias_t[:, hc:hc + 1], in_=pb[:, 0:1]),
                         reads=[pbn], writes=["bias_t"])
                    ph, phn = nps("S")
                    for pos in range(32):
                        P.op("pe", lambda e, k=k, hc=hc, pos=pos, ph=ph, pr=pr: e.matmul(
                            ph[:, 0:NCMP], lhsT=W1[k][pr, pos, hc * 128:(hc + 1) * 128],
                            rhs=srcs[k][pr, pos:pos + 16 * (NCMP - 1) + 1:16],
                            start=(pos == 0), stop=(pos == 31)), reads=[wn, srcn[k]], writes=[phn])
                    P.op("act", lambda e, hc=hc, ph=ph: e.activation(
                        out=xh[:, 0:NCMP], in_=ph[:, 0:NCMP], func=AF.Identity, bias=bias_t[:, hc:hc + 1]),
                        reads=[phn, "bias_t"], writes=["xh"])
                    P.op("dve", lambda e: e.tensor_tensor(out=x2[:, 0:NCMP], in0=xh[:, 0:NCMP], in1=xh[:, 0:NCMP],
                                                          op=ALU.mult), reads=["xh"], writes=["x2"])
                    P.op("dve", lambda e: e.tensor_scalar(out=x2[:, 0:NCMP], in0=x2[:, 0:NCMP], scalar1=0.044715,
                                                          scalar2=1.0, op0=ALU.mult, op1=ALU.add),
                         reads=["x2"], writes=["x2"])
                    P.op("dve", lambda e: e.tensor_tensor(out=x2[:, 0:NCMP], in0=x2[:, 0:NCMP], in1=xh[:, 0:NCMP],
                                                          op=ALU.mult), reads=["x2", "xh"], writes=["x2"])
                    P.op("act", lambda e: e.activation(out=x2[:, 0:NCMP], in_=x2[:, 0:NCMP], func=AF.Sigmoid,
                                                       scale=1.5957691216), reads=["x2"], writes=["x2"])
                    P.op("dve", lambda e, hc=hc: e.tensor_tensor(out=gT[:, hc, 0:NCMP], in0=x2[:, 0:NCMP],
                                                                 in1=xh[:, 0:NCMP], op=ALU.mult),
                         reads=["x2", "xh"], writes=["gT"])
                if k == 0:
                    pk, pkn = nps("S")
                    for hc in range(2):
                        P.op("pe", lambda e, hc=hc, pk=pk: e.matmul(
                            pk[:, 0:NCMP], lhsT=W2[0][:, hc, :], rhs=gT[:, hc, 0:NCMP],
                            start=(hc == 0), stop=(hc == 1)), reads=["W2_0", "gT"], writes=[pkn])
                    P.op("act", lambda e, pk=pk, pr=pr: e.copy(out=KCMP[pr, 0:NCMP], in_=pk[pr, 0:NCMP]),
                         reads=[pkn], writes=["KCMP"])
                else:
                    for j in range(NCT):
                        rows = min(128, NCMP - 128 * j)
                        pv, pvn = nps("I")
                        for hc in range(2):
                            P.op("pe", lambda e, hc=hc, pv=pv, j=j, rows=rows: e.matmul(
                                pv[0:rows, 0:64], lhsT=gT[:, hc, 128 * j:128 * j + rows], rhs=W2[1][:, hc, 0:64],
                                start=(hc == 0), stop=(hc == 1)), reads=["W2_1", "gT"], writes=[pvn])
                        P.op("act", lambda e, pv=pv, j=j, rows=rows, gl=gl: e.copy(
                            out=VCMP[0:rows, j, gl * 65:gl * 65 + 64], in_=pv[0:rows, 0:64]),
                            reads=[pvn], writes=["VCMP"])

        Qt = Tn("Qt", [128, 4, 512], BF16)
        QRt = Tn("QRt", [128, 4, 512], BF16)
        Gt = Tn("Gt", [128, 4, 24], F32)
        ECMP = [Tn("ECMP%d" % i, [128, NCT, 512], BF16) for i in range(2)]
        NEB = 4
        Eb = [Tn("E%d" % i, [128, 512], BF16) for i in range(NEB)]
        ectr = [0]
        mulctr = [0]
        OT = [Tn("OT%d" % i, [128, 512], F32) for i in range(3)]
        octr = [0]
        OACC = Tn("OACC", [128, 4, 512], F32)
        OB = Tn("OB", [128, 512], BF16)
        IMP = [Tn("IMP%d" % i, [128, 4, 128], F32) for i in range(2)]
        XT = [Tn("XT%d" % i, [128, 512], BF16) for i in range(2)]
        smc = [Tn("smc%d" % i, [128, 4], F32) for i in range(4)]
        smi = [Tn("smi%d" % i, [128, 2], F32) for i in range(4)]
        tm = Tn("tm", [128, 128], F32)
        tm2 = Tn("tm2", [128, 128], F32)
        m8 = Tn("m8", [128, 16], F32)
        Xs = [Tn("Xs%d" % i, [128, 128], BF16) for i in range(8)]
        BR_CMP, BR_SEL, BR_WIN = 0, 1, 2
        tsl = slice(0, 512)
        LOOK = 2

        def make_units(gl, r, i, kind, ecmp_i):
            pr = slice(64 * gl, 64 * gl + 64)
            if kind == BR_CMP:
                tiles = [j for j in range(NCT) if 4 * j - i <= 0]
            elif kind == BR_SEL:
                tiles = list(range(0, 4 * i + 4))
            else:
                tiles = [kt for kt in range(4 * i - 4, 4 * i + 4) if kt >= 0]
            pO, pOn = nps("O")
            us = []
            for idx, kt in enumerate(tiles):
                mms = []
                mul = None
                if kind == BR_CMP:
                    mms.append((KCMP[pr, 128 * kt:128 * kt + 128], Qt[pr, r, :], ["KCMP", "Qt"]))
                    d = 4 * kt - i
                    if d >= -4:
                        mms.append((IDB[:], CMPM[:, d + 4, :], ["IDB", "CMPM"]))
                    V, Vn = VCMP[:, kt, gl * 65:gl * 65 + 65], "VCMP"
                    Et, Etn = ECMP[ecmp_i][:, kt, :], "ECMP%d" % ecmp_i
                elif kind == BR_SEL:
                    mms.append((KS[pr, 128 * kt:128 * kt + 128], QRt[pr, r, :], ["KS", "QRt"]))
                    mms.append((EB[:, 128 * kt:128 * kt + 128], XT[gl][:], ["EBIG", "XT%d" % gl]))
                    d = kt - 4 * i
                    if d >= 0:
                        mul = CM[:, d + 4, :]
                    V, Vn = VS[:, kt, gl * 65:gl * 65 + 65], "VS"
                    Et = None
                else:
                    mms.append((KW[pr, 128 * kt:128 * kt + 128], QRt[pr, r, :], ["KW", "QRt"]))
                    mul = CM[:, kt - 4 * i + 4, :]
                    V, Vn = VW[:, kt, gl * 65:gl * 65 + 65], "VW"
                    Et = None
                us.append({"mms": mms, "V": V, "Vn": Vn, "Et": Et, "Etn": Etn if Et is not None else None, "mul": mul,
                           "pO": pO, "pOn": pOn, "first": idx == 0, "last": idx == len(tiles) - 1, "post": []})
            return us, pO, pOn, tiles

        def emit_S(u):
            pS, pSn = nps("S")
            u["pS"], u["pSn"] = pS, pSn
            nm = len(u["mms"])
            for mi, (l, rh, rd) in enumerate(u["mms"]):
                P.op("pe", lambda e, l=l, rh=rh, pS=pS, mi=mi, nm=nm: e.matmul(
                    pS[:, tsl], lhsT=l, rhs=rh, start=(mi == 0), stop=(mi == nm - 1)), reads=rd, writes=[pSn])

        def emit_EPV(u):
            if u["Et"] is None:
                bi = ectr[0] % NEB
                ectr[0] += 1
                Et, Etn = Eb[bi][:], "E%d" % bi
            else:
                Et, Etn = u["Et"], u["Etn"]
            pS, pSn, pO, pOn = u["pS"], u["pSn"], u["pO"], u["pOn"]
            P.op("act", lambda e, Et=Et, pS=pS: e.activation(out=Et, in_=pS[:, tsl], func=AF.Exp, scale=0.125),
                 reads=[pSn], writes=[Etn])
            if u["mul"] is not None:
                meng = ("pool", "dve")[mulctr[0] % 2]
                mulctr[0] += 1
                P.op(meng, lambda e, Et=Et, mul=u["mul"]: e.tensor_tensor(out=Et, in0=Et, in1=mul, op=ALU.mult),
                     reads=[Etn, "CM"], writes=[Etn])
            P.op("pe", lambda e, V=u["V"], Et=Et, pO=pO, f=u["first"], l=u["last"]: e.matmul(
                pO[0:65, tsl], lhsT=V, rhs=Et, start=f, stop=l), reads=[u["Vn"], Etn], writes=[pOn])

        def combine_a(pO, pOn):
            bi = octr[0] % 3
            octr[0] += 1
            ot, otn = OT[bi], "OT%d" % bi
            P.op("act", lambda e, ot=ot, pO=pO: e.copy(out=ot[0:65, :], in_=pO[0:65, :]), reads=[pOn], writes=[otn])
            return ot, otn

        def combine_b(gl, r, ot, otn, br, first):
            h = gl * 4 + r
            for sub in range(4):
                sm, smn = smc[sub], "smc%d" % sub
                pT, pTn = nps("T")
                P.op("pe", lambda e, ot=ot, pT=pT, sub=sub: e.matmul(
                    pT[:, 0:65], lhsT=ot[0:65, sub * 128:(sub + 1) * 128], rhs=IDF[0:65, 0:65],
                    start=True, stop=True), reads=[otn, "IDF"], writes=[pTn])
                P.op("dve", lambda e, pT=pT, sm=sm: e.tensor_scalar(out=sm[:, 0:1], in0=pT[:, 64:65], scalar1=1e-30,
                                                                    scalar2=None, op0=ALU.max),
                     reads=[pTn], writes=[smn])
                P.op("dve", lambda e, sm=sm: e.reciprocal(out=sm[:, 1:2], in_=sm[:, 0:1]), reads=[smn], writes=[smn])
                P.op("dve", lambda e, sub=sub, h=h, br=br, sm=sm: e.tensor_tensor(
                    out=sm[:, 2:3], in0=sm[:, 1:2], in1=Gt[:, sub, h * 3 + br:h * 3 + br + 1], op=ALU.mult),
                    reads=[smn, "Gt"], writes=[smn])
                dst = OACC[:, sub, h * 64:(h + 1) * 64]
                dn = "OACC%d_%d" % (sub, h)
                if first:
                    P.op("dve", lambda e, pT=pT, dst=dst, sm=sm: e.tensor_scalar(
                        out=dst, in0=pT[:, 0:64], scalar1=sm[:, 2:3], scalar2=None, op0=ALU.mult),
                        reads=[pTn, smn], writes=[dn])
                else:
                    P.op("dve", lambda e, pT=pT, dst=dst, sm=sm: e.scalar_tensor_tensor(
                        out=dst, in0=pT[:, 0:64], scalar=sm[:, 2:3], in1=dst, op0=ALU.mult, op1=ALU.add),
                        reads=[pTn, smn, dn], writes=[dn])

        def imp_head(gl, r, tiles, ecmp_i):
            for sub in range(4):
                sm, smn = smi[sub], "smi%d" % sub
                pI, pIn = nps("I")
                for idx, j in enumerate(tiles):
                    P.op("pe", lambda e, j=j, sub=sub, pI=pI, idx=idx, nt=len(tiles): e.matmul(
                        pI[:, 0:129], lhsT=ECMP[ecmp_i][:, j, sub * 128:(sub + 1) * 128], rhs=OV[:, j, :],
                        start=(idx == 0), stop=(idx == nt - 1)), reads=["ECMP%d" % ecmp_i, "OV"], writes=[pIn])
                P.op("dve", lambda e, pI=pI, sm=sm: e.tensor_scalar(out=sm[:, 0:1], in0=pI[:, 128:129], scalar1=1e-30,
                                                                    scalar2=None, op0=ALU.max),
                     reads=[pIn], writes=[smn])
                P.op("dve", lambda e, sm=sm: e.reciprocal(out=sm[:, 1:2], in_=sm[:, 0:1]), reads=[smn], writes=[smn])
                imn = "IMP%d_%d" % (gl, sub)
                if r == 0:
                    P.op("dve", lambda e, pI=pI, sub=sub, sm=sm: e.tensor_scalar(
                        out=IMP[gl][:, sub, :], in0=pI[:, 0:128], scalar1=sm[:, 1:2], scalar2=None, op0=ALU.mult),
                        reads=[pIn, smn], writes=[imn])
                else:
                    P.op("dve", lambda e, pI=pI, sub=sub, sm=sm: e.scalar_tensor_tensor(
                        out=IMP[gl][:, sub, :], in0=pI[:, 0:128], scalar=sm[:, 1:2], in1=IMP[gl][:, sub, :],
                        op0=ALU.mult, op1=ALU.add), reads=[pIn, smn, imn], writes=[imn])

        def select_a(gl, i, sub):
            sb = 8 * i + 2 * sub
            msl = slice(128 - sb, 256 - sb)
            xs, xsn = Xs[gl * 4 + sub], "Xs%d" % (gl * 4 + sub)
            imn = "IMP%d_%d" % (gl, sub)
            P.op("dve", lambda e, sub=sub, msl=msl: e.tensor_tensor(
                out=tm[:], in0=IMP[gl][:, sub, :], in1=M01[:, msl], op=ALU.mult), reads=[imn, "M01"], writes=["tm"])
            P.op("dve", lambda e, msl=msl: e.tensor_tensor(out=tm[:], in0=tm[:], in1=ADDM[:, msl], op=ALU.add),
                 reads=["tm", "ADDM"], writes=["tm"])
            P.op("dve", lambda e: e.memset(tm[:, 0:1], 1e6), reads=["tm"], writes=["tm"])
            P.op("dve", lambda e: e.max(out=m8[:, 0:8], in_=tm[:]), reads=["tm"], writes=["m8"])
            P.op("dve", lambda e: e.match_replace(out=tm2[:], in_to_replace=m8[:, 0:8], in_values=tm[:],
                                                  imm_value=-1e9), reads=["tm", "m8"], writes=["tm2"])
            P.op("dve", lambda e: e.max(out=m8[:, 8:16], in_=tm2[:]), reads=["tm2"], writes=["m8"])
            P.op("dve", lambda e: e.tensor_scalar(out=tm2[:], in0=tm[:], scalar1=m8[:, 15:16], scalar2=None,
                                                  op0=ALU.is_ge), reads=["tm", "m8"], writes=["tm2"])
            P.op("dve", lambda e, xs=xs: e.tensor_scalar(out=xs[:], in0=tm2[:], scalar1=-1.0, scalar2=-NEG,
                                                         op0=ALU.add, op1=ALU.mult), reads=["tm2"], writes=[xsn])

        def select_b(gl, sub):
            xs, xsn = Xs[gl * 4 + sub], "Xs%d" % (gl * 4 + sub)
            pT, pTn = nps("T")
            P.op("pe", lambda e, pT=pT, xs=xs: e.matmul(pT[:, 0:128], lhsT=xs[:], rhs=IDB[:], start=True, stop=True),
                 reads=[xsn, "IDB"], writes=[pTn])
            P.op("act", lambda e, pT=pT, sub=sub, gl=gl: e.copy(out=XT[gl][:, sub * 128:(sub + 1) * 128],
                                                                in_=pT[:, 0:128]),
                 reads=[pTn], writes=["XT%d" % gl])

        ecmp_ctr = [0]
        for i in range(NTT):
            t0 = 512 * i
            P.dma(Qt[:], Qd[:, :, t0:t0 + 512], writes=["Qt"])
            P.dma(QRt[:], QRd[:, :, t0:t0 + 512], writes=["QRt"], q="pool")
            P.dma(Gt[:], GAd[t0:t0 + 512, :].rearrange("(s p) c -> p s c", p=128), writes=["Gt"])
            units = []
            for gl in range(2):
                for r in range(4):
                    ei = ecmp_ctr[0] % 2
                    ecmp_ctr[0] += 1
                    us, pO, pOn, tiles = make_units(gl, r, i, BR_CMP, ei)
                    box = {}

                    def pa(pO=pO, pOn=pOn, box=box):
                        box["ot"] = combine_a(pO, pOn)
                    us[-1]["post"].append((0, pa))
                    us[-1]["post"].append((1, lambda gl=gl, r=r, tiles=tiles, ei=ei: imp_head(gl, r, tiles, ei)))
                    us[-1]["post"].append((2, lambda gl=gl, r=r, box=box: combine_b(gl, r, box["ot"][0], box["ot"][1],
                                                                                    BR_CMP, True)))
                    if r == 3:
                        for sub in range(4):
                            us[-1]["post"].append((2, lambda gl=gl, sub=sub: select_a(gl, i, sub)))
                            us[-1]["post"].append((6 + sub, lambda gl=gl, sub=sub: select_b(gl, sub)))
                    units += us
            for kind in (BR_WIN, BR_SEL):
                for gl in range(2):
                    for r in range(4):
                        us, pO, pOn, _ = make_units(gl, r, i, kind, 0)
                        box = {}

                        def pa(pO=pO, pOn=pOn, box=box):
                            box["ot"] = combine_a(pO, pOn)
                        us[-1]["post"].append((0, pa))
                        us[-1]["post"].append((2, lambda gl=gl, r=r, box=box, kind=kind: combine_b(
                            gl, r, box["ot"][0], box["ot"][1], kind, False)))
                        units += us
            N = len(units)
            deferred = {}
            maxdelay = 12
            for n in range(N + LOOK + maxdelay):
                if n < N:
                    emit_S(units[n])
                m = n - LOOK
                if m in deferred:
                    for fn in deferred.pop(m):
                        fn()
                if 0 <= m < N:
                    emit_EPV(units[m])
                    for (dl, fn) in units[m]["post"]:
                        if dl == 0:
                            fn()
                        else:
                            deferred.setdefault(m + dl, []).append(fn)
            assert not deferred, deferred.keys()
            oacc_names = ["OACC%d_%d" % (sub, h) for sub in range(4) for h in range(8)]
            for sub in range(4):
                P.op("act", lambda e, sub=sub: e.copy(out=OB[:], in_=OACC[:, sub, :]),
                     reads=["OACC%d_%d" % (sub, h) for h in range(8)], writes=["OB"])
                P.dma(Od[t0 + sub * 128:t0 + sub * 128 + 128, :], OB[:], reads=["OB"])
        P.emit()
    return nc


def nsa_b_inputs(pr, gate, params, T, gp, consts):
    q = pr[:, 0:1024].reshape(T, 4, 4, 64)
    qr = pr[:, 2560:3584].reshape(T, 4, 4, 64)
    kv = pr[:, 1024:2560].reshape(T, 6, 4, 64)
    gs = slice(2 * gp, 2 * gp + 2)

    def qlay(z):
        return np.ascontiguousarray(z[:, gs].transpose(1, 3, 2, 0).reshape(128, 4, T))

    def klay(z):
        return np.ascontiguousarray(z[:, gs].transpose(1, 2, 0).reshape(128, T))

    def vlay(z):
        o = np.ones((T, 2, 65), dtype=z.dtype)
        o[:, :, 0:64] = z[:, gs]
        return o.reshape(T, 130)
    d = {"Q": qlay(q), "QR": qlay(qr), "KC": klay(kv[:, 0]), "VC": klay(kv[:, 1]), "KS": klay(kv[:, 2]),
         "VS": vlay(kv[:, 3]), "KW": klay(kv[:, 4]), "VW": vlay(kv[:, 5]),
         "GA": np.ascontiguousarray(gate.reshape(T, 16, 3)[:, 8 * gp:8 * gp + 8].reshape(T, 24))}
    for nm, w1, w2, pe in (("K", params["cmp_k_w1"], params["cmp_k_w2"], params["pe_k"]),
                           ("V", params["cmp_v_w1"], params["cmp_v_w2"], params["pe_v"])):
        w1r = w1.reshape(32, 64, 256).transpose(1, 0, 2).reshape(64, 32 * 256)
        d["W1" + nm] = np.ascontiguousarray(np.concatenate([w1r, w1r], 0))
        w2r = w2.reshape(2, 128, 64).transpose(1, 0, 2)
        d["W2" + nm] = np.ascontiguousarray(np.concatenate([w2r, w2r], 2).reshape(128, 256))
        d["PE" + nm] = np.ascontiguousarray(np.concatenate([pe.T, pe.T], 0))
    d.update(consts)
    return d


D = 1024
EPS = 1e-6


def build_proj_a(NT, NOUT, f32_cols=None, name="pa"):
    nc = bass.Bass("TRN2", target_bir_lowering=False)
    xT = nc.dram_tensor("xT", [D, NT], F32, kind="ExternalInput").ap()
    w_in = nc.dram_tensor("w_in", [D, NOUT], F32, kind="ExternalInput").ap()
    gn = nc.dram_tensor("gn", [128, 8], F32, kind="ExternalInput").ap()
    pr = nc.dram_tensor("pr", [NT, NOUT], BF16, kind="ExternalOutput").ap()
    if f32_cols:
        nf = f32_cols[1] - f32_cols[0]
        pf = nc.dram_tensor("pf", [NT, nf], F32, kind="ExternalOutput").ap()
    P = Prog(nc)
    with contextlib.ExitStack() as es:
        T = lambda name, shape, dt: es.enter_context(nc.sbuf_tensor(name, shape, dt))
        nextps = make_ps(es, nc, 7)
        stg = [T("stg%d" % i, [128, 512], F32) for i in range(2)]
        w_t = load_cast_weight(P, es, nc, w_in, D, NOUT, "w_t", stg, ["stg0", "stg1"])
        fr = Front(P, es, nc, gn, nps=nextps)
        OUT = T("OUT", [128, NOUT], BF16)
        if f32_cols:
            OF = T("OF", [128, nf], F32)
        blocks = [(c0, min(512, NOUT - c0)) for c0 in range(0, NOUT, 512)]
        for t0 in range(0, NT, 512):
            n = min(512, NT - t0)
            fr.run(xT[:, t0:t0 + n], n)
            for s0 in range(0, n, 128):
                tt = t0 + s0
                for bi, (c0, cwid) in enumerate(blocks):
                    pt, pn = nextps()
                    for kc in range(8):
                        P.op("pe", lambda e, kc=kc, pt=pt, c0=c0, cwid=cwid, s0=s0: e.matmul(
                            pt[:, 0:cwid], lhsT=fr.h_t[:, kc, s0:s0 + 128], rhs=w_t[:, kc, c0:c0 + cwid],
                            start=(kc == 0), stop=(kc == 7)), reads=["h_t", "w_t"], writes=[pn])
                    if bi % 2 == 0:
                        P.op("act", lambda e, pt=pt, c0=c0, cwid=cwid: e.copy(out=OUT[:, c0:c0 + cwid],
                                                                              in_=pt[:, 0:cwid]),
                             reads=[pn], writes=["OUT"])
                    else:
                        P.op("dve", lambda e, pt=pt, c0=c0, cwid=cwid: e.tensor_copy(out=OUT[:, c0:c0 + cwid],
                                                                                     in_=pt[:, 0:cwid]),
                             reads=[pn], writes=["OUT"])
                    if f32_cols and c0 <= f32_cols[0] and f32_cols[1] <= c0 + cwid:
                        a, b = f32_cols[0] - c0, f32_cols[1] - c0
                        P.op("dve", lambda e, pt=pt, a=a, b=b: e.tensor_copy(out=OF[:], in_=pt[:, a:b]),
                             reads=[pn], writes=["OF"])
                P.dma(pr[tt:tt + 128, :], OUT[:], reads=["OUT"])
                if f32_cols:
                    P.dma(pf[tt:tt + 128, :], OF[:], reads=["OF"])
        P.emit()
    return nc


def mlstm_consts():
    import ml_dtypes
    s = np.arange(128)[:, None]
    t = np.arange(128)[None, :]
    tri = (s <= t).astype(np.float32)
    sel = np.zeros((128, 128), np.float32)
    sel[127, :] = 1.0
    return {"TRI": tri, "SEL": sel, "MASK2": tri.copy(),
            "IDB": np.eye(128, dtype=np.float32).astype(ml_dtypes.bfloat16)}


def build_mlstm_b(T):
    nc = bass.Bass("TRN2", target_bir_lowering=False)
    din = lambda name, shape, dt: nc.dram_tensor(name, shape, dt, kind="ExternalInput").ap()
    QTd = din("QT", [128, 2, T + 3], BF16)
    KTd = din("KT", [128, 2, T + 3], BF16)
    CWd = din("CW", [128, 16], F32)
    CBd = din("CB", [128, 4], F32)
    V1d = din("V1", [T, 4 * 129], BF16)
    OGd = din("OG", [T, 512], BF16)
    IGd = din("IG", [T, 4], F32)
    FGd = din("FG", [T, 4], F32)
    BGd = din("BG", [128, 8], F32)
    NGd = din("NG", [128, 512], F32)
    TRId = din("TRI", [128, 128], F32)
    SELd = din("SEL", [128, 128], F32)
    M2d = din("MASK2", [128, 128], F32)
    IDBd = din("IDB", [128, 128], BF16)
    Od = nc.dram_tensor("O", [T, 512], BF16, kind="ExternalOutput").ap()
    P = Prog(nc)
    with contextlib.ExitStack() as es:
        Tn = lambda name, shape, dt: es.enter_context(nc.sbuf_tensor("sb_" + name, shape, dt))

        def cl(name, dram, shape, dt, q="sp"):
            t = Tn(name, shape, dt)
            P.dma(t[:], dram, writes=[name], q=q)
            return t
        CW = cl("CW", CWd, [128, 16], F32)
        CB = cl("CB", CBd, [128, 4], F32)
        BG = cl("BG", BGd, [128, 8], F32)
        NG = cl("NG", NGd, [128, 512], F32)
        TRI = cl("TRI", TRId, [128, 128], F32)
        SEL = cl("SEL", SELd, [128, 128], F32)
        M2 = cl("MASK2", M2d, [128, 128], F32)
        IDB = cl("IDB", IDBd, [128, 128], BF16)
        nps_ = make_ps(es, nc, 8)
        XQ = Tn("XQ", [128, 2, 515], BF16)
        XK = Tn("XK", [128, 2, 515], BF16)
        acc = Tn("acc", [128, 512], F32)
        QS = Tn("QS", [128, 2, 512], BF16)
        KSs = Tn("KSs", [128, 2, 512], BF16)
        V1 = Tn("V1", [128, 4, 4 * 129], BF16)
        OG = Tn("OG", [128, 4, 512], BF16)
        IG = Tn("IG", [128, 4, 4], F32)
        FG = Tn("FG", [128, 4, 4], F32)
        lf = Tn("lf", [128, 4], F32)
        bcs = Tn("bcs", [128, 4], F32)
        ea = Tn("ea", [128, 4], F32)
        eb = Tn("eb", [128, 4], F32)
        GE = Tn("GE", [128, 4], F32)
        KW = Tn("KW", [128, 4, 128], BF16)
        ATTh = [Tn("ATT%d" % h, [128, 128], BF16) for h in range(4)]
        smH = [Tn("smH%d" % h, [128, 8], F32) for h in range(4)]
        hhH = [Tn("hhH%d" % h, [128, 128], F32) for h in range(4)]
        sqH = [Tn("sqH%d" % h, [128, 128], F32) for h in range(4)]
        ST32 = Tn("ST32", [128, 4, 129], F32)
        STB = Tn("STB", [128, 4, 129], BF16)
        sm = Tn("sm", [128, 8], F32)
        hh = Tn("hh", [128, 128], F32)
        sq = Tn("sq", [128, 128], F32)
        sg = Tn("sg", [128, 512], F32)
        OUT = Tn("OUT", [128, 512], BF16)
        P.op("pool", lambda e: e.memset(ST32[:], 0.0), writes=["ST32_%d" % h for h in range(4)])
        P.op("pool", lambda e: e.memset(STB[:], 0.0), writes=["STB_%d" % h for h in range(4)])

        def conv_silu(X, Xn, qk, dst, dstn, scale):
            for pair in range(2):
                ci = (qk * 2 + pair) * 4
                P.op("dve", lambda e, pair=pair, ci=ci: e.tensor_scalar(
                    out=acc[:], in0=X[:, pair, 0:512], scalar1=CW[:, ci:ci + 1], scalar2=None, op0=ALU.mult),
                    reads=[Xn, "CW"], writes=["acc"])
                for j in (1, 2, 3):
                    P.op("dve", lambda e, pair=pair, ci=ci, j=j: e.scalar_tensor_tensor(
                        out=acc[:], in0=X[:, pair, j:j + 512], scalar=CW[:, ci + j:ci + j + 1], in1=acc[:],
                        op0=ALU.mult, op1=ALU.add), reads=[Xn, "CW", "acc"], writes=["acc"])
                P.op("act", lambda e, pair=pair, qk=qk: e.activation(
                    out=acc[:], in_=acc[:], func=AF.Silu, bias=CB[:, qk * 2 + pair:qk * 2 + pair + 1]),
                    reads=["acc", "CB"], writes=["acc"])
                P.op("pool", lambda e, pair=pair: e.tensor_scalar(
                    out=dst[:, pair, :], in0=acc[:], scalar1=scale, scalar2=None, op0=ALU.mult),
                    reads=["acc"], writes=[dstn])

        for t0 in range(0, T, 512):
            P.dma(XQ[:], QTd[:, :, t0:t0 + 515], writes=["XQ"])
            P.dma(XK[:], KTd[:, :, t0:t0 + 515], writes=["XK"], q="pool")
            P.dma(V1[:], V1d[t0:t0 + 512, :].rearrange("(s p) c -> p s c", p=128), writes=["V1"])
            P.dma(OG[:], OGd[t0:t0 + 512, :].rearrange("(s p) c -> p s c", p=128), writes=["OG"], q="pool")
            P.dma(IG[:], IGd[t0:t0 + 512, :].rearrange("(s p) c -> p s c", p=128), writes=["IG"])
            P.dma(FG[:], FGd[t0:t0 + 512, :].rearrange("(s p) c -> p s c", p=128), writes=["FG"])
            conv_silu(XQ, "XQ", 0, QS, "QS", 1.0)
            conv_silu(XK, "XK", 1, KSs, "KSs", 0.125)
            for sub in range(4):
                cs = slice(sub * 128, sub * 128 + 128)
                P.op("dve", lambda e, sub=sub: e.tensor_tensor(out=lf[:], in0=FG[:, sub, :], in1=BG[:, 4:8],
                                                               op=ALU.add), reads=["FG", "BG"], writes=["lf"])
                P.op("act", lambda e: e.activation(out=lf[:], in_=lf[:], func=AF.Sigmoid), reads=["lf"], writes=["lf"])
                P.op("act", lambda e: e.activation(out=lf[:], in_=lf[:], func=AF.Ln), reads=["lf"], writes=["lf"])
                pb, pbn = nps_()
                P.op("pe", lambda e, pb=pb: e.matmul(pb[:, 0:4], lhsT=TRI[:], rhs=lf[:], start=True, stop=True),
                     reads=["TRI", "lf"], writes=[pbn])
                P.op("act", lambda e, pb=pb: e.activation(out=eb[:], in_=pb[:, 0:4], func=AF.Exp),
                     reads=[pbn], writes=["eb"])
                P.op("dve", lambda e, sub=sub: e.tensor_tensor(out=bcs[:], in0=IG[:, sub, :], in1=BG[:, 0:4],
                                                               op=ALU.add), reads=["IG", "BG"], writes=["bcs"])
                P.op("dve", lambda e, pb=pb: e.tensor_tensor(out=bcs[:], in0=bcs[:], in1=pb[:, 0:4], op=ALU.subtract),
                     reads=["bcs", pbn], writes=["bcs"])
                P.op("act", lambda e: e.activation(out=ea[:], in_=bcs[:], func=AF.Exp), reads=["bcs"], writes=["ea"])
                pg, pgn = nps_()
                P.op("pe", lambda e, pg=pg: e.matmul(pg[:, 0:4], lhsT=SEL[:], rhs=eb[:], start=True, stop=True),
                     reads=["SEL", "eb"], writes=[pgn])
                P.op("act", lambda e, pg=pg: e.copy(out=GE[:], in_=pg[:, 0:4]), reads=[pgn], writes=["GE"])
                P.op("act", lambda e, sub=sub: e.activation(out=sg[:], in_=OG[:, sub, :], func=AF.Sigmoid),
                     reads=["OG"], writes=["sg"])
                for pair in range(2):
                    pk, pkn = nps_()
                    P.op("pe", lambda e, pk=pk, pair=pair, cs=cs: e.matmul(
                        pk[:, 0:128], lhsT=KSs[:, pair, cs], rhs=IDB[:], start=True, stop=True),
                        reads=["KSs", "IDB"], writes=[pkn])
                    for hl in range(2):
                        h = pair * 2 + hl
                        for dup in range(2):
                            P.op("dve", lambda e, pk=pk, hl=hl, h=h, dup=dup: e.tensor_scalar(
                                out=KW[:, h, dup * 64:dup * 64 + 64], in0=pk[:, hl * 64:hl * 64 + 64],
                                scalar1=ea[:, h:h + 1], scalar2=None, op0=ALU.mult),
                                reads=[pkn, "ea"], writes=["KW"])
                def head_chain(h, sub=sub, cs=cs):
                    pair, hl = h // 2, h % 2
                    pr = slice(64 * hl, 64 * hl + 64)
                    Vh = V1[:, sub, h * 129:(h + 1) * 129]
                    att, attn, smh, smn = ATTh[h], "ATT%d" % h, smH[h], "sm%d" % h
                    hhh, hhn, sqh, sqn = hhH[h], "hh%d" % h, sqH[h], "sq%d" % h
                    stn, stbn = "ST32_%d" % h, "STB_%d" % h
                    ps_, psn = nps_()
                    P.op("pe", lambda e: e.matmul(ps_[:, 0:128], lhsT=KSs[pr, pair, cs], rhs=QS[pr, pair, cs],
                                                  start=True, stop=True), reads=["KSs", "QS"], writes=[psn])
                    yield
                    P.op("dve", lambda e: e.scalar_tensor_tensor(
                        out=att[:], in0=ps_[:, 0:128], scalar=ea[:, h:h + 1], in1=M2[:], op0=ALU.mult, op1=ALU.mult),
                        reads=[psn, "ea", "MASK2"], writes=[attn])
                    yield
                    pn_, pnn = nps_()
                    P.op("pe", lambda e: e.matmul(pn_[:, 0:129], lhsT=att[:], rhs=Vh, start=True, stop=False),
                         reads=[attn, "V1"], writes=[pnn])
                    P.op("pe", lambda e: e.matmul(pn_[:, 0:129], lhsT=QS[pr, pair, cs], rhs=STB[pr, h, :],
                                                  start=False, stop=True), reads=["QS", stbn], writes=[pnn])
                    pkv, pkvn = nps_()
                    P.op("pe", lambda e: e.matmul(pkv[:, 0:129], lhsT=KW[:, h, :], rhs=Vh, start=True, stop=True),
                         reads=["KW", "V1"], writes=[pkvn])
                    yield
                    P.op("dve", lambda e: e.tensor_tensor(out=ST32[pr, h, :], in0=ST32[pr, h, :], in1=pkv[pr, 0:129],
                                                          op=ALU.add), reads=[pkvn, stn], writes=[stn])
                    P.op("dve", lambda e: e.tensor_scalar(out=ST32[pr, h, :], in0=ST32[pr, h, :],
                                                          scalar1=GE[pr, h:h + 1], scalar2=None, op0=ALU.mult),
                         reads=[stn, "GE"], writes=[stn])
                    P.op("act", lambda e: e.copy(out=STB[pr, h, :], in_=ST32[pr, h, :]), reads=[stn], writes=[stbn])
                    P.op("dve", lambda e: e.tensor_tensor(out=smh[:, 0:1], in0=pn_[:, 128:129], in1=eb[:, h:h + 1],
                                                          op=ALU.mult), reads=[pnn, "eb"], writes=[smn])
                    P.op("dve", lambda e: e.tensor_scalar(out=smh[:, 1:2], in0=smh[:, 0:1], scalar1=-1.0, scalar2=None,
                                                          op0=ALU.mult), reads=[smn], writes=[smn])
                    P.op("dve", lambda e: e.tensor_tensor(out=smh[:, 0:1], in0=smh[:, 0:1], in1=smh[:, 1:2], op=ALU.max),
                         reads=[smn], writes=[smn])
                    P.op("dve", lambda e: e.tensor_scalar(out=smh[:, 0:1], in0=smh[:, 0:1], scalar1=1.0, scalar2=None,
                                                          op0=ALU.max), reads=[smn], writes=[smn])
                    P.op("dve", lambda e: e.reciprocal(out=smh[:, 1:2], in_=smh[:, 0:1]), reads=[smn], writes=[smn])
                    P.op("dve", lambda e: e.tensor_tensor(out=smh[:, 2:3], in0=smh[:, 1:2], in1=eb[:, h:h + 1],
                                                          op=ALU.mult), reads=[smn, "eb"], writes=[smn])
                    P.op("dve", lambda e: e.tensor_scalar(out=hhh[:], in0=pn_[:, 0:128], scalar1=smh[:, 2:3],
                                                          scalar2=None, op0=ALU.mult), reads=[pnn, smn], writes=[hhn])
                    yield
                    P.op("dve", lambda e: e.tensor_tensor(out=sqh[:], in0=hhh[:], in1=hhh[:], op=ALU.mult),
                         reads=[hhn], writes=[sqn])
                    P.op("dve", lambda e: e.reduce_sum(out=smh[:, 3:4], in_=sqh[:], axis=AX.X), reads=[sqn], writes=[smn])
                    P.op("dve", lambda e: e.tensor_scalar(out=smh[:, 3:4], in0=smh[:, 3:4], scalar1=1.0 / 128,
                                                          scalar2=EPS, op0=ALU.mult, op1=ALU.add),
                         reads=[smn], writes=[smn])
                    yield
                    P.op("act", lambda e: e.sqrt(out=smh[:, 3:4], in_=smh[:, 3:4]), reads=[smn], writes=[smn])
                    yield
                    P.op("dve", lambda e: e.reciprocal(out=smh[:, 4:5], in_=smh[:, 3:4]), reads=[smn], writes=[smn])
                    P.op("dve", lambda e: e.scalar_tensor_tensor(
                        out=sqh[:], in0=hhh[:], scalar=smh[:, 4:5], in1=NG[:, h * 128:(h + 1) * 128],
                        op0=ALU.mult, op1=ALU.mult), reads=[hhn, smn, "NG"], writes=[sqn])
                    P.op("dve", lambda e: e.tensor_tensor(
                        out=OUT[:, h * 128:(h + 1) * 128], in0=sqh[:], in1=sg[:, h * 128:(h + 1) * 128], op=ALU.mult),
                        reads=[sqn, "sg"], writes=["OUT%d" % h])
                gens = [head_chain(h) for h in range(4)]
                while gens:
                    for g in list(gens):
                        try:
                            next(g)
                        except StopIteration:
                            gens.remove(g)
                tt = t0 + sub * 128
                P.dma(Od[tt:tt + 128, :], OUT[:], reads=["OUT%d" % h for h in range(4)])
        P.emit()
    return nc


def mlstm_b_inputs(pr, pf, params, T, hh_, consts):
    hs = slice(4 * hh_, 4 * hh_ + 4)
    qk = pr[:, 0:1024]
    q = qk[:, 0:512].reshape(T, 8, 64)[:, hs]
    k = qk[:, 512:1024].reshape(T, 8, 64)[:, hs]

    def lay(z):
        o = np.zeros((128, 2, T + 3), dtype=z.dtype)
        o[:, :, 3:] = z.reshape(T, 2, 2, 64).transpose(2, 3, 1, 0).reshape(128, 2, T)
        return o
    cw = params["conv_w"]
    cb = params["conv_b"]
    CW = np.zeros((128, 16), np.float32)
    CB = np.zeros((128, 4), np.float32)
    for qki in range(2):
        for pair in range(2):
            for hl in range(2):
                head = 4 * hh_ + pair * 2 + hl
                f0 = qki * 512 + head * 64
                CW[hl * 64:(hl + 1) * 64, (qki * 2 + pair) * 4:(qki * 2 + pair) * 4 + 4] = cw[:, f0:f0 + 64].T
                CB[hl * 64:(hl + 1) * 64, qki * 2 + pair] = cb[f0:f0 + 64]
    v = pr[:, 1024:2048].reshape(T, 8, 128)[:, hs]
    V1 = np.ones((T, 4, 129), dtype=pr.dtype)
    V1[:, :, 0:128] = v
    og = pr[:, 2064:3088].reshape(T, 8, 128)[:, hs].reshape(T, 512)
    bg = params["b_gates"]
    BG = np.tile(np.concatenate([bg[0:8][hs], bg[8:16][hs]])[None], (128, 1)).astype(np.float32)
    NG = np.tile(params["norm"].reshape(8, 128)[hs].reshape(1, 512), (128, 1)).astype(np.float32)
    d = {"QT": lay(q), "KT": lay(k), "CW": CW, "CB": CB, "V1": V1.reshape(T, 516),
         "OG": np.ascontiguousarray(og), "IG": np.ascontiguousarray(pf[:, 0:8][:, hs]),
         "FG": np.ascontiguousarray(pf[:, 8:16][:, hs]), "BG": BG, "NG": NG}
    d.update(consts)
    return d


D = 1024
EPS = 1e-6
GN_EPS = 64e-5
DECAY_C = -0.6065306597126334


def build_rwkv_a(NT):
    nc = bass.Bass("TRN2", target_bir_lowering=False)
    din = lambda name, shape, dt=F32: nc.dram_tensor(name, shape, dt, kind="ExternalInput").ap()
    xT = din("xT", [D, NT + 1])
    gn = din("gn", [128, 8])
    mu = din("mu", [128, 48])
    Wd = {n: din(n, [D, D]) for n in ("w_r", "w_k", "w_v")}
    w_w1, a_w1, g_w1 = din("w_w1", [D, 64]), din("a_w1", [D, 64]), din("g_w1", [D, 128])
    w_w2, a_w2, g_w2 = din("w_w2", [64, D]), din("a_w2", [64, D]), din("g_w2", [128, D])
    BC = din("BC", [128, 5 * D])
    OB = nc.dram_tensor("OB", [NT, 6 * D], BF16, kind="ExternalOutput").ap()
    OLW = nc.dram_tensor("OLW", [NT, D], F32, kind="ExternalOutput").ap()
    OBS = nc.dram_tensor("OBS", [NT, 16], F32, kind="ExternalOutput").ap()
    P = Prog(nc)
    with contextlib.ExitStack() as es:
        T = lambda name, shape, dt: es.enter_context(nc.sbuf_tensor("sb_" + name, shape, dt))
        nextps = make_ps(es, nc, 7)
        stg = [T("stg%d" % i, [128, 512], F32) for i in range(2)]
        stgn = ["stg0", "stg1"]
        Wt = {n: load_cast_weight(P, es, nc, Wd[n], D, D, n, stg, stgn) for n in ("w_r", "w_k", "w_v")}
        W1 = {"w": load_cast_weight(P, es, nc, w_w1, D, 64, "w_w1", stg, stgn),
              "a": load_cast_weight(P, es, nc, a_w1, D, 64, "a_w1", stg, stgn),
              "g": load_cast_weight(P, es, nc, g_w1, D, 128, "g_w1", stg, stgn)}
        W2 = {}
        for nm, dr, kk_ in (("w", w_w2, 64), ("a", a_w2, 64), ("g", g_w2, 128)):
            t = T("w2" + nm, [128, D], BF16)
            for c0 in (0, 512):
                s, sn = stg[(c0 // 512) % 2], stgn[(c0 // 512) % 2]
                P.dma(s[0:kk_, :], dr[:, c0:c0 + 512], writes=[sn])
                P.op("act", lambda e, t=t, s=s, c0=c0, kk_=kk_: e.copy(out=t[0:kk_, c0:c0 + 512], in_=s[0:kk_, :]),
                     reads=[sn], writes=["w2" + nm])
            W2[nm] = t
        fr = Front(P, es, nc, gn, nps=nextps)
        mu_t = T("mu_t", [128, 48], F32)
        P.dma(mu_t[:], mu, writes=["mu_t"])
        BCt = T("BCt", [128, 5 * D], F32)
        P.dma(BCt[:], BC, writes=["BCt"], q="pool")
        Dt = T("Dt", [128, 8, 256], F32)
        XJ = [T("XJ%d" % j, [128, 8, 256], BF16) for j in range(6)]
        Rt, Kt, Vt = T("Rt", [128, D], F32), T("Kt", [128, D], F32), T("Vt", [128, D], F32)
        At, Lt, t1, t2 = T("At", [128, D], F32), T("Lt", [128, D], F32), T("t1", [128, D], F32), T("t2", [128, D], F32)
        L1 = {"w": T("L1w", [128, 128], BF16), "a": T("L1a", [128, 128], BF16), "g": T("L1g", [128, 128], BF16)}
        sm = T("sm", [128, 64], F32)
        OBt = T("OBt", [128, 6 * D], BF16)
        W0, A0, KK_, KA_, RK_ = (BCt[:, i * D:(i + 1) * D] for i in range(5))
        v3 = lambda ap: ap.rearrange("p (h d) -> p h d", d=64)

        for t0 in range(0, NT, 256):
            fr.run(xT[:, t0:t0 + 257], 257)
            h = fr.h_t
            P.op("dve", lambda e: e.tensor_tensor(out=Dt[:], in0=h[:, :, 0:256], in1=h[:, :, 1:257], op=ALU.subtract),
                 reads=["h_t"], writes=["Dt"])
            for j in range(6):
                for c in range(8):
                    eng = "dve"
                    P.op(eng, lambda e, j=j, c=c: e.scalar_tensor_tensor(
                        out=XJ[j][:, c, :], in0=Dt[:, c, :], scalar=mu_t[:, j * 8 + c:j * 8 + c + 1],
                        in1=h[:, c, 1:257], op0=ALU.mult, op1=ALU.add),
                        reads=["Dt", "mu_t", "h_t"], writes=["XJ%d" % j])
            for sub in range(2):
                cs = slice(sub * 128, sub * 128 + 128)
                tt = t0 + sub * 128

                def dense_tok(xj, w, wn, dst, dstn, evac):
                    for blk in range(2):
                        pt, pn = nextps()
                        for kc in range(8):
                            P.op("pe", lambda e, kc=kc, pt=pt, blk=blk, cs=cs, xj=xj, w=w: e.matmul(
                                pt[:, 0:512], lhsT=XJ[xj][:, kc, cs], rhs=w[:, kc, blk * 512:(blk + 1) * 512],
                                start=(kc == 0), stop=(kc == 7)), reads=["XJ%d" % xj, wn], writes=[pn])
                        evac(pt, pn, blk)
                dense_tok(0, Wt["w_r"], "w_r", Rt, "Rt", lambda pt, pn, blk: P.op(
                    "act", lambda e, pt=pt, blk=blk: e.copy(out=Rt[:, blk * 512:(blk + 1) * 512], in_=pt[:, 0:512]), reads=[pn], writes=["Rt"]))
                dense_tok(2, Wt["w_k"], "w_k", Kt, "Kt", lambda pt, pn, blk: P.op(
                    "act", lambda e, pt=pt, blk=blk: e.copy(out=Kt[:, blk * 512:(blk + 1) * 512], in_=pt[:, 0:512]), reads=[pn], writes=["Kt"]))
                dense_tok(3, Wt["w_v"], "w_v", Vt, "Vt", lambda pt, pn, blk: P.op(
                    "act", lambda e, pt=pt, blk=blk: e.copy(out=Vt[:, blk * 512:(blk + 1) * 512], in_=pt[:, 0:512]), reads=[pn], writes=["Vt"]))
                for nm, xj, kk_, fn in (("w", 1, 64, AF.Tanh), ("a", 4, 64, AF.Copy), ("g", 5, 128, AF.Sigmoid)):
                    pt, pn = nextps()
                    for kc in range(8):
                        P.op("pe", lambda e, kc=kc, pt=pt, nm=nm, xj=xj, kk_=kk_, cs=cs: e.matmul(
                            pt[0:kk_, 0:128], lhsT=W1[nm][:, kc, :], rhs=XJ[xj][:, kc, cs],
                            start=(kc == 0), stop=(kc == 7)), reads=["XJ%d" % xj, nm + "_w1"], writes=[pn])
                    if fn == AF.Copy:
                        P.op("act", lambda e, pt=pt, nm=nm, kk_=kk_: e.copy(out=L1[nm][0:kk_, :], in_=pt[0:kk_, 0:128]),
                             reads=[pn], writes=["L1" + nm])
                    else:
                        P.op("act", lambda e, pt=pt, nm=nm, kk_=kk_, fn=fn: e.activation(
                            out=L1[nm][0:kk_, :], in_=pt[0:kk_, 0:128], func=fn), reads=[pn], writes=["L1" + nm])
                for nm, kk_ in (("w", 64), ("a", 64), ("g", 128)):
                    for blk in range(2):
                        bs_ = slice(blk * 512, (blk + 1) * 512)
                        pt, pn = nextps()
                        P.op("pe", lambda e, pt=pt, nm=nm, kk_=kk_, bs_=bs_: e.matmul(
                            pt[:, 0:512], lhsT=L1[nm][0:kk_, :], rhs=W2[nm][0:kk_, bs_], start=True, stop=True),
                            reads=["L1" + nm, "w2" + nm], writes=[pn])
                        if nm == "w":
                            P.op("dve", lambda e, pt=pt, bs_=bs_: e.tensor_tensor(out=Lt[:, bs_], in0=pt[:, 0:512],
                                                                                  in1=W0[:, bs_], op=ALU.add),
                                 reads=[pn, "BCt"], writes=["Lt"])
                        elif nm == "a":
                            P.op("dve", lambda e, pt=pt, bs_=bs_: e.tensor_tensor(out=At[:, bs_], in0=pt[:, 0:512],
                                                                                  in1=A0[:, bs_], op=ALU.add),
                                 reads=[pn, "BCt"], writes=["At"])
                        else:
                            P.op("act", lambda e, pt=pt, bs_=bs_: e.copy(out=OBt[:, 5 * D + bs_.start:5 * D + bs_.stop],
                                                                         in_=pt[:, 0:512]), reads=[pn], writes=["OBt"])
                P.op("act", lambda e: e.activation(out=Lt[:], in_=Lt[:], func=AF.Sigmoid), reads=["Lt"], writes=["Lt"])
                P.op("pool", lambda e: e.tensor_scalar(out=Lt[:], in0=Lt[:], scalar1=DECAY_C, scalar2=None, op0=ALU.mult),
                     reads=["Lt"], writes=["Lt"])
                P.dma(OLW[tt:tt + 128, :], Lt[:], reads=["Lt"])
                P.op("act", lambda e: e.activation(out=At[:], in_=At[:], func=AF.Sigmoid), reads=["At"], writes=["At"])
                P.op("dve", lambda e: e.tensor_tensor(out=t1[:], in0=Kt[:], in1=KK_, op=ALU.mult),
                     reads=["Kt", "BCt"], writes=["t1"])
                P.op("pool", lambda e: e.tensor_tensor(out=t2[:], in0=t1[:], in1=t1[:], op=ALU.mult),
                     reads=["t1"], writes=["t2"])
                P.op("dve", lambda e: e.tensor_reduce(out=sm[:, 0:16], in_=v3(t2[:]), axis=AX.X, op=ALU.add),
                     reads=["t2"], writes=["sm"])
                P.op("act", lambda e: e.sqrt(out=sm[:, 0:16], in_=sm[:, 0:16]), reads=["sm"], writes=["sm"])
                P.op("dve", lambda e: e.tensor_scalar(out=sm[:, 0:16], in0=sm[:, 0:16], scalar1=1e-12, scalar2=None,
                                                      op0=ALU.max), reads=["sm"], writes=["sm"])
                P.op("dve", lambda e: e.reciprocal(out=sm[:, 16:32], in_=sm[:, 0:16]), reads=["sm"], writes=["sm"])
                P.op("dve", lambda e: e.tensor_tensor(out=v3(t1[:]), in0=v3(t1[:]),
                                                      in1=sm[:, 16:32].unsqueeze(2).to_broadcast([128, 16, 64]),
                                                      op=ALU.mult), reads=["t1", "sm"], writes=["t1"])
                P.op("act", lambda e: e.copy(out=OBt[:, 3 * D:4 * D], in_=t1[:]), reads=["t1"], writes=["OBt"])
                P.op("dve", lambda e: e.tensor_tensor(out=OBt[:, 4 * D:5 * D], in0=t1[:], in1=At[:], op=ALU.mult),
                     reads=["t1", "At"], writes=["OBt"])
                P.op("dve", lambda e: e.scalar_tensor_tensor(out=t2[:], in0=At[:], scalar=-1.0, in1=KA_,
                                                             op0=ALU.add, op1=ALU.mult),
                     reads=["At", "BCt"], writes=["t2"])
                P.op("dve", lambda e: e.tensor_tensor(out=t2[:], in0=t2[:], in1=Kt[:], op=ALU.mult),
                     reads=["t2", "Kt"], writes=["t2"])
                P.op("dve", lambda e: e.tensor_tensor(out=t2[:], in0=t2[:], in1=Kt[:], op=ALU.add),
                     reads=["t2", "Kt"], writes=["t2"])
                P.op("act", lambda e: e.copy(out=OBt[:, 1 * D:2 * D], in_=t2[:]), reads=["t2"], writes=["OBt"])
                P.op("dve", lambda e: e.tensor_tensor(out=t2[:], in0=t2[:], in1=Rt[:], op=ALU.mult),
                     reads=["t2", "Rt"], writes=["t2"])
                P.op("pool", lambda e: e.tensor_tensor(out=t2[:], in0=t2[:], in1=RK_, op=ALU.mult),
                     reads=["t2", "BCt"], writes=["t2"])
                P.op("dve", lambda e: e.tensor_reduce(out=sm[:, 32:48], in_=v3(t2[:]), axis=AX.X, op=ALU.add),
                     reads=["t2"], writes=["sm"])
                P.dma(OBS[tt:tt + 128, :], sm[:, 32:48], reads=["sm"])
                P.op("act", lambda e: e.copy(out=OBt[:, 0:D], in_=Rt[:]), reads=["Rt"], writes=["OBt"])
                P.op("pool", lambda e: e.tensor_copy(out=OBt[:, 2 * D:3 * D], in_=Vt[:]), reads=["Vt"], writes=["OBt"])
                P.dma(OB[tt:tt + 128, :], OBt[:], reads=["OBt"], q="pool")
        P.emit()
    return nc


def rwkv_a_inputs(xT_halo, p, gnv):
    BC = np.concatenate([np.tile(p[k].reshape(1, D), (128, 1)) for k in ("w0", "a0", "k_k", "k_a", "r_k")], axis=1)
    mu = np.concatenate([pack_vec(p["mu"][j]) for j in (0, 1, 2, 3, 4, 5)], axis=1)
    return {"xT": xT_halo, "gn": pack_vec(gnv), "mu": np.ascontiguousarray(mu), "w_r": p["w_r"], "w_k": p["w_k"],
            "w_v": p["w_v"], "w_w1": p["w_w1"], "a_w1": p["a_w1"], "g_w1": p["g_w1"], "w_w2": p["w_w2"],
            "a_w2": p["a_w2"], "g_w2": p["g_w2"], "BC": np.ascontiguousarray(BC.astype(np.float32))}


def rwkv_consts():
    import ml_dtypes
    s = np.arange(128)[:, None]
    t = np.arange(128)[None, :]
    up = (t > s).astype(np.float32)
    upe = (t >= s).astype(np.float32)
    return {"TRI": upe.copy(), "TRIS": up.copy(),
            "MS1": np.concatenate([-up, -upe], 1), "MS2": np.concatenate([up, upe], 1),
            "MS3": -(s > t).astype(np.float32),
            "IDB": np.eye(128, dtype=np.float32).astype(ml_dtypes.bfloat16), "IDF": np.eye(128, dtype=np.float32)}


def build_rwkv_b(T):
    nc = bass.Bass("TRN2", target_bir_lowering=False)
    din = lambda name, shape, dt=F32: nc.dram_tensor(name, shape, dt, kind="ExternalInput").ap()
    FMd = {n: din(n, [128, 4, T], BF16) for n in ("RT", "KT", "KKT", "BT")}
    LWd = din("LW", [T, 512])
    Vd = din("V", [T, 512], BF16)
    Gd = din("G", [T, 512], BF16)
    BSd = din("BS", [T, 8])
    LNd = din("LN", [128, 1024])
    Cd = {n: din(n, [128, w], dt) for n, w, dt in (("TRI", 128, F32), ("TRIS", 128, F32), ("MS1", 256, F32),
                                                    ("MS2", 256, F32), ("MS3", 128, F32), ("IDB", 128, BF16),
                                                    ("IDF", 128, F32))}
    Od = nc.dram_tensor("O", [T, 512], BF16, kind="ExternalOutput").ap()
    P = Prog(nc)
    with contextlib.ExitStack() as es:
        Tn = lambda name, shape, dt: es.enter_context(nc.sbuf_tensor("sb_" + name, shape, dt))
        C = {}
        for n, (w, dt) in (("TRI", (128, F32)), ("TRIS", (128, F32)), ("MS1", (256, F32)), ("MS2", (256, F32)),
                           ("MS3", (128, F32)), ("IDB", (128, BF16)), ("IDF", (128, F32))):
            C[n] = Tn(n, [128, w], dt)
            P.dma(C[n][:], Cd[n], writes=[n])
        LN = Tn("LN", [128, 1024], F32)
        P.dma(LN[:], LNd, writes=["LN"])
        nps_ = make_ps(es, nc, 8)
        FM = {n: Tn(n, [128, 4, 128], BF16) for n in ("RT", "KT", "KKT", "BT")}
        LW = Tn("LW", [128, 512], F32)
        Vt = Tn("Vt", [128, 512], BF16)
        Gt = Tn("Gt", [128, 512], BF16)
        BS = Tn("BS", [128, 8], F32)
        eg = [Tn("eg%d" % i, [128, 128], F32) for i in range(4)]
        egx = [Tn("egx%d" % i, [128, 128], F32) for i in range(4)]
        egi = [Tn("egi%d" % i, [128, 128], F32) for i in range(4)]
        KR = [Tn("KR%d" % i, [128, 256], BF16) for i in range(4)]
        KTl = [Tn("KTl%d" % i, [128, 128], BF16) for i in range(4)]
        BTl = [Tn("BTl%d" % i, [128, 128], BF16) for i in range(4)]
        hat = [Tn("hat%d" % i, [128, 128], BF16) for i in range(8)]
        KH = [Tn("KH%d" % i, [128, 128], BF16) for i in range(4)]
        BH = [Tn("BH%d" % i, [128, 128], BF16) for i in range(4)]
        A = [[Tn("A%d_%d" % (h, i), [128, 128], F32) for i in range(2)] for h in range(8)]
        AT = [[Tn("AT%d_%d" % (h, i), [128, 128], F32) for i in range(2)] for h in range(8)]
        PT = [Tn("PT%d" % h, [128, 128], F32) for h in range(8)]
        MVK = [Tn("MVK%d" % h, [128, 256], BF16) for h in range(8)]
        NAUB = [Tn("NAUB%d" % h, [128, 128], BF16) for h in range(8)]
        RHS = [Tn("RHS%d" % h, [128, 64], F32) for h in range(8)]
        Ub = [Tn("Ub%d" % h, [128, 64], BF16) for h in range(8)]
        S32 = Tn("S32", [128, 8, 64], F32)
        S0b = Tn("S0b", [128, 8, 64], BF16)
        yc = [Tn("yc%d" % h, [128, 64], F32) for h in range(8)]
        ysq = [Tn("ysq%d" % h, [128, 64], F32) for h in range(8)]
        sm = [Tn("sm%d" % h, [128, 4], F32) for h in range(8)]
        OUT = Tn("OUT", [128, 512], BF16)
        P.op("pool", lambda e: e.memset(S32[:], 0.0), writes=["S32_%d" % h for h in range(8)])
        P.op("pool", lambda e: e.memset(S0b[:], 0.0), writes=["S0b_%d" % h for h in range(8)])

        def pair_chain(pair):
            pc = slice(pair * 128, pair * 128 + 128)
            egn, egxn, egin = "eg%d" % pair, "egx%d" % pair, "egi%d" % pair
            krn, ktn, btn = "KR%d" % pair, "KTl%d" % pair, "BTl%d" % pair
            pc_, pcn = nps_()
            P.op("pe", lambda e: e.matmul(pc_[:, 0:128], lhsT=LW[:, pc], rhs=C["TRI"][:], start=True, stop=True),
                 reads=["LW", "TRI"], writes=[pcn])
            px_, pxn = nps_()
            P.op("pe", lambda e: e.matmul(px_[:, 0:128], lhsT=LW[:, pc], rhs=C["TRIS"][:], start=True, stop=True),
                 reads=["LW", "TRIS"], writes=[pxn])
            yield
            P.op("act", lambda e: e.activation(out=eg[pair][:], in_=pc_[:, 0:128], func=AF.Exp), reads=[pcn], writes=[egn])
            P.op("act", lambda e: e.activation(out=egi[pair][:], in_=pc_[:, 0:128], func=AF.Exp, scale=-1.0),
                 reads=[pcn], writes=[egin])
            P.op("act", lambda e: e.activation(out=egx[pair][:], in_=px_[:, 0:128], func=AF.Exp), reads=[pxn], writes=[egxn])
            yield
            P.op("dve", lambda e: e.tensor_tensor(out=KR[pair][:, 0:128], in0=FM["KKT"][:, pair, :], in1=egx[pair][:],
                                                  op=ALU.mult), reads=["KKT", egxn], writes=[krn])
            P.op("dve", lambda e: e.tensor_tensor(out=KR[pair][:, 128:256], in0=FM["RT"][:, pair, :], in1=eg[pair][:],
                                                  op=ALU.mult), reads=["RT", egn], writes=[krn])
            P.op("dve", lambda e: e.tensor_tensor(out=KTl[pair][:], in0=FM["KT"][:, pair, :], in1=egi[pair][:],
                                                  op=ALU.mult), reads=["KT", egin], writes=[ktn])
            P.op("dve", lambda e: e.tensor_tensor(out=BTl[pair][:], in0=FM["BT"][:, pair, :], in1=egi[pair][:],
                                                  op=ALU.mult), reads=["BT", egin], writes=[btn])
            for qi, (src, srcn, dst, dstn, sc) in enumerate(((KTl[pair], ktn, KH[pair], "KH%d" % pair, 1.0),
                                                             (BTl[pair], btn, BH[pair], "BH%d" % pair, -1.0))):
                ht, htn = hat[pair * 2 + qi], "hat%d" % (pair * 2 + qi)
                P.op("dve", lambda e, src=src, ht=ht: e.tensor_scalar(out=ht[:], in0=src[:], scalar1=eg[pair][:, 127:128],
                                                                      scalar2=None, op0=ALU.mult),
                     reads=[srcn, egn], writes=[htn])
                yield
                ph, phn = nps_()
                P.op("pe", lambda e, ph=ph, ht=ht: e.matmul(ph[:, 0:128], lhsT=ht[:], rhs=C["IDB"][:], start=True, stop=True),
                     reads=[htn, "IDB"], writes=[phn])
                yield
                P.op("act", lambda e, ph=ph, dst=dst, sc=sc: e.activation(out=dst[:], in_=ph[:, 0:128], func=AF.Copy,
                                                                          scale=sc), reads=[phn], writes=[dstn])

        def head_chain(h):
            pair, hl = h // 2, h % 2
            pr = slice(64 * hl, 64 * hl + 64)
            Vh = Vt[:, h * 64:(h + 1) * 64]
            krn, ktn, btn = "KR%d" % pair, "KTl%d" % pair, "BTl%d" % pair
            KRp, KTp, BTp = KR[pair], KTl[pair], BTl[pair]
            An = ["A%d_%d" % (h, i) for i in range(2)]
            ATn = ["AT%d_%d" % (h, i) for i in range(2)]
            PTn, mvkn, naubn, rhsn, ubn = "PT%d" % h, "MVK%d" % h, "NAUB%d" % h, "RHS%d" % h, "Ub%d" % h
            s32n, s0bn, ycn, ysqn, smn = "S32_%d" % h, "S0b_%d" % h, "yc%d" % h, "ysq%d" % h, "sm%d" % h
            Ah, ATh, PTh, smh, ych, ysqh = A[h], AT[h], PT[h], sm[h], yc[h], ysq[h]
            p1, p1n = nps_()
            P.op("pe", lambda e: e.matmul(p1[:, 0:256], lhsT=BTp[pr, :], rhs=KRp[pr, :], start=True, stop=True),
                 reads=[btn, krn], writes=[p1n])
            yield
            P.op("dve", lambda e: e.tensor_tensor(out=ATh[0][:], in0=p1[:, 0:128], in1=C["MS1"][:, 0:128], op=ALU.mult),
                 reads=[p1n, "MS1"], writes=[ATn[0]])
            P.op("dve", lambda e: e.tensor_tensor(out=NAUB[h][:], in0=p1[:, 128:256], in1=C["MS1"][:, 128:256],
                                                  op=ALU.mult), reads=[p1n, "MS1"], writes=[naubn])
            p3, p3n = nps_()
            P.op("pe", lambda e: e.matmul(p3[:, 0:128], lhsT=KRp[pr, 0:128], rhs=BTp[pr, :], start=True, stop=True),
                 reads=[btn, krn], writes=[p3n])
            yield
            P.op("dve", lambda e: e.tensor_tensor(out=Ah[0][:], in0=p3[:, 0:128], in1=C["MS3"][:], op=ALU.mult),
                 reads=[p3n, "MS3"], writes=[An[0]])
            P.op("dve", lambda e: e.tensor_tensor(out=PTh[:], in0=ATh[0][:], in1=C["IDF"][:], op=ALU.add),
                 reads=[ATn[0], "IDF"], writes=[PTn])
            p2, p2n = nps_()
            P.op("pe", lambda e: e.matmul(p2[:, 0:256], lhsT=KTp[pr, :], rhs=KRp[pr, :], start=True, stop=True),
                 reads=[ktn, krn], writes=[p2n])
            yield
            P.op("dve", lambda e: e.tensor_tensor(out=MVK[h][:], in0=p2[:, 0:256], in1=C["MS2"][:], op=ALU.mult),
                 reads=[p2n, "MS2"], writes=[mvkn])
            for k in range(6):
                a, b = k % 2, (k + 1) % 2
                pa, pan = nps_()
                P.op("pe", lambda e, pa=pa, a=a: e.matmul(pa[:, 0:128], lhsT=ATh[a][:], rhs=Ah[a][:], start=True, stop=True),
                     reads=[ATn[a], An[a]], writes=[pan])
                if k < 5:
                    pb, pbn = nps_()
                    P.op("pe", lambda e, pb=pb, a=a: e.matmul(pb[:, 0:128], lhsT=Ah[a][:], rhs=ATh[a][:],
                                                             start=True, stop=True),
                         reads=[ATn[a], An[a]], writes=[pbn])
                yield
                P.op("act", lambda e, pa=pa, b=b: e.copy(out=Ah[b][:], in_=pa[:, 0:128]), reads=[pan], writes=[An[b]])
                if k < 5:
                    P.op("act", lambda e, pb=pb, b=b: e.copy(out=ATh[b][:], in_=pb[:, 0:128]), reads=[pbn], writes=[ATn[b]])
                yield
                pp, ppn = nps_()
                P.op("pe", lambda e, pp=pp, b=b: e.matmul(pp[:, 0:128], lhsT=Ah[b][:], rhs=PTh[:], start=True, stop=True),
                     reads=[An[b], PTn], writes=[ppn])
                yield
                P.op("dve", lambda e, pp=pp: e.tensor_tensor(out=PTh[:], in0=pp[:, 0:128], in1=PTh[:], op=ALU.add),
                     reads=[ppn, PTn], writes=[PTn])
            pr_, prn = nps_()
            P.op("pe", lambda e: e.matmul(pr_[:, 0:64], lhsT=KRp[pr, 0:128], rhs=S0b[pr, h, :], start=True, stop=False),
                 reads=[krn, s0bn], writes=[prn])
            P.op("pe", lambda e: e.matmul(pr_[:, 0:64], lhsT=MVK[h][:, 0:128], rhs=Vh, start=False, stop=True),
                 reads=[mvkn, "Vt"], writes=[prn])
            yield
            P.op("act", lambda e: e.copy(out=RHS[h][:], in_=pr_[:, 0:64]), reads=[prn], writes=[rhsn])
            yield
            pu, pun = nps_()
            P.op("pe", lambda e: e.matmul(pu[:, 0:64], lhsT=PTh[:], rhs=RHS[h][:], start=True, stop=True),
                 reads=[PTn, rhsn], writes=[pun])
            yield
            P.op("act", lambda e: e.copy(out=Ub[h][:], in_=pu[:, 0:64]), reads=[pun], writes=[ubn])
            yield
            py, pyn = nps_()
            P.op("pe", lambda e: e.matmul(py[:, 0:64], lhsT=KRp[pr, 128:256], rhs=S0b[pr, h, :], start=True, stop=False),
                 reads=[krn, s0bn], writes=[pyn])
            P.op("pe", lambda e: e.matmul(py[:, 0:64], lhsT=MVK[h][:, 128:256], rhs=Vh, start=False, stop=False),
                 reads=[mvkn, "Vt"], writes=[pyn])
            P.op("pe", lambda e: e.matmul(py[:, 0:64], lhsT=NAUB[h][:], rhs=Ub[h][:], start=False, stop=True),
                 reads=[naubn, ubn], writes=[pyn])
            pst, pstn = nps_()
            P.op("pe", lambda e: e.matmul(pst[:, 0:64], lhsT=KH[pair][:], rhs=Vh, start=True, stop=False),
                 reads=["KH%d" % pair, "Vt"], writes=[pstn])
            P.op("pe", lambda e: e.matmul(pst[:, 0:64], lhsT=BH[pair][:], rhs=Ub[h][:], start=False, stop=True),
                 reads=["BH%d" % pair, ubn], writes=[pstn])
            yield
            P.op("dve", lambda e: e.scalar_tensor_tensor(
                out=S32[pr, h, :], in0=S32[pr, h, :], scalar=eg[pair][pr, 127:128], in1=pst[pr, 0:64],
                op0=ALU.mult, op1=ALU.add), reads=[pstn, s32n, "eg%d" % pair], writes=[s32n])
            P.op("act", lambda e: e.copy(out=S0b[pr, h, :], in_=S32[pr, h, :]), reads=[s32n], writes=[s0bn])
            P.op("dve", lambda e: e.reduce_sum(out=smh[:, 0:1], in_=py[:, 0:64], axis=AX.X), reads=[pyn], writes=[smn])
            P.op("dve", lambda e: e.tensor_scalar(out=smh[:, 0:1], in0=smh[:, 0:1], scalar1=-1.0 / 64, scalar2=None,
                                                  op0=ALU.mult), reads=[smn], writes=[smn])
            P.op("dve", lambda e: e.tensor_scalar(out=ych[:], in0=py[:, 0:64], scalar1=smh[:, 0:1], scalar2=None,
                                                  op0=ALU.add), reads=[pyn, smn], writes=[ycn])
            yield
            P.op("dve", lambda e: e.tensor_tensor(out=ysqh[:], in0=ych[:], in1=ych[:], op=ALU.mult), reads=[ycn], writes=[ysqn])
            P.op("dve", lambda e: e.reduce_sum(out=smh[:, 1:2], in_=ysqh[:], axis=AX.X), reads=[ysqn], writes=[smn])
            P.op("dve", lambda e: e.tensor_scalar(out=smh[:, 1:2], in0=smh[:, 1:2], scalar1=1.0 / 64, scalar2=GN_EPS,
                                                  op0=ALU.mult, op1=ALU.add), reads=[smn], writes=[smn])
            yield
            P.op("act", lambda e: e.sqrt(out=smh[:, 1:2], in_=smh[:, 1:2]), reads=[smn], writes=[smn])
            yield
            P.op("dve", lambda e: e.reciprocal(out=smh[:, 2:3], in_=smh[:, 1:2]), reads=[smn], writes=[smn])
            P.op("dve", lambda e: e.scalar_tensor_tensor(
                out=ych[:], in0=ych[:], scalar=smh[:, 2:3], in1=LN[:, h * 64:(h + 1) * 64],
                op0=ALU.mult, op1=ALU.mult), reads=[ycn, smn, "LN"], writes=[ycn])
            P.op("dve", lambda e: e.tensor_tensor(out=ych[:], in0=ych[:], in1=LN[:, 512 + h * 64:512 + (h + 1) * 64],
                                                  op=ALU.add), reads=[ycn, "LN"], writes=[ycn])
            P.op("dve", lambda e: e.scalar_tensor_tensor(
                out=ych[:], in0=Vh, scalar=BS[:, h:h + 1], in1=ych[:], op0=ALU.mult, op1=ALU.add),
                reads=["Vt", "BS", ycn], writes=[ycn])
            P.op("dve", lambda e: e.tensor_tensor(out=OUT[:, h * 64:(h + 1) * 64], in0=ych[:],
                                                  in1=Gt[:, h * 64:(h + 1) * 64], op=ALU.mult),
                 reads=[ycn, "Gt"], writes=["OUT%d" % h])

        def round_robin(gens):
            gens = list(gens)
            while gens:
                for g in list(gens):
                    try:
                        next(g)
                    except StopIteration:
                        gens.remove(g)

        for t0 in range(0, T, 128):
            for qi, n in enumerate(("RT", "KT", "KKT", "BT")):
                P.dma(FM[n][:], FMd[n][:, :, t0:t0 + 128], writes=[n], q=("sp", "pool")[qi % 2])
            P.dma(LW[:], LWd[t0:t0 + 128, :], writes=["LW"])
            P.dma(Vt[:], Vd[t0:t0 + 128, :], writes=["Vt"], q="pool")
            P.dma(Gt[:], Gd[t0:t0 + 128, :], writes=["Gt"])
            P.dma(BS[:], BSd[t0:t0 + 128, :], writes=["BS"], q="pool")
            round_robin([pair_chain(p_) for p_ in range(4)])
            round_robin([head_chain(h) for h in range(0, 4)])
            round_robin([head_chain(h) for h in range(4, 8)])
            P.dma(Od[t0:t0 + 128, :], OUT[:], reads=["OUT%d" % h for h in range(8)])
        P.emit()
    return nc


def rwkv_b_inputs(OB, OLW, OBS, p, T, hh_, consts):
    cs = slice(512 * hh_, 512 * hh_ + 512)
    r, k, v, kk, b, g = (OB[:, i * D:(i + 1) * D][:, cs] for i in range(6))

    def fm(z):
        return np.ascontiguousarray(z.reshape(T, 4, 128).transpose(2, 1, 0))
    LN = np.concatenate([np.tile(p["ln_w"][cs][None], (128, 1)), np.tile(p["ln_b"][cs][None], (128, 1))], 1)
    d = {"RT": fm(r), "KT": fm(k), "KKT": fm(kk), "BT": fm(b), "LW": np.ascontiguousarray(OLW[:, cs]),
         "V": np.ascontiguousarray(v), "G": np.ascontiguousarray(g),
         "BS": np.ascontiguousarray(OBS[:, 8 * hh_:8 * hh_ + 8]), "LN": np.ascontiguousarray(LN.astype(np.float32))}
    d.update(consts)
    return d


_PROGS = {}
NCORES = 8


def _prog(key, fn):
    if key not in _PROGS:
        _PROGS[key] = fn()
    return _PROGS[key]


def _run(nc, in_maps):
    res = run_bass_kernel_spmd(nc, in_maps, core_ids=list(range(NCORES)))
    return res.results


def _halo_slice(xT, c, per, T, halo):
    t0 = c * per
    if halo == 0:
        return np.ascontiguousarray(xT[:, t0:t0 + per])
    out = np.zeros((xT.shape[0], per + halo), dtype=xT.dtype)
    out[:, halo:] = xT[:, t0:t0 + per]
    if t0 % T != 0:
        out[:, :halo] = xT[:, t0 - halo:t0]
    return out


def _nsa_layer(xT, inp, i, j, B, T):
    per = xT.shape[1] // NCORES
    cos, sin = rope_tables_np(T)
    p = {k[4:]: np.asarray(v[j]) for k, v in inp.items() if k.startswith("nsa_")}
    nca = _prog(("nsa_a", per), lambda: build_nsa_a(per))
    gn = pack_vec(np.asarray(inp["norm_mixer"][i]))
    bg = np.ascontiguousarray(np.tile(p["b_gate"][None], (128, 1)).astype(np.float32))
    ims = []
    for c in range(NCORES):
        pos0 = (c * per) % T
        ims.append({"xT": _halo_slice(xT, c, per, T, 0), "w_in": p["w_in"], "gn": gn,
                    "cos": np.ascontiguousarray(cos[pos0:pos0 + per]), "sin": np.ascontiguousarray(sin[pos0:pos0 + per]),
                    "bg": bg})
    ra = _run(nca, ims)
    pr = np.concatenate([r["pr"] for r in ra], axis=0).reshape(B, T, 3584)
    gate = np.concatenate([r["gate"] for r in ra], axis=0).reshape(B, T, 48)
    consts = nsa_consts(T)
    ncb = _prog(("nsa_b", T), lambda: build_nsa_b(T))
    ims = [nsa_b_inputs(pr[c // 2], gate[c // 2], p, T, c % 2, consts) for c in range(NCORES)]
    rb = _run(ncb, ims)
    o = np.concatenate([np.concatenate([rb[2 * b]["O"], rb[2 * b + 1]["O"]], axis=1) for b in range(B)], axis=0)
    return o, p["w_out"]


def _mlstm_layer(xT, inp, i, j, B, T):
    per = xT.shape[1] // NCORES
    p = {k[6:]: np.asarray(v[j]) for k, v in inp.items() if k.startswith("mlstm_")}
    nca = _prog(("proj_a", per), lambda: build_proj_a(per, 3088, f32_cols=(2048, 2176)))
    gn = pack_vec(np.asarray(inp["norm_mixer"][i]))
    ims = [{"xT": _halo_slice(xT, c, per, T, 0), "w_in": p["w_in"], "gn": gn} for c in range(NCORES)]
    ra = _run(nca, ims)
    pr = np.concatenate([r["pr"] for r in ra], axis=0).reshape(B, T, 3088)
    pf = np.concatenate([r["pf"] for r in ra], axis=0).reshape(B, T, 128)
    consts = mlstm_consts()
    ncb = _prog(("mlstm_b", T), lambda: build_mlstm_b(T))
    ims = [mlstm_b_inputs(pr[c // 2], pf[c // 2], p, T, c % 2, consts) for c in range(NCORES)]
    rb = _run(ncb, ims)
    o = np.concatenate([np.concatenate([rb[2 * b]["O"], rb[2 * b + 1]["O"]], axis=1) for b in range(B)], axis=0)
    return o, p["w_out"]


def _rwkv_layer(xT, inp, i, j, B, T):
    per = xT.shape[1] // NCORES
    p = {k[5:]: np.asarray(v[j]) for k, v in inp.items() if k.startswith("rwkv_")}
    p["r_k"] = p["r_k"].reshape(-1)
    nca = _prog(("rwkv_a", per), lambda: build_rwkv_a(per))
    gnv = np.asarray(inp["norm_mixer"][i])
    ims = [rwkv_a_inputs(_halo_slice(xT, c, per, T, 1), p, gnv) for c in range(NCORES)]
    ra = _run(nca, ims)
    OB = np.concatenate([r["OB"] for r in ra], axis=0).reshape(B, T, 6 * 1024)
    OLW = np.concatenate([r["OLW"] for r in ra], axis=0).reshape(B, T, 1024)
    OBS = np.concatenate([r["OBS"] for r in ra], axis=0).reshape(B, T, 16)
    consts = rwkv_consts()
    ncb = _prog(("rwkv_b", T), lambda: build_rwkv_b(T))
    ims = [rwkv_b_inputs(OB[c // 2], OLW[c // 2], OBS[c // 2], p, T, c % 2, consts) for c in range(NCORES)]
    rb = _run(ncb, ims)
    o = np.concatenate([np.concatenate([rb[2 * b]["O"], rb[2 * b + 1]["O"]], axis=1) for b in range(B)], axis=0)
    return o, p["w_o"]


def _ffn_layer(xT, o, w_out, inp, i, B, T, final):
    per = xT.shape[1] // NCORES
    ncf = _prog(("ffn", per, final), lambda: build_ffn(per, final_norm=final))
    oT = np.ascontiguousarray(o.T)
    cw = np.asarray(inp["ffn_conv_w"][i])
    small = {"gn": pack_vec(np.asarray(inp["norm_ffn"][i])), "gfin": pack_vec(np.asarray(inp["final_norm"])),
             "cw": np.ascontiguousarray(np.concatenate([pack_vec(cw[k]) for k in range(3)], axis=1)),
             "cb": pack_vec(np.asarray(inp["ffn_conv_b"][i]))}
    ims = []
    for c in range(NCORES):
        d = {"xT": _halo_slice(xT, c, per, T, 2), "oT": _halo_slice(oT, c, per, T, 2),
             "w_out": np.asarray(w_out), "w_up": np.asarray(inp["ffn_w_up"][i]),
             "w_down": np.asarray(inp["ffn_w_down"][i])}
        d.update(small)
        ims.append(d)
    rf = _run(ncf, ims)
    return np.concatenate([r["yT"] for r in rf], axis=1)


def kernel(**inputs):
    inp = {k: np.asarray(v) for k, v in inputs.items()}
    x = inp["x"]
    B, T, Dm = x.shape
    xT = np.ascontiguousarray(x.reshape(B * T, Dm).T)
    depth = inp["norm_mixer"].shape[0]
    for i in range(depth):
        kind, j = i % 3, i // 3
        if kind == 0:
            o, w_out = _nsa_layer(xT, inp, i, j, B, T)
        elif kind == 1:
            o, w_out = _mlstm_layer(xT, inp, i, j, B, T)
        else:
            o, w_out = _rwkv_layer(xT, inp, i, j, B, T)
        xT = _ffn_layer(xT, o, w_out, inp, i, B, T, final=(i == depth - 1))
    return np.ascontiguousarray(xT.T).reshape(B, T, Dm).astype(np.float32, copy=False)
```

```python
import numpy as np
import contextlib
import concourse.bass as bass
import concourse.mybir as mybir
from concourse.bass_utils import run_bass_kernel_spmd


F32 = mybir.dt.float32
BF16 = mybir.dt.bfloat16
AF = mybir.ActivationFunctionType
ALU = mybir.AluOpType
AX = mybir.AxisListType

ENGS = ("pe", "act", "dve", "pool", "sp")
NDSEM = 12


class Prog:
    def __init__(self, nc):
        self.nc = nc
        self.streams = {e: [] for e in ENGS}
        self.cnt = {e: 0 for e in ENGS}
        self.waited = {e: {} for e in ENGS}
        self.lastw = {}
        self.readers = {}
        self.ndma = 0
        self.pe_same_engine_sync = False

    def _need(self, eng, tok, waits):
        if tok is None:
            return
        if tok[0] == "e":
            _, e2, idx = tok
            if e2 == eng and eng == "pe":
                return
            key = ("e", e2)
            val = idx
        else:
            did = tok[1]
            key = ("d", did % NDSEM)
            val = 16 * (did // NDSEM + 1)
        if self.waited[eng].get(key, 0) >= val:
            return
        if waits.get(key, 0) < val:
            waits[key] = val

    def _deps(self, eng, reads, writes):
        waits = {}
        for r in reads:
            self._need(eng, self.lastw.get(r), waits)
        for w in writes:
            self._need(eng, self.lastw.get(w), waits)
            for t in self.readers.get(w, ()):
                self._need(eng, t, waits)
        for key, val in waits.items():
            self.streams[eng].append(("wait", key, val))
            self.waited[eng][key] = val

    def _commit(self, tok, reads, writes):
        for r in reads:
            self.readers.setdefault(r, []).append(tok)
        for w in writes:
            self.lastw[w] = tok
            self.readers[w] = []

    def op(self, eng, fn, reads=(), writes=()):
        px = [r for r in reads if r.startswith("ps") and r not in writes]
        if px:
            writes = list(writes) + px
        self._deps(eng, reads, writes)
        self.cnt[eng] += 1
        tok = ("e", eng, self.cnt[eng])
        self.streams[eng].append(("op", fn, None))
        self._commit(tok, reads, writes)
        return tok

    def dma(self, out, in_, reads=(), writes=(), q="sp", **kw):
        did = self.ndma
        self.ndma += 1
        self._deps(q, reads, writes)
        if did >= NDSEM:
            w = {}
            self._need(q, ("d", did - NDSEM), w)
            for key, val in w.items():
                self.streams[q].append(("wait", key, val))
                self.waited[q][key] = val
        self.streams[q].append(("dma", (out, in_, kw), did % NDSEM))
        tok = ("d", did)
        self._commit(tok, reads, writes)
        return tok

    def emit(self, final_wait_all=True):
        nc = self.nc
        import contextlib
        with contextlib.ExitStack() as es:
            esem = {e: es.enter_context(nc.semaphore("s_" + e)) for e in ENGS}
            dsem = [es.enter_context(nc.semaphore("d_%d" % i)) for i in range(NDSEM)]
            block = es.enter_context(nc.Block())

            def semof(key):
                return esem[key[1]] if key[0] == "e" else dsem[key[1]]

            def run(engname, eng):
                for item in self.streams[engname]:
                    if item[0] == "wait":
                        eng.wait_ge(semof(item[1]), item[2])
                    elif item[0] == "op":
                        ins = item[1](eng)
                        ins.then_inc(esem[engname], 1)
                    else:
                        out, in_, kw = item[1]
                        eng.dma_start(out=out, in_=in_, **kw).then_inc(dsem[item[2]], 16)
                if engname == "sp" and final_wait_all:
                    for i in range(NDSEM):
                        n = (self.ndma - 1 - i) // NDSEM + 1 if self.ndma > i else 0
                        if n > 0:
                            eng.wait_ge(dsem[i], 16 * n)
                    for e in ENGS:
                        if e != "sp" and self.cnt[e] > 0:
                            eng.wait_ge(esem[e], self.cnt[e])

            @block.sync
            def _(sync):
                run("sp", sync)

            @block.tensor
            def _(tensor):
                run("pe", tensor)

            @block.scalar
            def _(scalar):
                run("act", scalar)

            @block.vector
            def _(vector):
                run("dve", vector)

            @block.gpsimd
            def _(gpsimd):
                run("pool", gpsimd)


D = 1024
EPS = 1e-6
NSA_IN = 2608


def load_cast_weight(P, es, nc, w_dram, K, N, name, stg, stgn, CW=512):
    KC = K // 128
    wt = es.enter_context(nc.sbuf_tensor("sbw_" + name, [128, KC, N], BF16))
    i = 0
    for kc in range(KC):
        for c0 in range(0, N, CW):
            cw = min(CW, N - c0)
            s = stg[i % len(stg)]
            sn = stgn[i % len(stg)]
            P.dma(s[:, 0:cw], w_dram[kc * 128:(kc + 1) * 128, c0:c0 + cw], writes=[sn])
            if i % 2 == 0:
                P.op("act", lambda e, o=wt[:, kc, c0:c0 + cw], a=s[:, 0:cw]: e.copy(out=o, in_=a),
                     reads=[sn], writes=[name])
            else:
                P.op("pool", lambda e, o=wt[:, kc, c0:c0 + cw], a=s[:, 0:cw]: e.tensor_copy(out=o, in_=a),
                     reads=[sn], writes=[name])
            i += 1
    return wt


class Front:
    def __init__(self, P, es, nc, gn_dram, TILE=512, nps=None):
        self.P, self.nc, self.TILE = P, nc, TILE
        T = lambda name, shape, dt: es.enter_context(nc.sbuf_tensor(name, shape, dt))
        self.x_t = T("x_t", [128, 8, TILE], F32)
        self.sq_t = T("sq_t", [128, 8, TILE], BF16)
        self.h_t = T("h_t", [128, 8, TILE], BF16)
        self.rs_t = T("rs_t", [128, TILE], F32)
        self.gn_t = T("gn_t", [128, 8], F32)
        self.ones = T("ones", [128, 128], BF16)
        P.dma(self.gn_t[:], gn_dram, writes=["gn_t"])
        P.op("pool", lambda e: e.memset(self.ones[:], 1.0), writes=["ones"])
        self.nps = nps

    def run(self, xT_cols, n):
        P = self.P
        x_t, sq_t, h_t, rs_t, gn_t, ones = self.x_t, self.sq_t, self.h_t, self.rs_t, self.gn_t, self.ones
        P.dma(x_t[:, :, 0:n], xT_cols.rearrange("(c p) t -> p c t", p=128), writes=["x_t"])
        P.op("act", lambda e: e.activation(out=sq_t[:, :, 0:n], in_=x_t[:, :, 0:n], func=AF.Square),
             reads=["x_t"], writes=["sq_t"])
        pt, pn = self.nps()
        for c in range(8):
            P.op("pe", lambda e, c=c, pt=pt: e.matmul(pt[:, 0:n], lhsT=ones[:], rhs=sq_t[:, c, 0:n],
                                                     start=(c == 0), stop=(c == 7)),
                 reads=["ones", "sq_t"], writes=[pn])
        P.op("dve", lambda e, pt=pt: e.tensor_scalar(out=rs_t[:, 0:n], in0=pt[:, 0:n], scalar1=1.0 / D,
                                                    scalar2=EPS, op0=ALU.mult, op1=ALU.add),
             reads=[pn], writes=["rs_t"])
        P.op("act", lambda e: e.sqrt(out=rs_t[:, 0:n], in_=rs_t[:, 0:n]), reads=["rs_t"], writes=["rs_t"])
        P.op("dve", lambda e: e.reciprocal(out=rs_t[:, 0:n], in_=rs_t[:, 0:n]), reads=["rs_t"], writes=["rs_t"])
        for c in range(8):
            P.op("dve", lambda e, c=c: e.scalar_tensor_tensor(
                out=h_t[:, c, 0:n], in0=x_t[:, c, 0:n], scalar=gn_t[:, c:c + 1], in1=rs_t[:, 0:n],
                op0=ALU.mult, op1=ALU.mult), reads=["x_t", "rs_t", "gn_t"], writes=["h_t"])


def make_ps(es, nc, n):
    ps = [es.enter_context(nc.psum_tensor("ps%d" % i, [128, 512], F32)) for i in range(n)]
    ctr = [0]

    def nextps():
        i = ctr[0] % n
        ctr[0] += 1
        return ps[i], "ps%d" % i
    return nextps


def rope_ops(P, src, dst, nh, cs, sn, t1, t2, rd, wr):
    cb = cs[:, :].unsqueeze(1).to_broadcast([128, nh, 8])
    sb = sn[:, :].unsqueeze(1).to_broadcast([128, nh, 8])
    x1 = src[:, :, 0:8]
    x2 = src[:, :, 8:16]
    a = t1[:, 0:nh, :]
    b = t2[:, 0:nh, :]
    P.op("dve", lambda e: e.tensor_tensor(out=a, in0=x1, in1=cb, op=ALU.mult), reads=rd, writes=["rt1"])
    P.op("dve", lambda e: e.tensor_tensor(out=b, in0=x2, in1=sb, op=ALU.mult), reads=rd, writes=["rt2"])
    P.op("dve", lambda e: e.tensor_tensor(out=dst[:, :, 0:8], in0=a, in1=b, op=ALU.subtract),
         reads=["rt1", "rt2"], writes=wr)
    P.op("dve", lambda e: e.tensor_tensor(out=a, in0=x1, in1=sb, op=ALU.mult), reads=rd, writes=["rt1"])
    P.op("dve", lambda e: e.tensor_tensor(out=b, in0=x2, in1=cb, op=ALU.mult), reads=rd, writes=["rt2"])
    P.op("dve", lambda e: e.tensor_tensor(out=dst[:, :, 8:16], in0=a, in1=b, op=ALU.add),
         reads=["rt1", "rt2"], writes=wr)


def build_nsa_a(NT):
    nc = bass.Bass("TRN2", target_bir_lowering=False)
    xT = nc.dram_tensor("xT", [D, NT], F32, kind="ExternalInput").ap()
    w_in = nc.dram_tensor("w_in", [D, NSA_IN], F32, kind="ExternalInput").ap()
    gn = nc.dram_tensor("gn", [128, 8], F32, kind="ExternalInput").ap()
    cosd = nc.dram_tensor("cos", [NT, 8], F32, kind="ExternalInput").ap()
    sind = nc.dram_tensor("sin", [NT, 8], F32, kind="ExternalInput").ap()
    bg = nc.dram_tensor("bg", [128, 48], F32, kind="ExternalInput").ap()
    pr = nc.dram_tensor("pr", [NT, 3584], BF16, kind="ExternalOutput").ap()
    gate = nc.dram_tensor("gate", [NT, 48], F32, kind="ExternalOutput").ap()
    P = Prog(nc)
    with contextlib.ExitStack() as es:
        T = lambda name, shape, dt: es.enter_context(nc.sbuf_tensor(name, shape, dt))
        nextps = make_ps(es, nc, 7)
        stg = [T("stg%d" % i, [128, 512], F32) for i in range(2)]
        w_t = load_cast_weight(P, es, nc, w_in, D, NSA_IN, "w_t", stg, ["stg0", "stg1"])
        fr = Front(P, es, nc, gn, nps=nextps)
        bg_t = T("bg_t", [128, 48], F32)
        P.dma(bg_t[:], bg, writes=["bg_t"])
        PR = T("PR", [128, NSA_IN], F32)
        OUT = T("OUT", [128, 3584], BF16)
        GT = T("GT", [128, 48], F32)
        cs = T("cs", [128, 8], F32)
        sn = T("sn", [128, 8], F32)
        rt1 = T("rt1", [128, 16, 8], F32)
        rt2 = T("rt2", [128, 16, 8], F32)
        blocks = [(c0, min(512, NSA_IN - c0)) for c0 in range(0, NSA_IN, 512)]
        for t0 in range(0, NT, 512):
            n = min(512, NT - t0)
            fr.run(xT[:, t0:t0 + n], n)
            for s0 in range(0, n, 128):
                tt = t0 + s0
                P.dma(cs[:], cosd[tt:tt + 128, :], writes=["cs"], q="pool")
                P.dma(sn[:], sind[tt:tt + 128, :], writes=["sn"], q="pool")
                for (c0, cwid) in blocks:
                    pt, pn = nextps()
                    for kc in range(8):
                        P.op("pe", lambda e, kc=kc, pt=pt, c0=c0, cwid=cwid, s0=s0: e.matmul(
                            pt[:, 0:cwid], lhsT=fr.h_t[:, kc, s0:s0 + 128], rhs=w_t[:, kc, c0:c0 + cwid],
                            start=(kc == 0), stop=(kc == 7)), reads=["h_t", "w_t"], writes=[pn])
                    P.op("act", lambda e, pt=pt, c0=c0, cwid=cwid: e.copy(out=PR[:, c0:c0 + cwid],
                                                                          in_=pt[:, 0:cwid]),
                         reads=[pn], writes=["PR"])
                P.op("act", lambda e: e.copy(out=OUT[:, 0:2560], in_=PR[:, 0:2560]), reads=["PR"], writes=["OUT"])
                P.op("pool", lambda e: e.tensor_copy(out=OUT[:, 2560:3584], in_=PR[:, 0:1024]),
                     reads=["PR"], writes=["OUT"])
                P.op("dve", lambda e: e.tensor_tensor(out=GT[:], in0=PR[:, 2560:2608], in1=bg_t[:], op=ALU.add),
                     reads=["PR", "bg_t"], writes=["GT"])
                P.op("act", lambda e: e.activation(out=GT[:], in_=GT[:], func=AF.Sigmoid), reads=["GT"], writes=["GT"])
                qv = PR[:, 0:1024].rearrange("p (h d) -> p h d", d=64)
                rope_ops(P, qv, OUT[:, 2560:3584].rearrange("p (h d) -> p h d", d=64), 16, cs, sn, rt1, rt2,
                         ["PR", "cs", "sn"], ["OUT"])
                for off in (1024 + 512, 1024 + 1024):
                    kv_ = PR[:, off:off + 256].rearrange("p (h d) -> p h d", d=64)
                    rope_ops(P, kv_, OUT[:, off:off + 256].rearrange("p (h d) -> p h d", d=64), 4, cs, sn,
                             rt1, rt2, ["PR", "cs", "sn"], ["OUT"])
                P.dma(pr[tt:tt + 128, :], OUT[:], reads=["OUT"])
                P.dma(gate[tt:tt + 128, :], GT[:], reads=["GT"], q="pool")
        P.emit()
    return nc


def pack_vec(v):
    return np.ascontiguousarray(v.reshape(-1, 128).T)


def rope_tables_np(T):
    half = 8
    inv = 500000.0 ** (-np.arange(half, dtype=np.float32) / half)
    ang = np.arange(T, dtype=np.float32)[:, None] * inv[None, :]
    return np.cos(ang).astype(np.float32), np.sin(ang).astype(np.float32)


D = 1024
FF = 2816
NFC = FF // 128
EPS = 1e-6


def load_cast_weight_ffn(P, es, nc, w_dram, K, N, name, stg, qs=("sp",)):
    KC = K // 128
    wt = es.enter_context(nc.sbuf_tensor(name, [128, KC, N], BF16))
    CW = 512
    i = 0
    for kc in range(KC):
        for c0 in range(0, N, CW):
            cw = min(CW, N - c0)
            s = stg[i % len(stg)]
            sn = 'stg%d' % (i % len(stg))
            P.dma(s[:, 0:cw], w_dram[kc * 128:(kc + 1) * 128, c0:c0 + cw],
                  writes=[sn], q=qs[i % len(qs)])
            eng = ("act", "pool")[i % 2]
            if eng == "act":
                P.op("act", lambda e, o=wt[:, kc, c0:c0 + cw], a=s[:, 0:cw]: e.copy(out=o, in_=a),
                     reads=[sn], writes=[name])
            else:
                P.op("pool", lambda e, o=wt[:, kc, c0:c0 + cw], a=s[:, 0:cw]: e.tensor_copy(out=o, in_=a),
                     reads=[sn], writes=[name])
            i += 1
    return wt


def build_ffn(NT, final_norm=False, TILE=512):
    nc = bass.Bass("TRN2", target_bir_lowering=False)
    xT = nc.dram_tensor("xT", [D, NT + 2], F32, kind="ExternalInput").ap()
    oT = nc.dram_tensor("oT", [D, NT + 2], BF16, kind="ExternalInput").ap()
    w_out = nc.dram_tensor("w_out", [D, D], F32, kind="ExternalInput").ap()
    w_up = nc.dram_tensor("w_up", [D, 2 * FF], F32, kind="ExternalInput").ap()
    w_down = nc.dram_tensor("w_down", [FF, D], F32, kind="ExternalInput").ap()
    gn = nc.dram_tensor("gn", [128, 8], F32, kind="ExternalInput").ap()
    gfin = nc.dram_tensor("gfin", [128, 8], F32, kind="ExternalInput").ap()
    cw = nc.dram_tensor("cw", [128, 3 * NFC], F32, kind="ExternalInput").ap()
    cb = nc.dram_tensor("cb", [128, NFC], F32, kind="ExternalInput").ap()
    yT = nc.dram_tensor("yT", [D, NT], F32, kind="ExternalOutput").ap()

    P = Prog(nc)
    with contextlib.ExitStack() as es:
        T = lambda name, shape, dt: es.enter_context(nc.sbuf_tensor(name, shape, dt))
        stg = [T("stg%d" % i, [128, 512], F32) for i in range(2)]
        gn_t = T("gn_t", [128, 8], F32)
        gf_t = T("gf_t", [128, 8], F32)
        cw_t = T("cw_t", [128, 3 * NFC], F32)
        cb_t = T("cb_t", [128, NFC], F32)
        ones = T("ones", [128, 128], BF16)
        P.dma(gn_t[:], gn, writes=["gn_t"])
        P.dma(gf_t[:], gfin, writes=["gf_t"])
        P.dma(cw_t[:], cw, writes=["cw_t"])
        P.dma(cb_t[:], cb, writes=["cb_t"])
        P.op("pool", lambda e: e.memset(ones[:], 1.0), writes=["ones"])
        wo_t = load_cast_weight_ffn(P, es, nc, w_out, D, D, "wo_t", stg)
        wu_t = load_cast_weight_ffn(P, es, nc, w_up, D, 2 * FF, "wu_t", stg)
        wd_t = load_cast_weight_ffn(P, es, nc, w_down, FF, D, "wd_t", stg)

        x_t = T("x_t", [128, 8, TILE], F32)
        o_t = T("o_t", [128, 8, TILE], BF16)
        h_t = o_t
        rs_t = T("rs_t", [128, TILE], F32)
        Gc = T("Gc", [128, NFC, 2], F32)
        Gw = [T("Gw%d" % i, [128, TILE + 2], F32) for i in range(1)]
        tt = [T("tt%d" % i, [128, TILE], F32) for i in range(1)]
        ss = [T("ss%d" % i, [128, TILE], BF16) for i in range(1)]
        A_t = T("A_t", [128, NFC, TILE], BF16)
        sq_t = A_t
        NPS = 6
        ps = [es.enter_context(nc.psum_tensor("ps%d" % i, [128, 512], F32)) for i in range(NPS)]
        psi = [0]

        def nextps():
            i = psi[0] % NPS
            psi[0] += 1
            return ps[i], "ps%d" % i

        def rmsnorm(n, g_t, gname, dst, dstname, dst_dt_bf16=True):
            P.op("act", lambda e: e.activation(out=sq_t[:, 0:8, 0:n], in_=x_t[:, :, 0:n], func=AF.Square),
                 reads=["x_t"], writes=["A_t"])
            pt, pn = nextps()
            for c in range(8):
                P.op("pe", lambda e, c=c, pt=pt: e.matmul(pt[:, 0:n], lhsT=ones[:], rhs=sq_t[:, c, 0:n],
                                                         start=(c == 0), stop=(c == 7)),
                     reads=["ones", "A_t"], writes=[pn])
            P.op("dve", lambda e, pt=pt: e.tensor_scalar(out=rs_t[:, 0:n], in0=pt[:, 0:n], scalar1=1.0 / D,
                                                        scalar2=EPS, op0=ALU.mult, op1=ALU.add),
                 reads=[pn], writes=["rs_t"])
            P.op("act", lambda e: e.sqrt(out=rs_t[:, 0:n], in_=rs_t[:, 0:n]),
                 reads=["rs_t"], writes=["rs_t"])
            P.op("dve", lambda e: e.reciprocal(out=rs_t[:, 0:n], in_=rs_t[:, 0:n]),
                 reads=["rs_t"], writes=["rs_t"])
            for c in range(8):
                P.op("dve", lambda e, c=c: e.scalar_tensor_tensor(
                    out=dst[:, c, 0:n], in0=x_t[:, c, 0:n], scalar=g_t[:, c:c + 1], in1=rs_t[:, 0:n],
                    op0=ALU.mult, op1=ALU.mult), reads=["x_t", "rs_t", gname], writes=[dstname])

        def do_tile(c0, n, halo):
            P.dma(x_t[:, :, 0:n], xT[:, c0:c0 + n].rearrange("(c p) t -> p c t", p=128), writes=["x_t"])
            P.dma(o_t[:, :, 0:n], oT[:, c0:c0 + n].rearrange("(c p) t -> p c t", p=128), writes=["o_t"],
                  q="pool")
            for dc in range(8):
                pt, pn = nextps()
                for kc in range(8):
                    P.op("pe", lambda e, dc=dc, kc=kc, pt=pt: e.matmul(
                        pt[:, 0:n], lhsT=wo_t[:, kc, dc * 128:(dc + 1) * 128], rhs=o_t[:, kc, 0:n],
                        start=(kc == 0), stop=(kc == 7)), reads=["wo_t", "o_t"], writes=[pn])
                P.op("dve", lambda e, dc=dc, pt=pt: e.tensor_tensor(
                    out=x_t[:, dc, 0:n], in0=pt[:, 0:n], in1=x_t[:, dc, 0:n], op=ALU.add),
                    reads=[pn, "x_t"], writes=["x_t"])
            rmsnorm(n, gn_t, "gn_t", h_t, "o_t")
            for fc in range(NFC):
                pt, pn = nextps()
                for kc in range(8):
                    P.op("pe", lambda e, fc=fc, kc=kc, pt=pt: e.matmul(
                        pt[:, 0:n], lhsT=wu_t[:, kc, fc * 128:(fc + 1) * 128], rhs=h_t[:, kc, 0:n],
                        start=(kc == 0), stop=(kc == 7)), reads=["wu_t", "o_t"], writes=[pn])
                gk = "Gw0"
                g = Gw[0]
                if halo:
                    P.op("act", lambda e, fc=fc, pt=pt: e.copy(out=Gc[:, fc, :], in_=pt[:, 0:2]),
                         reads=[pn], writes=["Gc"])
                    continue
                P.op("act", lambda e, fc=fc, g=g: e.copy(out=g[:, 0:2], in_=Gc[:, fc, :]),
                     reads=["Gc"], writes=[gk])
                P.op("act", lambda e, fc=fc, pt=pt, g=g: e.copy(out=g[:, 2:2 + n], in_=pt[:, 0:n]),
                     reads=[pn], writes=[gk])
                P.op("act", lambda e, fc=fc, g=g: e.copy(out=Gc[:, fc, :], in_=g[:, n:n + 2]),
                     reads=[gk], writes=["Gc"])
                pv, pvn = nextps()
                for kc in range(8):
                    P.op("pe", lambda e, fc=fc, kc=kc, pv=pv: e.matmul(
                        pv[:, 0:n], lhsT=wu_t[:, kc, FF + fc * 128:FF + (fc + 1) * 128], rhs=h_t[:, kc, 0:n],
                        start=(kc == 0), stop=(kc == 7)), reads=["wu_t", "o_t"], writes=[pvn])
                t = tt[0]
                tn = "tt0"
                s = ss[0]
                sn = "ss0"
                P.op("dve", lambda e, fc=fc, t=t, g=g: e.tensor_scalar(
                    out=t[:, 0:n], in0=g[:, 0:n], scalar1=cw_t[:, fc:fc + 1], scalar2=None, op0=ALU.mult),
                    reads=[gk, "cw_t"], writes=[tn])
                for k in (1, 2):
                    P.op("dve", lambda e, fc=fc, t=t, k=k, g=g: e.scalar_tensor_tensor(
                        out=t[:, 0:n], in0=g[:, k:k + n], scalar=cw_t[:, k * NFC + fc:k * NFC + fc + 1],
                        in1=t[:, 0:n], op0=ALU.mult, op1=ALU.add), reads=[gk, "cw_t", tn], writes=[tn])
                P.op("act", lambda e, fc=fc, t=t, s=s: e.activation(
                    out=s[:, 0:n], in_=t[:, 0:n], func=AF.Silu, bias=cb_t[:, fc:fc + 1]),
                    reads=[tn, "cb_t"], writes=[sn])
                P.op("dve", lambda e, fc=fc, s=s, pv=pv: e.tensor_tensor(
                    out=A_t[:, fc, 0:n], in0=pv[:, 0:n], in1=s[:, 0:n], op=ALU.mult),
                    reads=[pvn, sn], writes=["A_t"])
            if halo:
                return
            for dc in range(8):
                pt, pn = nextps()
                for fc in range(NFC):
                    P.op("pe", lambda e, dc=dc, fc=fc, pt=pt: e.matmul(
                        pt[:, 0:n], lhsT=wd_t[:, fc, dc * 128:(dc + 1) * 128], rhs=A_t[:, fc, 0:n],
                        start=(fc == 0), stop=(fc == NFC - 1)), reads=["wd_t", "A_t"], writes=[pn])
                P.op("dve", lambda e, dc=dc, pt=pt: e.tensor_tensor(
                    out=x_t[:, dc, 0:n], in0=pt[:, 0:n], in1=x_t[:, dc, 0:n], op=ALU.add),
                    reads=[pn, "x_t"], writes=["x_t"])
            src, srcn = x_t, "x_t"
            if final_norm:
                rmsnorm(n, gf_t, "gf_t", x_t, "x_t")
            P.dma(yT[:, c0 - 2:c0 - 2 + n].rearrange("(c p) t -> p c t", p=128), src[:, :, 0:n], reads=[srcn])

        es_fin = [None]
        do_tile(0, 2, True)
        for c0 in range(2, NT + 2, TILE):
            do_tile(c0, min(TILE, NT + 2 - c0), False)
        P.emit()
    return nc


def ffn_ref(x, o, w_out, gn, w_up, cw, cb, w_down, gfin=None):
    x1 = x + o @ w_out
    h = x1 / np.sqrt((x1 * x1).mean(-1, keepdims=True) + EPS) * gn
    u = h @ w_up
    gate, val = u[:, :FF], u[:, FF:]
    g = cw[0] * gate[:-2] + cw[1] * gate[1:-1] + cw[2] * gate[2:] + cb
    a = g / (1 + np.exp(-g)) * val[2:]
    y = x1[2:] + a @ w_down
    if gfin is not None:
        y = y / np.sqrt((y * y).mean(-1, keepdims=True) + EPS) * gfin
    return y


def pack_vec(v):
    return np.ascontiguousarray(v.reshape(-1, 128).T)


NEG = -30000.0


def nsa_consts(T):
    import ml_dtypes
    bf = ml_dtypes.bfloat16
    kl = np.arange(128)[:, None]
    tl = np.arange(512)[None, :]
    cm = np.zeros((128, 8, 512), np.float32)
    for di, d in enumerate(range(-4, 4)):
        rel = 128 * d + kl - tl
        cm[:, di, :] = np.where((rel <= 0) & (rel >= -511), 0.0, NEG)
    cmpm = np.zeros((128, 5, 512), np.float32)
    for di, d in enumerate(range(-4, 1)):
        cmpm[:, di, :] = np.where(16 * kl + 31 + 512 * d <= tl, 0.0, NEG)
    nct = (T // 16 + 127) // 128
    ov = np.zeros((128, nct, 129), np.float32)
    for j in range(nct):
        c = 128 * j + np.arange(128)[:, None]
        s = np.arange(128)[None, :]
        ov[:, j, 0:128] = ((c >= 4 * s - 1) & (c <= 4 * s + 3)).astype(np.float32)
        ov[:, j, 128] = 1.0
    ebig = (np.arange(128)[:, None] == (np.arange(T)[None, :] // 64)).astype(np.float32)
    p = np.arange(128)[:, None]
    sp = np.arange(256)[None, :] - 128
    tb = (p >= 64).astype(np.int64)
    causal = sp <= tb
    forced = (sp == tb) | (sp == tb - 1)
    m01 = causal.astype(np.float32)
    add = np.where(forced, 1e6, np.where(causal, 0.0, -1.0)).astype(np.float32)
    m01 = np.where(forced, 0.0, m01).astype(np.float32)
    return {"CM": cm.reshape(128, -1).astype(bf), "CMPM": cmpm.reshape(128, -1).astype(bf),
            "OV": ov.reshape(128, -1).astype(bf), "EBIG": ebig.astype(bf),
            "M01": m01, "ADDM": add, "IDB": np.eye(128, dtype=np.float32).astype(bf),
            "IDF": np.eye(128, dtype=np.float32)}


def build_nsa_b(T):
    NTT = T // 512
    NKT = T // 128
    NCMP = T // 16 - 1
    NCT = (T // 16 + 127) // 128
    nc = bass.Bass("TRN2", target_bir_lowering=False)
    dt_in = lambda name, shape, dt: nc.dram_tensor(name, shape, dt, kind="ExternalInput").ap()
    Qd = dt_in("Q", [128, 4, T], BF16)
    QRd = dt_in("QR", [128, 4, T], BF16)
    KCd = dt_in("KC", [128, T], BF16)
    VCd = dt_in("VC", [128, T], BF16)
    KSd = dt_in("KS", [128, T], BF16)
    KWd = dt_in("KW", [128, T], BF16)
    VSd = dt_in("VS", [T, 2 * 65], BF16)
    VWd = dt_in("VW", [T, 2 * 65], BF16)
    GAd = dt_in("GA", [T, 24], F32)
    W1d = [dt_in("W1K", [128, 32 * 256], F32), dt_in("W1V", [128, 32 * 256], F32)]
    W2d = [dt_in("W2K", [128, 2 * 128], F32), dt_in("W2V", [128, 2 * 128], F32)]
    PEd = [dt_in("PEK", [128, 32], F32), dt_in("PEV", [128, 32], F32)]
    CMd = dt_in("CM", [128, 8 * 512], BF16)
    CMPMd = dt_in("CMPM", [128, 5 * 512], BF16)
    OVd = dt_in("OV", [128, NCT * 129], BF16)
    EBd = dt_in("EBIG", [128, T], BF16)
    M01d = dt_in("M01", [128, 256], F32)
    ADDd = dt_in("ADDM", [128, 256], F32)
    IDBd = dt_in("IDB", [128, 128], BF16)
    IDFd = dt_in("IDF", [128, 128], F32)
    Od = nc.dram_tensor("O", [T, 512], BF16, kind="ExternalOutput").ap()

    P = Prog(nc)
    with contextlib.ExitStack() as es:
        Tn = lambda name, shape, dt: es.enter_context(nc.sbuf_tensor("sb_" + name, shape, dt))

        def const_load(name, dram, shape, dt, q="sp"):
            t = Tn(name, shape, dt)
            if len(shape) == 2:
                P.dma(t[:], dram, writes=[name], q=q)
            elif len(shape) == 3:
                P.dma(t[:], dram.rearrange("p (a b) -> p a b", b=shape[2]), writes=[name], q=q)
            return t
        KVC = Tn("KVC", [128, T], BF16)
        KS = const_load("KS", KSd, [128, T], BF16)
        KW = const_load("KW", KWd, [128, T], BF16, q="pool")
        VS = Tn("VS", [128, NKT, 130], BF16)
        VW = Tn("VW", [128, NKT, 130], BF16)
        P.dma(VS[:], VSd.rearrange("(k p) c -> p k c", p=128), writes=["VS"])
        P.dma(VW[:], VWd.rearrange("(k p) c -> p k c", p=128), writes=["VW"], q="pool")
        CM = const_load("CM", CMd, [128, 8, 512], BF16)
        CMPM = const_load("CMPM", CMPMd, [128, 5, 512], BF16)
        OV = const_load("OV", OVd, [128, NCT, 129], BF16)
        EB = const_load("EBIG", EBd, [128, T], BF16)
        M01 = const_load("M01", M01d, [128, 256], F32)
        ADDM = const_load("ADDM", ADDd, [128, 256], F32)
        IDB = const_load("IDB", IDBd, [128, 128], BF16)
        IDF = const_load("IDF", IDFd, [128, 128], F32)

        psS = [es.enter_context(nc.psum_tensor("psS%d" % i, [128, 512], F32)) for i in range(3)]
        psO = [es.enter_context(nc.psum_tensor("psO%d" % i, [128, 512], F32)) for i in range(2)]
        psI = [es.enter_context(nc.psum_tensor("psI%d" % i, [128, 512], F32)) for i in range(1)]
        psT = [es.enter_context(nc.psum_tensor("psT%d" % i, [128, 512], F32)) for i in range(2)]
        ctr = {"S": 0, "O": 0, "I": 0, "T": 0}
        pools = {"S": psS, "O": psO, "I": psI, "T": psT}

        def nps(k):
            i = ctr[k] % len(pools[k])
            ctr[k] += 1
            return pools[k][i], "ps%s%d" % (k, i)

        stg = [Tn("stg%d" % i, [128, 1024], F32) for i in range(2)]
        W1b = Tn("W1b", [128, 32, 256], BF16)
        W1 = [W1b, W1b]
        W2 = [Tn("W2_%d" % k, [128, 2, 128], BF16) for k in range(2)]
        PEt = [Tn("PE_%d" % k, [128, 32], BF16) for k in range(2)]
        sictr = [0]

        def load_cmp_weights(k):
            for c0 in range(0, 32 * 256, 1024):
                s, sn = stg[sictr[0] % 2], "stg%d" % (sictr[0] % 2)
                sictr[0] += 1
                P.dma(s[:], W1d[k][:, c0:c0 + 1024], writes=[sn])
                P.op("act", lambda e, c0=c0, s=s: e.copy(
                    out=W1b[:].rearrange("p a b -> p (a b)")[:, c0:c0 + 1024], in_=s[:]),
                    reads=[sn], writes=["W1b"])
            s, sn = stg[sictr[0] % 2], "stg%d" % (sictr[0] % 2)
            sictr[0] += 1
            P.dma(s[:, 0:256], W2d[k], writes=[sn])
            P.dma(s[:, 256:288], PEd[k], writes=[sn])
            P.op("act", lambda e, k=k, s=s: e.copy(out=W2[k][:].rearrange("p a b -> p (a b)"), in_=s[:, 0:256]),
                 reads=[sn], writes=["W2_%d" % k])
            P.op("act", lambda e, k=k, s=s: e.copy(out=PEt[k][:], in_=s[:, 256:288]),
                 reads=[sn], writes=["PE_%d" % k])
            P.dma(KVC[:], (KCd, VCd)[k], writes=["KVC"], q="pool")
        KCMP = Tn("KCMP", [128, NCT * 128], BF16)
        VCMP = Tn("VCMP", [128, NCT, 130], BF16)
        P.op("pool", lambda e: e.memset(KCMP[:], 0.0), writes=["KCMP"])
        P.op("pool", lambda e: e.memset(VCMP[:], 0.0), writes=["VCMP"])
        for gl in range(2):
            P.op("pool", lambda e, gl=gl: e.memset(VCMP[:, :, gl * 65 + 64:gl * 65 + 65], 1.0), writes=["VCMP"])
        bias_t = Tn("bias_t", [128, 2], F32)
        xh = Tn("xh", [128, 512], F32)
        x2 = Tn("x2", [128, 512], F32)
        gT = Tn("gT", [128, 2, 512], BF16)
        srcs = [KVC, KVC]
        srcn = ["KVC", "KVC"]
        for k in range(2):
            load_cmp_weights(k)
            for gl in range(2):
                pr = slice(64 * gl, 64 * gl + 64)
                wn = "W1b"
                for hc in range(2):
                    pb, pbn = nps("I")
                    for pos in range(32):
                        P.op("pe", lambda e, k=k, hc=hc, pos=pos, pb=pb, pr=pr: e.matmul(
                            pb[:, 0:1], lhsT=W1[k][pr, pos, hc * 128:(hc + 1) * 128], rhs=PEt[k][pr, pos:pos + 1],
                            start=(pos == 0), stop=(pos == 31)), reads=[wn, "PE_%d" % k], writes=[pbn])
                    P.op("act", lambda e, hc=hc, pb=pb: e.copy(out=bias_t[:, hc:hc + 1], in_=pb[:, 0:1]),
                         reads=[pbn], writes=["bias_t"])
                    ph, phn = nps("S")
                    for pos in range(32):
                        P.op("pe", lambda e, k=k, hc=hc, pos=pos, ph=ph, pr=pr: e.matmul(
                            ph[:, 0:NCMP], lhsT=W1[k][pr, pos, hc * 128:(hc + 1) * 128],
                            rhs=srcs[k][pr, pos:pos + 16 * (NCMP - 1) + 1:16],
                            start=(pos == 0), stop=(pos == 31)), reads=[wn, srcn[k]], writes=[phn])
                    P.op("act", lambda e, hc=hc, ph=ph: e.activation(
                        out=xh[:, 0:NCMP], in_=ph[:, 0:NCMP], func=AF.Identity, bias=bias_t[:, hc:hc + 1]),
                        reads=[phn, "bias_t"], writes=["xh"])
                    P.op("dve", lambda e: e.tensor_tensor(out=x2[:, 0:NCMP], in0=xh[:, 0:NCMP], in1=xh[:, 0:NCMP],
                                                          op=ALU.mult), reads=["xh"], writes=["x2"])
                    P.op("dve", lambda e: e.tensor_scalar(out=x2[:, 0:NCMP], in0=x2[:, 0:NCMP], scalar1=0.044715,
                                                          scalar2=1.0, op0=ALU.mult, op1=ALU.add),
                         reads=["x2"], writes=["x2"])
                    P.op("dve", lambda e: e.tensor_tensor(out=x2[:, 0:NCMP], in0=x2[:, 0:NCMP], in1=xh[:, 0:NCMP],
                                                          op=ALU.mult), reads=["x2", "xh"], writes=["x2"])
                    P.op("act", lambda e: e.activation(out=x2[:, 0:NCMP], in_=x2[:, 0:NCMP], func=AF.Sigmoid,
                                                       scale=1.5957691216), reads=["x2"], writes=["x2"])
                    P.op("dve", lambda e, hc=hc: e.tensor_tensor(out=gT[:, hc, 0:NCMP], in0=x2[:, 0:NCMP],
                                                                 in1=xh[:, 0:NCMP], op=ALU.mult),
                         reads=["x2", "xh"], writes=["gT"])
                if k == 0:
                    pk, pkn = nps("S")
                    for hc in range(2):
                        P.op("pe", lambda e, hc=hc, pk=pk: e.matmul(
                            pk[:, 0:NCMP], lhsT=W2[0][:, hc, :], rhs=gT[:, hc, 0:NCMP],
                            start=(hc == 0), stop=(hc == 1)), reads=["W2_0", "gT"], writes=[pkn])
                    P.op("act", lambda e, pk=pk, pr=pr: e.copy(out=KCMP[pr, 0:NCMP], in_=pk[pr, 0:NCMP]),
                         reads=[pkn], writes=["KCMP"])
                else:
                    for j in range(NCT):
                        rows = min(128, NCMP - 128 * j)
                        pv, pvn = nps("I")
                        for hc in range(2):
                            P.op("pe", lambda e, hc=hc, pv=pv, j=j, rows=rows: e.matmul(
                                pv[0:rows, 0:64], lhsT=gT[:, hc, 128 * j:128 * j + rows], rhs=W2[1][:, hc, 0:64],
                                start=(hc == 0), stop=(hc == 1)), reads=["W2_1", "gT"], writes=[pvn])
                        P.op("act", lambda e, pv=pv, j=j, rows=rows, gl=gl: e.copy(
                            out=VCMP[0:rows, j, gl * 65:gl * 65 + 64], in_=pv[0:rows, 0:64]),
                            reads=[pvn], writes=["VCMP"])

        Qt = Tn("Qt", [128, 4, 512], BF16)
        QRt = Tn("QRt", [128, 4, 512], BF16)
        Gt = Tn("Gt", [128, 4, 24], F32)
        ECMP = [Tn("ECMP%d" % i, [128, NCT, 512], BF16) for i in range(2)]
        NEB = 4
        Eb = [Tn("E%d" % i, [128, 512], BF16) for i in range(NEB)]
        ectr = [0]
        OT = [Tn("OT%d" % i, [128, 512], F32) for i in range(3)]
        octr = [0]
        OACC = Tn("OACC", [128, 4, 512], F32)
        OB = Tn("OB", [128, 512], BF16)
        IMP = [Tn("IMP%d" % i, [128, 4, 128], F32) for i in range(2)]
        XT = [Tn("XT%d" % i, [128, 512], BF16) for i in range(2)]
        smc = [Tn("smc%d" % i, [128, 4], F32) for i in range(4)]
        smi = [Tn("smi%d" % i, [128, 2], F32) for i in range(4)]
        tm = Tn("tm", [128, 128], F32)
        tm2 = Tn("tm2", [128, 128], F32)
        m8 = Tn("m8", [128, 16], F32)
        Xs = [Tn("Xs%d" % i, [128, 128], BF16) for i in range(8)]
        BR_CMP, BR_SEL, BR_WIN = 0, 1, 2
        tsl = slice(0, 512)
        LOOK = 2

        def make_units(gl, r, i, kind, ecmp_i):
            pr = slice(64 * gl, 64 * gl + 64)
            if kind == BR_CMP:
                tiles = [j for j in range(NCT) if 4 * j - i <= 0]
            elif kind == BR_SEL:
                tiles = list(range(0, 4 * i + 4))
            else:
                tiles = [kt for kt in range(4 * i - 4, 4 * i + 4) if kt >= 0]
            pO, pOn = nps("O")
            us = []
            for idx, kt in enumerate(tiles):
                mms = []
                if kind == BR_CMP:
                    mms.append((KCMP[pr, 128 * kt:128 * kt + 128], Qt[pr, r, :], ["KCMP", "Qt"]))
                    d = 4 * kt - i
                    if d >= -4:
                        mms.append((IDB[:], CMPM[:, d + 4, :], ["IDB", "CMPM"]))
                    V, Vn = VCMP[:, kt, gl * 65:gl * 65 + 65], "VCMP"
                    Et, Etn = ECMP[ecmp_i][:, kt, :], "ECMP%d" % ecmp_i
                elif kind == BR_SEL:
                    mms.append((KS[pr, 128 * kt:128 * kt + 128], QRt[pr, r, :], ["KS", "QRt"]))
                    mms.append((EB[:, 128 * kt:128 * kt + 128], XT[gl][:], ["EBIG", "XT%d" % gl]))
                    d = kt - 4 * i
                    if d >= 0:
                        mms.append((IDB[:], CM[:, d + 4, :], ["IDB", "CM"]))
                    V, Vn = VS[:, kt, gl * 65:gl * 65 + 65], "VS"
                    Et = None
                else:
                    mms.append((KW[pr, 128 * kt:128 * kt + 128], QRt[pr, r, :], ["KW", "QRt"]))
                    mms.append((IDB[:], CM[:, kt - 4 * i + 4, :], ["IDB", "CM"]))
                    V, Vn = VW[:, kt, gl * 65:gl * 65 + 65], "VW"
                    Et = None
                us.append({"mms": mms, "V": V, "Vn": Vn, "Et": Et, "Etn": Etn if Et is not None else None,
                           "pO": pO, "pOn": pOn, "first": idx == 0, "last": idx == len(tiles) - 1, "post": []})
            return us, pO, pOn, tiles

        def emit_S(u):
            pS, pSn = nps("S")
            u["pS"], u["pSn"] = pS, pSn
            nm = len(u["mms"])
            for mi, (l, rh, rd) in enumerate(u["mms"]):
                P.op("pe", lambda e, l=l, rh=rh, pS=pS, mi=mi, nm=nm: e.matmul(
                    pS[:, tsl], lhsT=l, rhs=rh, start=(mi == 0), stop=(mi == nm - 1)), reads=rd, writes=[pSn])

        def emit_EPV(u):
            if u["Et"] is None:
                bi = ectr[0] % NEB
                ectr[0] += 1
                Et, Etn = Eb[bi][:], "E%d" % bi
            else:
                Et, Etn = u["Et"], u["Etn"]
            pS, pSn, pO, pOn = u["pS"], u["pSn"], u["pO"], u["pOn"]
            P.op("act", lambda e, Et=Et, pS=pS: e.activation(out=Et, in_=pS[:, tsl], func=AF.Exp, scale=0.125),
                 reads=[pSn], writes=[Etn])
            P.op("pe", lambda e, V=u["V"], Et=Et, pO=pO, f=u["first"], l=u["last"]: e.matmul(
                pO[0:65, tsl], lhsT=V, rhs=Et, start=f, stop=l), reads=[u["Vn"], Etn], writes=[pOn])

        def combine_a(pO, pOn):
            bi = octr[0] % 3
            octr[0] += 1
            ot, otn = OT[bi], "OT%d" % bi
            P.op("act", lambda e, ot=ot, pO=pO: e.copy(out=ot[0:65, :], in_=pO[0:65, :]), reads=[pOn], writes=[otn])
            return ot, otn

        def combine_b(gl, r, ot, otn, br, first):
            h = gl * 4 + r
            for sub in range(4):
                sm, smn = smc[sub], "smc%d" % sub
                pT, pTn = nps("T")
                P.op("pe", lambda e, ot=ot, pT=pT, sub=sub: e.matmul(
                    pT[:, 0:65], lhsT=ot[0:65, sub * 128:(sub + 1) * 128], rhs=IDF[0:65, 0:65],
                    start=True, stop=True), reads=[otn, "IDF"], writes=[pTn])
                P.op("dve", lambda e, pT=pT, sm=sm: e.tensor_scalar(out=sm[:, 0:1], in0=pT[:, 64:65], scalar1=1e-30,
                                                                    scalar2=None, op0=ALU.max),
                     reads=[pTn], writes=[smn])
                P.op("dve", lambda e, sm=sm: e.reciprocal(out=sm[:, 1:2], in_=sm[:, 0:1]), reads=[smn], writes=[smn])
                P.op("dve", lambda e, sub=sub, h=h, br=br, sm=sm: e.tensor_tensor(
                    out=sm[:, 2:3], in0=sm[:, 1:2], in1=Gt[:, sub, h * 3 + br:h * 3 + br + 1], op=ALU.mult),
                    reads=[smn, "Gt"], writes=[smn])
                dst = OACC[:, sub, h * 64:(h + 1) * 64]
                dn = "OACC%d_%d" % (sub, h)
                if first:
                    P.op("dve", lambda e, pT=pT, dst=dst, sm=sm: e.tensor_scalar(
                        out=dst, in0=pT[:, 0:64], scalar1=sm[:, 2:3], scalar2=None, op0=ALU.mult),
                        reads=[pTn, smn], writes=[dn])
                else:
                    P.op("dve", lambda e, pT=pT, dst=dst, sm=sm: e.scalar_tensor_tensor(
                        out=dst, in0=pT[:, 0:64], scalar=sm[:, 2:3], in1=dst, op0=ALU.mult, op1=ALU.add),
                        reads=[pTn, smn, dn], writes=[dn])

        def imp_head(gl, r, tiles, ecmp_i):
            for sub in range(4):
                sm, smn = smi[sub], "smi%d" % sub
                pI, pIn = nps("I")
                for idx, j in enumerate(tiles):
                    P.op("pe", lambda e, j=j, sub=sub, pI=pI, idx=idx, nt=len(tiles): e.matmul(
                        pI[:, 0:129], lhsT=ECMP[ecmp_i][:, j, sub * 128:(sub + 1) * 128], rhs=OV[:, j, :],
                        start=(idx == 0), stop=(idx == nt - 1)), reads=["ECMP%d" % ecmp_i, "OV"], writes=[pIn])
                P.op("dve", lambda e, pI=pI, sm=sm: e.tensor_scalar(out=sm[:, 0:1], in0=pI[:, 128:129], scalar1=1e-30,
                                                                    scalar2=None, op0=ALU.max),
                     reads=[pIn], writes=[smn])
                P.op("dve", lambda e, sm=sm: e.reciprocal(out=sm[:, 1:2], in_=sm[:, 0:1]), reads=[smn], writes=[smn])
                imn = "IMP%d_%d" % (gl, sub)
                if r == 0:
                    P.op("dve", lambda e, pI=pI, sub=sub, sm=sm: e.tensor_scalar(
                        out=IMP[gl][:, sub, :], in0=pI[:, 0:128], scalar1=sm[:, 1:2], scalar2=None, op0=ALU.mult),
                        reads=[pIn, smn], writes=[imn])
                else:
                    P.op("dve", lambda e, pI=pI, sub=sub, sm=sm: e.scalar_tensor_tensor(
                        out=IMP[gl][:, sub, :], in0=pI[:, 0:128], scalar=sm[:, 1:2], in1=IMP[gl][:, sub, :],
                        op0=ALU.mult, op1=ALU.add), reads=[pIn, smn, imn], writes=[imn])

        def select_a(gl, i, sub):
            sb = 8 * i + 2 * sub
            msl = slice(128 - sb, 256 - sb)
            xs, xsn = Xs[gl * 4 + sub], "Xs%d" % (gl * 4 + sub)
            imn = "IMP%d_%d" % (gl, sub)
            P.op("dve", lambda e, sub=sub, msl=msl: e.tensor_tensor(
                out=tm[:], in0=IMP[gl][:, sub, :], in1=M01[:, msl], op=ALU.mult), reads=[imn, "M01"], writes=["tm"])
            P.op("dve", lambda e, msl=msl: e.tensor_tensor(out=tm[:], in0=tm[:], in1=ADDM[:, msl], op=ALU.add),
                 reads=["tm", "ADDM"], writes=["tm"])
            P.op("dve", lambda e: e.memset(tm[:, 0:1], 1e6), reads=["tm"], writes=["tm"])
            P.op("dve", lambda e: e.max(out=m8[:, 0:8], in_=tm[:]), reads=["tm"], writes=["m8"])
            P.op("dve", lambda e: e.match_replace(out=tm2[:], in_to_replace=m8[:, 0:8], in_values=tm[:],
                                                  imm_value=-1e9), reads=["tm", "m8"], writes=["tm2"])
            P.op("dve", lambda e: e.max(out=m8[:, 8:16], in_=tm2[:]), reads=["tm2"], writes=["m8"])
            P.op("dve", lambda e: e.tensor_scalar(out=tm2[:], in0=tm[:], scalar1=m8[:, 15:16], scalar2=None,
                                                  op0=ALU.is_ge), reads=["tm", "m8"], writes=["tm2"])
            P.op("dve", lambda e, xs=xs: e.tensor_scalar(out=xs[:], in0=tm2[:], scalar1=-1.0, scalar2=-NEG,
                                                         op0=ALU.add, op1=ALU.mult), reads=["tm2"], writes=[xsn])

        def select_b(gl, sub):
            xs, xsn = Xs[gl * 4 + sub], "Xs%d" % (gl * 4 + sub)
            pT, pTn = nps("T")
            P.op("pe", lambda e, pT=pT, xs=xs: e.matmul(pT[:, 0:128], lhsT=xs[:], rhs=IDB[:], start=True, stop=True),
                 reads=[xsn, "IDB"], writes=[pTn])
            P.op("act", lambda e, pT=pT, sub=sub, gl=gl: e.copy(out=XT[gl][:, sub * 128:(sub + 1) * 128],
                                                                in_=pT[:, 0:128]),
                 reads=[pTn], writes=["XT%d" % gl])

        ecmp_ctr = [0]
        for i in range(NTT):
            t0 = 512 * i
            P.dma(Qt[:], Qd[:, :, t0:t0 + 512], writes=["Qt"])
            P.dma(QRt[:], QRd[:, :, t0:t0 + 512], writes=["QRt"], q="pool")
            P.dma(Gt[:], GAd[t0:t0 + 512, :].rearrange("(s p) c -> p s c", p=128), writes=["Gt"])
            units = []
            for gl in range(2):
                for r in range(4):
                    ei = ecmp_ctr[0] % 2
                    ecmp_ctr[0] += 1
                    us, pO, pOn, tiles = make_units(gl, r, i, BR_CMP, ei)
                    box = {}

                    def pa(pO=pO, pOn=pOn, box=box):
                        box["ot"] = combine_a(pO, pOn)
                    us[-1]["post"].append((0, pa))
                    us[-1]["post"].append((1, lambda gl=gl, r=r, tiles=tiles, ei=ei: imp_head(gl, r, tiles, ei)))
                    us[-1]["post"].append((2, lambda gl=gl, r=r, box=box: combine_b(gl, r, box["ot"][0], box["ot"][1],
                                                                                    BR_CMP, True)))
                    if r == 3:
                        for sub in range(4):
                            us[-1]["post"].append((2, lambda gl=gl, sub=sub: select_a(gl, i, sub)))
                            us[-1]["post"].append((6 + sub, lambda gl=gl, sub=sub: select_b(gl, sub)))
                    units += us
            for kind in (BR_WIN, BR_SEL):
                for gl in range(2):
                    for r in range(4):
                        us, pO, pOn, _ = make_units(gl, r, i, kind, 0)
                        box = {}

                        def pa(pO=pO, pOn=pOn, box=box):
                            box["ot"] = combine_a(pO, pOn)
                        us[-1]["post"].append((0, pa))
                        us[-1]["post"].append((2, lambda gl=gl, r=r, box=box, kind=kind: combine_b(
                            gl, r, box["ot"][0], box["ot"][1], kind, False)))
                        units += us
            N = len(units)
            deferred = {}
            maxdelay = 12
            for n in range(N + LOOK + maxdelay):
                if n < N:
                    emit_S(units[n])
                m = n - LOOK
                if m in deferred:
                    for fn in deferred.pop(m):
                        fn()
                if 0 <= m < N:
                    emit_EPV(units[m])
                    for (dl, fn) in units[m]["post"]:
                        if dl == 0:
                            fn()
                        else:
                            deferred.setdefault(m + dl, []).append(fn)
            assert not deferred, deferred.keys()
            oacc_names = ["OACC%d_%d" % (sub, h) for sub in range(4) for h in range(8)]
            for sub in range(4):
                P.op("act", lambda e, sub=sub: e.copy(out=OB[:], in_=OACC[:, sub, :]),
                     reads=["OACC%d_%d" % (sub, h) for h in range(8)], writes=["OB"])
                P.dma(Od[t0 + sub * 128:t0 + sub * 128 + 128, :], OB[:], reads=["OB"])
        P.emit()
    return nc


def nsa_b_inputs(pr, gate, params, T, gp, consts):
    q = pr[:, 0:1024].reshape(T, 4, 4, 64)
    qr = pr[:, 2560:3584].reshape(T, 4, 4, 64)
    kv = pr[:, 1024:2560].reshape(T, 6, 4, 64)
    gs = slice(2 * gp, 2 * gp + 2)

    def qlay(z):
        return np.ascontiguousarray(z[:, gs].transpose(1, 3, 2, 0).reshape(128, 4, T))

    def klay(z):
        return np.ascontiguousarray(z[:, gs].transpose(1, 2, 0).reshape(128, T))

    def vlay(z):
        o = np.ones((T, 2, 65), dtype=z.dtype)
        o[:, :, 0:64] = z[:, gs]
        return o.reshape(T, 130)
    d = {"Q": qlay(q), "QR": qlay(qr), "KC": klay(kv[:, 0]), "VC": klay(kv[:, 1]), "KS": klay(kv[:, 2]),
         "VS": vlay(kv[:, 3]), "KW": klay(kv[:, 4]), "VW": vlay(kv[:, 5]),
         "GA": np.ascontiguousarray(gate.reshape(T, 16, 3)[:, 8 * gp:8 * gp + 8].reshape(T, 24))}
    for nm, w1, w2, pe in (("K", params["cmp_k_w1"], params["cmp_k_w2"], params["pe_k"]),
                           ("V", params["cmp_v_w1"], params["cmp_v_w2"], params["pe_v"])):
        w1r = w1.reshape(32, 64, 256).transpose(1, 0, 2).reshape(64, 32 * 256)
        d["W1" + nm] = np.ascontiguousarray(np.concatenate([w1r, w1r], 0))
        w2r = w2.reshape(2, 128, 64).transpose(1, 0, 2)
        d["W2" + nm] = np.ascontiguousarray(np.concatenate([w2r, w2r], 2).reshape(128, 256))
        d["PE" + nm] = np.ascontiguousarray(np.concatenate([pe.T, pe.T], 0))
    d.update(consts)
    return d


D = 1024
EPS = 1e-6


def build_proj_a(NT, NOUT, f32_cols=None, name="pa"):
    nc = bass.Bass("TRN2", target_bir_lowering=False)
    xT = nc.dram_tensor("xT", [D, NT], F32, kind="ExternalInput").ap()
    w_in = nc.dram_tensor("w_in", [D, NOUT], F32, kind="ExternalInput").ap()
    gn = nc.dram_tensor("gn", [128, 8], F32, kind="ExternalInput").ap()
    pr = nc.dram_tensor("pr", [NT, NOUT], BF16, kind="ExternalOutput").ap()
    if f32_cols:
        nf = f32_cols[1] - f32_cols[0]
        pf = nc.dram_tensor("pf", [NT, nf], F32, kind="ExternalOutput").ap()
    P = Prog(nc)
    with contextlib.ExitStack() as es:
        T = lambda name, shape, dt: es.enter_context(nc.sbuf_tensor(name, shape, dt))
        nextps = make_ps(es, nc, 7)
        stg = [T("stg%d" % i, [128, 512], F32) for i in range(2)]
        w_t = load_cast_weight(P, es, nc, w_in, D, NOUT, "w_t", stg, ["stg0", "stg1"])
        fr = Front(P, es, nc, gn, nps=nextps)
        OUT = T("OUT", [128, NOUT], BF16)
        if f32_cols:
            OF = T("OF", [128, nf], F32)
        blocks = [(c0, min(512, NOUT - c0)) for c0 in range(0, NOUT, 512)]
        for t0 in range(0, NT, 512):
            n = min(512, NT - t0)
            fr.run(xT[:, t0:t0 + n], n)
            for s0 in range(0, n, 128):
                tt = t0 + s0
                for bi, (c0, cwid) in enumerate(blocks):
                    pt, pn = nextps()
                    for kc in range(8):
                        P.op("pe", lambda e, kc=kc, pt=pt, c0=c0, cwid=cwid, s0=s0: e.matmul(
                            pt[:, 0:cwid], lhsT=fr.h_t[:, kc, s0:s0 + 128], rhs=w_t[:, kc, c0:c0 + cwid],
                            start=(kc == 0), stop=(kc == 7)), reads=["h_t", "w_t"], writes=[pn])
                    if bi % 2 == 0:
                        P.op("act", lambda e, pt=pt, c0=c0, cwid=cwid: e.copy(out=OUT[:, c0:c0 + cwid],
                                                                              in_=pt[:, 0:cwid]),
                             reads=[pn], writes=["OUT"])
                    else:
                        P.op("dve", lambda e, pt=pt, c0=c0, cwid=cwid: e.tensor_copy(out=OUT[:, c0:c0 + cwid],
                                                                                     in_=pt[:, 0:cwid]),
                             reads=[pn], writes=["OUT"])
                    if f32_cols and c0 <= f32_cols[0] and f32_cols[1] <= c0 + cwid:
                        a, b = f32_cols[0] - c0, f32_cols[1] - c0
                        P.op("dve", lambda e, pt=pt, a=a, b=b: e.tensor_copy(out=OF[:], in_=pt[:, a:b]),
                             reads=[pn], writes=["OF"])
                P.dma(pr[tt:tt + 128, :], OUT[:], reads=["OUT"])
                if f32_cols:
                    P.dma(pf[tt:tt + 128, :], OF[:], reads=["OF"])
        P.emit()
    return nc


def mlstm_consts():
    import ml_dtypes
    s = np.arange(128)[:, None]
    t = np.arange(128)[None, :]
    tri = (s <= t).astype(np.float32)
    sel = np.zeros((128, 128), np.float32)
    sel[127, :] = 1.0
    return {"TRI": tri, "SEL": sel, "MASK2": tri.copy(),
            "IDB": np.eye(128, dtype=np.float32).astype(ml_dtypes.bfloat16)}


def build_mlstm_b(T):
    nc = bass.Bass("TRN2", target_bir_lowering=False)
    din = lambda name, shape, dt: nc.dram_tensor(name, shape, dt, kind="ExternalInput").ap()
    QTd = din("QT", [128, 2, T + 3], BF16)
    KTd = din("KT", [128, 2, T + 3], BF16)
    CWd = din("CW", [128, 16], F32)
    CBd = din("CB", [128, 4], F32)
    V1d = din("V1", [T, 4 * 129], BF16)
    OGd = din("OG", [T, 512], BF16)
    IGd = din("IG", [T, 4], F32)
    FGd = din("FG", [T, 4], F32)
    BGd = din("BG", [128, 8], F32)
    NGd = din("NG", [128, 512], F32)
    TRId = din("TRI", [128, 128], F32)
    SELd = din("SEL", [128, 128], F32)
    M2d = din("MASK2", [128, 128], F32)
    IDBd = din("IDB", [128, 128], BF16)
    Od = nc.dram_tensor("O", [T, 512], BF16, kind="ExternalOutput").ap()
    P = Prog(nc)
    with contextlib.ExitStack() as es:
        Tn = lambda name, shape, dt: es.enter_context(nc.sbuf_tensor("sb_" + name, shape, dt))

        def cl(name, dram, shape, dt, q="sp"):
            t = Tn(name, shape, dt)
            P.dma(t[:], dram, writes=[name], q=q)
            return t
        CW = cl("CW", CWd, [128, 16], F32)
        CB = cl("CB", CBd, [128, 4], F32)
        BG = cl("BG", BGd, [128, 8], F32)
        NG = cl("NG", NGd, [128, 512], F32)
        TRI = cl("TRI", TRId, [128, 128], F32)
        SEL = cl("SEL", SELd, [128, 128], F32)
        M2 = cl("MASK2", M2d, [128, 128], F32)
        IDB = cl("IDB", IDBd, [128, 128], BF16)
        nps_ = make_ps(es, nc, 8)
        XQ = Tn("XQ", [128, 2, 515], BF16)
        XK = Tn("XK", [128, 2, 515], BF16)
        acc = Tn("acc", [128, 512], F32)
        QS = Tn("QS", [128, 2, 512], BF16)
        KSs = Tn("KSs", [128, 2, 512], BF16)
        V1 = Tn("V1", [128, 4, 4 * 129], BF16)
        OG = Tn("OG", [128, 4, 512], BF16)
        IG = Tn("IG", [128, 4, 4], F32)
        FG = Tn("FG", [128, 4, 4], F32)
        lf = Tn("lf", [128, 4], F32)
        bcs = Tn("bcs", [128, 4], F32)
        ea = Tn("ea", [128, 4], F32)
        eb = Tn("eb", [128, 4], F32)
        GE = Tn("GE", [128, 4], F32)
        KW = Tn("KW", [128, 4, 128], BF16)
        ATTh = [Tn("ATT%d" % h, [128, 128], BF16) for h in range(4)]
        smH = [Tn("smH%d" % h, [128, 8], F32) for h in range(4)]
        hhH = [Tn("hhH%d" % h, [128, 128], F32) for h in range(4)]
        sqH = [Tn("sqH%d" % h, [128, 128], F32) for h in range(4)]
        ST32 = Tn("ST32", [128, 4, 129], F32)
        STB = Tn("STB", [128, 4, 129], BF16)
        sm = Tn("sm", [128, 8], F32)
        hh = Tn("hh", [128, 128], F32)
        sq = Tn("sq", [128, 128], F32)
        sg = Tn("sg", [128, 512], F32)
        OUT = Tn("OUT", [128, 512], BF16)
        P.op("pool", lambda e: e.memset(ST32[:], 0.0), writes=["ST32_%d" % h for h in range(4)])
        P.op("pool", lambda e: e.memset(STB[:], 0.0), writes=["STB_%d" % h for h in range(4)])

        def conv_silu(X, Xn, qk, dst, dstn, scale):
            for pair in range(2):
                ci = (qk * 2 + pair) * 4
                P.op("dve", lambda e, pair=pair, ci=ci: e.tensor_scalar(
                    out=acc[:], in0=X[:, pair, 0:512], scalar1=CW[:, ci:ci + 1], scalar2=None, op0=ALU.mult),
                    reads=[Xn, "CW"], writes=["acc"])
                for j in (1, 2, 3):
                    P.op("dve", lambda e, pair=pair, ci=ci, j=j: e.scalar_tensor_tensor(
                        out=acc[:], in0=X[:, pair, j:j + 512], scalar=CW[:, ci + j:ci + j + 1], in1=acc[:],
                        op0=ALU.mult, op1=ALU.add), reads=[Xn, "CW", "acc"], writes=["acc"])
                P.op("act", lambda e, pair=pair, qk=qk: e.activation(
                    out=acc[:], in_=acc[:], func=AF.Silu, bias=CB[:, qk * 2 + pair:qk * 2 + pair + 1]),
                    reads=["acc", "CB"], writes=["acc"])
                P.op("pool", lambda e, pair=pair: e.tensor_scalar(
                    out=dst[:, pair, :], in0=acc[:], scalar1=scale, scalar2=None, op0=ALU.mult),
                    reads=["acc"], writes=[dstn])

        for t0 in range(0, T, 512):
            P.dma(XQ[:], QTd[:, :, t0:t0 + 515], writes=["XQ"])
            P.dma(XK[:], KTd[:, :, t0:t0 + 515], writes=["XK"], q="pool")
            P.dma(V1[:], V1d[t0:t0 + 512, :].rearrange("(s p) c -> p s c", p=128), writes=["V1"])
            P.dma(OG[:], OGd[t0:t0 + 512, :].rearrange("(s p) c -> p s c", p=128), writes=["OG"], q="pool")
            P.dma(IG[:], IGd[t0:t0 + 512, :].rearrange("(s p) c -> p s c", p=128), writes=["IG"])
            P.dma(FG[:], FGd[t0:t0 + 512, :].rearrange("(s p) c -> p s c", p=128), writes=["FG"])
            conv_silu(XQ, "XQ", 0, QS, "QS", 1.0)
            conv_silu(XK, "XK", 1, KSs, "KSs", 0.125)
            for sub in range(4):
                cs = slice(sub * 128, sub * 128 + 128)
                P.op("dve", lambda e, sub=sub: e.tensor_tensor(out=lf[:], in0=FG[:, sub, :], in1=BG[:, 4:8],
                                                               op=ALU.add), reads=["FG", "BG"], writes=["lf"])
                P.op("act", lambda e: e.activation(out=lf[:], in_=lf[:], func=AF.Sigmoid), reads=["lf"], writes=["lf"])
                P.op("act", lambda e: e.activation(out=lf[:], in_=lf[:], func=AF.Ln), reads=["lf"], writes=["lf"])
                pb, pbn = nps_()
                P.op("pe", lambda e, pb=pb: e.matmul(pb[:, 0:4], lhsT=TRI[:], rhs=lf[:], start=True, stop=True),
                     reads=["TRI", "lf"], writes=[pbn])
                P.op("act", lambda e, pb=pb: e.activation(out=eb[:], in_=pb[:, 0:4], func=AF.Exp),
                     reads=[pbn], writes=["eb"])
                P.op("dve", lambda e, sub=sub: e.tensor_tensor(out=bcs[:], in0=IG[:, sub, :], in1=BG[:, 0:4],
                                                               op=ALU.add), reads=["IG", "BG"], writes=["bcs"])
                P.op("dve", lambda e, pb=pb: e.tensor_tensor(out=bcs[:], in0=bcs[:], in1=pb[:, 0:4], op=ALU.subtract),
                     reads=["bcs", pbn], writes=["bcs"])
                P.op("act", lambda e: e.activation(out=ea[:], in_=bcs[:], func=AF.Exp), reads=["bcs"], writes=["ea"])
                pg, pgn = nps_()
                P.op("pe", lambda e, pg=pg: e.matmul(pg[:, 0:4], lhsT=SEL[:], rhs=eb[:], start=True, stop=True),
                     reads=["SEL", "eb"], writes=[pgn])
                P.op("act", lambda e, pg=pg: e.copy(out=GE[:], in_=pg[:, 0:4]), reads=[pgn], writes=["GE"])
                P.op("act", lambda e, sub=sub: e.activation(out=sg[:], in_=OG[:, sub, :], func=AF.Sigmoid),
                     reads=["OG"], writes=["sg"])
                for pair in range(2):
                    pk, pkn = nps_()
                    P.op("pe", lambda e, pk=pk, pair=pair, cs=cs: e.matmul(
                        pk[:, 0:128], lhsT=KSs[:, pair, cs], rhs=IDB[:], start=True, stop=True),
                        reads=["KSs", "IDB"], writes=[pkn])
                    for hl in range(2):
                        h = pair * 2 + hl
                        for dup in range(2):
                            P.op("dve", lambda e, pk=pk, hl=hl, h=h, dup=dup: e.tensor_scalar(
                                out=KW[:, h, dup * 64:dup * 64 + 64], in0=pk[:, hl * 64:hl * 64 + 64],
                                scalar1=ea[:, h:h + 1], scalar2=None, op0=ALU.mult),
                                reads=[pkn, "ea"], writes=["KW"])
                def head_chain(h, sub=sub, cs=cs):
                    pair, hl = h // 2, h % 2
                    pr = slice(64 * hl, 64 * hl + 64)
                    Vh = V1[:, sub, h * 129:(h + 1) * 129]
                    att, attn, smh, smn = ATTh[h], "ATT%d" % h, smH[h], "sm%d" % h
                    hhh, hhn, sqh, sqn = hhH[h], "hh%d" % h, sqH[h], "sq%d" % h
                    stn, stbn = "ST32_%d" % h, "STB_%d" % h
                    ps_, psn = nps_()
                    P.op("pe", lambda e: e.matmul(ps_[:, 0:128], lhsT=KSs[pr, pair, cs], rhs=QS[pr, pair, cs],
                                                  start=True, stop=True), reads=["KSs", "QS"], writes=[psn])
                    yield
                    P.op("dve", lambda e: e.scalar_tensor_tensor(
                        out=att[:], in0=ps_[:, 0:128], scalar=ea[:, h:h + 1], in1=M2[:], op0=ALU.mult, op1=ALU.mult),
                        reads=[psn, "ea", "MASK2"], writes=[attn])
                    yield
                    pn_, pnn = nps_()
                    P.op("pe", lambda e: e.matmul(pn_[:, 0:129], lhsT=att[:], rhs=Vh, start=True, stop=False),
                         reads=[attn, "V1"], writes=[pnn])
                    P.op("pe", lambda e: e.matmul(pn_[:, 0:129], lhsT=QS[pr, pair, cs], rhs=STB[pr, h, :],
                                                  start=False, stop=True), reads=["QS", stbn], writes=[pnn])
                    pkv, pkvn = nps_()
                    P.op("pe", lambda e: e.matmul(pkv[:, 0:129], lhsT=KW[:, h, :], rhs=Vh, start=True, stop=True),
                         reads=["KW", "V1"], writes=[pkvn])
                    yield
                    P.op("dve", lambda e: e.tensor_tensor(out=ST32[pr, h, :], in0=ST32[pr, h, :], in1=pkv[pr, 0:129],
                                                          op=ALU.add), reads=[pkvn, stn], writes=[stn])
                    P.op("dve", lambda e: e.tensor_scalar(out=ST32[pr, h, :], in0=ST32[pr, h, :],
                                                          scalar1=GE[pr, h:h + 1], scalar2=None, op0=ALU.mult),
                         reads=[stn, "GE"], writes=[stn])
                    P.op("act", lambda e: e.copy(out=STB[pr, h, :], in_=ST32[pr, h, :]), reads=[stn], writes=[stbn])
                    P.op("dve", lambda e: e.tensor_tensor(out=smh[:, 0:1], in0=pn_[:, 128:129], in1=eb[:, h:h + 1],
                                                          op=ALU.mult), reads=[pnn, "eb"], writes=[smn])
                    P.op("dve", lambda e: e.tensor_scalar(out=smh[:, 1:2], in0=smh[:, 0:1], scalar1=-1.0, scalar2=None,
                                                          op0=ALU.mult), reads=[smn], writes=[smn])
                    P.op("dve", lambda e: e.tensor_tensor(out=smh[:, 0:1], in0=smh[:, 0:1], in1=smh[:, 1:2], op=ALU.max),
                         reads=[smn], writes=[smn])
                    P.op("dve", lambda e: e.tensor_scalar(out=smh[:, 0:1], in0=smh[:, 0:1], scalar1=1.0, scalar2=None,
                                                          op0=ALU.max), reads=[smn], writes=[smn])
                    P.op("dve", lambda e: e.reciprocal(out=smh[:, 1:2], in_=smh[:, 0:1]), reads=[smn], writes=[smn])
                    P.op("dve", lambda e: e.tensor_tensor(out=smh[:, 2:3], in0=smh[:, 1:2], in1=eb[:, h:h + 1],
                                                          op=ALU.mult), reads=[smn, "eb"], writes=[smn])
                    P.op("dve", lambda e: e.tensor_scalar(out=hhh[:], in0=pn_[:, 0:128], scalar1=smh[:, 2:3],
                                                          scalar2=None, op0=ALU.mult), reads=[pnn, smn], writes=[hhn])
                    yield
                    P.op("dve", lambda e: e.tensor_tensor(out=sqh[:], in0=hhh[:], in1=hhh[:], op=ALU.mult),
                         reads=[hhn], writes=[sqn])
                    P.op("dve", lambda e: e.reduce_sum(out=smh[:, 3:4], in_=sqh[:], axis=AX.X), reads=[sqn], writes=[smn])
                    P.op("dve", lambda e: e.tensor_scalar(out=smh[:, 3:4], in0=smh[:, 3:4], scalar1=1.0 / 128,
                                                          scalar2=EPS, op0=ALU.mult, op1=ALU.add),
                         reads=[smn], writes=[smn])
                    yield
                    P.op("act", lambda e: e.sqrt(out=smh[:, 3:4], in_=smh[:, 3:4]), reads=[smn], writes=[smn])
                    yield
                    P.op("dve", lambda e: e.reciprocal(out=smh[:, 4:5], in_=smh[:, 3:4]), reads=[smn], writes=[smn])
                    P.op("dve", lambda e: e.scalar_tensor_tensor(
                        out=sqh[:], in0=hhh[:], scalar=smh[:, 4:5], in1=NG[:, h * 128:(h + 1) * 128],
                        op0=ALU.mult, op1=ALU.mult), reads=[hhn, smn, "NG"], writes=[sqn])
                    P.op("dve", lambda e: e.tensor_tensor(
                        out=OUT[:, h * 128:(h + 1) * 128], in0=sqh[:], in1=sg[:, h * 128:(h + 1) * 128], op=ALU.mult),
                        reads=[sqn, "sg"], writes=["OUT%d" % h])
                gens = [head_chain(h) for h in range(4)]
                while gens:
                    for g in list(gens):
                        try:
                            next(g)
                        except StopIteration:
                            gens.remove(g)
                tt = t0 + sub * 128
                P.dma(Od[tt:tt + 128, :], OUT[:], reads=["OUT%d" % h for h in range(4)])
        P.emit()
    return nc


def mlstm_b_inputs(pr, pf, params, T, hh_, consts):
    hs = slice(4 * hh_, 4 * hh_ + 4)
    qk = pr[:, 0:1024]
    q = qk[:, 0:512].reshape(T, 8, 64)[:, hs]
    k = qk[:, 512:1024].reshape(T, 8, 64)[:, hs]

    def lay(z):
        o = np.zeros((128, 2, T + 3), dtype=z.dtype)
        o[:, :, 3:] = z.reshape(T, 2, 2, 64).transpose(2, 3, 1, 0).reshape(128, 2, T)
        return o
    cw = params["conv_w"]
    cb = params["conv_b"]
    CW = np.zeros((128, 16), np.float32)
    CB = np.zeros((128, 4), np.float32)
    for qki in range(2):
        for pair in range(2):
            for hl in range(2):
                head = 4 * hh_ + pair * 2 + hl
                f0 = qki * 512 + head * 64
                CW[hl * 64:(hl + 1) * 64, (qki * 2 + pair) * 4:(qki * 2 + pair) * 4 + 4] = cw[:, f0:f0 + 64].T
                CB[hl * 64:(hl + 1) * 64, qki * 2 + pair] = cb[f0:f0 + 64]
    v = pr[:, 1024:2048].reshape(T, 8, 128)[:, hs]
    V1 = np.ones((T, 4, 129), dtype=pr.dtype)
    V1[:, :, 0:128] = v
    og = pr[:, 2064:3088].reshape(T, 8, 128)[:, hs].reshape(T, 512)
    bg = params["b_gates"]
    BG = np.tile(np.concatenate([bg[0:8][hs], bg[8:16][hs]])[None], (128, 1)).astype(np.float32)
    NG = np.tile(params["norm"].reshape(8, 128)[hs].reshape(1, 512), (128, 1)).astype(np.float32)
    d = {"QT": lay(q), "KT": lay(k), "CW": CW, "CB": CB, "V1": V1.reshape(T, 516),
         "OG": np.ascontiguousarray(og), "IG": np.ascontiguousarray(pf[:, 0:8][:, hs]),
         "FG": np.ascontiguousarray(pf[:, 8:16][:, hs]), "BG": BG, "NG": NG}
    d.update(consts)
    return d


D = 1024
EPS = 1e-6
GN_EPS = 64e-5
DECAY_C = -0.6065306597126334


def build_rwkv_a(NT):
    nc = bass.Bass("TRN2", target_bir_lowering=False)
    din = lambda name, shape, dt=F32: nc.dram_tensor(name, shape, dt, kind="ExternalInput").ap()
    xT = din("xT", [D, NT + 1])
    gn = din("gn", [128, 8])
    mu = din("mu", [128, 48])
    Wd = {n: din(n, [D, D]) for n in ("w_r", "w_k", "w_v")}
    w_w1, a_w1, g_w1 = din("w_w1", [D, 64]), din("a_w1", [D, 64]), din("g_w1", [D, 128])
    w_w2, a_w2, g_w2 = din("w_w2", [64, D]), din("a_w2", [64, D]), din("g_w2", [128, D])
    BC = din("BC", [128, 5 * D])
    OB = nc.dram_tensor("OB", [NT, 6 * D], BF16, kind="ExternalOutput").ap()
    OLW = nc.dram_tensor("OLW", [NT, D], F32, kind="ExternalOutput").ap()
    OBS = nc.dram_tensor("OBS", [NT, 16], F32, kind="ExternalOutput").ap()
    P = Prog(nc)
    with contextlib.ExitStack() as es:
        T = lambda name, shape, dt: es.enter_context(nc.sbuf_tensor("sb_" + name, shape, dt))
        nextps = make_ps(es, nc, 7)
        stg = [T("stg%d" % i, [128, 512], F32) for i in range(2)]
        stgn = ["stg0", "stg1"]
        Wt = {n: load_cast_weight(P, es, nc, Wd[n], D, D, n, stg, stgn) for n in ("w_r", "w_k", "w_v")}
        W1 = {"w": load_cast_weight(P, es, nc, w_w1, D, 64, "w_w1", stg, stgn),
              "a": load_cast_weight(P, es, nc, a_w1, D, 64, "a_w1", stg, stgn),
              "g": load_cast_weight(P, es, nc, g_w1, D, 128, "g_w1", stg, stgn)}
        W2 = {}
        for nm, dr, kk_ in (("w", w_w2, 64), ("a", a_w2, 64), ("g", g_w2, 128)):
            t = T("w2" + nm, [128, D], BF16)
            for c0 in (0, 512):
                s, sn = stg[(c0 // 512) % 2], stgn[(c0 // 512) % 2]
                P.dma(s[0:kk_, :], dr[:, c0:c0 + 512], writes=[sn])
                P.op("act", lambda e, t=t, s=s, c0=c0, kk_=kk_: e.copy(out=t[0:kk_, c0:c0 + 512], in_=s[0:kk_, :]),
                     reads=[sn], writes=["w2" + nm])
            W2[nm] = t
        fr = Front(P, es, nc, gn, nps=nextps)
        mu_t = T("mu_t", [128, 48], F32)
        P.dma(mu_t[:], mu, writes=["mu_t"])
        BCt = T("BCt", [128, 5 * D], F32)
        P.dma(BCt[:], BC, writes=["BCt"], q="pool")
        Dt = T("Dt", [128, 8, 256], F32)
        XJ = [T("XJ%d" % j, [128, 8, 256], BF16) for j in range(6)]
        Rt, Kt, Vt = T("Rt", [128, D], F32), T("Kt", [128, D], F32), T("Vt", [128, D], F32)
        At, Lt, t1, t2 = T("At", [128, D], F32), T("Lt", [128, D], F32), T("t1", [128, D], F32), T("t2", [128, D], F32)
        L1 = {"w": T("L1w", [128, 128], BF16), "a": T("L1a", [128, 128], BF16), "g": T("L1g", [128, 128], BF16)}
        sm = T("sm", [128, 64], F32)
        OBt = T("OBt", [128, 6 * D], BF16)
        W0, A0, KK_, KA_, RK_ = (BCt[:, i * D:(i + 1) * D] for i in range(5))
        v3 = lambda ap: ap.rearrange("p (h d) -> p h d", d=64)

        for t0 in range(0, NT, 256):
            fr.run(xT[:, t0:t0 + 257], 257)
            h = fr.h_t
            P.op("dve", lambda e: e.tensor_tensor(out=Dt[:], in0=h[:, :, 0:256], in1=h[:, :, 1:257], op=ALU.subtract),
                 reads=["h_t"], writes=["Dt"])
            for j in range(6):
                for c in range(8):
                    eng = "dve"
                    P.op(eng, lambda e, j=j, c=c: e.scalar_tensor_tensor(
                        out=XJ[j][:, c, :], in0=Dt[:, c, :], scalar=mu_t[:, j * 8 + c:j * 8 + c + 1],
                        in1=h[:, c, 1:257], op0=ALU.mult, op1=ALU.add),
                        reads=["Dt", "mu_t", "h_t"], writes=["XJ%d" % j])
            for sub in range(2):
                cs = slice(sub * 128, sub * 128 + 128)
                tt = t0 + sub * 128

                def dense_tok(xj, w, wn, dst, dstn, evac):
                    for blk in range(2):
                        pt, pn = nextps()
                        for kc in range(8):
                            P.op("pe", lambda e, kc=kc, pt=pt, blk=blk, cs=cs, xj=xj, w=w: e.matmul(
                                pt[:, 0:512], lhsT=XJ[xj][:, kc, cs], rhs=w[:, kc, blk * 512:(blk + 1) * 512],
                                start=(kc == 0), stop=(kc == 7)), reads=["XJ%d" % xj, wn], writes=[pn])
                        evac(pt, pn, blk)
                dense_tok(0, Wt["w_r"], "w_r", Rt, "Rt", lambda pt, pn, blk: P.op(
                    "act", lambda e, pt=pt, blk=blk: e.copy(out=Rt[:, blk * 512:(blk + 1) * 512], in_=pt[:, 0:512]), reads=[pn], writes=["Rt"]))
                dense_tok(2, Wt["w_k"], "w_k", Kt, "Kt", lambda pt, pn, blk: P.op(
                    "act", lambda e, pt=pt, blk=blk: e.copy(out=Kt[:, blk * 512:(blk + 1) * 512], in_=pt[:, 0:512]), reads=[pn], writes=["Kt"]))
                dense_tok(3, Wt["w_v"], "w_v", Vt, "Vt", lambda pt, pn, blk: P.op(
                    "act", lambda e, pt=pt, blk=blk: e.copy(out=Vt[:, blk * 512:(blk + 1) * 512], in_=pt[:, 0:512]), reads=[pn], writes=["Vt"]))
                for nm, xj, kk_, fn in (("w", 1, 64, AF.Tanh), ("a", 4, 64, AF.Copy), ("g", 5, 128, AF.Sigmoid)):
                    pt, pn = nextps()
                    for kc in range(8):
                        P.op("pe", lambda e, kc=kc, pt=pt, nm=nm, xj=xj, kk_=kk_, cs=cs: e.matmul(
                            pt[0:kk_, 0:128], lhsT=W1[nm][:, kc, :], rhs=XJ[xj][:, kc, cs],
                            start=(kc == 0), stop=(kc == 7)), reads=["XJ%d" % xj, nm + "_w1"], writes=[pn])
                    if fn == AF.Copy:
                        P.op("act", lambda e, pt=pt, nm=nm, kk_=kk_: e.copy(out=L1[nm][0:kk_, :], in_=pt[0:kk_, 0:128]),
                             reads=[pn], writes=["L1" + nm])
                    else:
                        P.op("act", lambda e, pt=pt, nm=nm, kk_=kk_, fn=fn: e.activation(
                            out=L1[nm][0:kk_, :], in_=pt[0:kk_, 0:128], func=fn), reads=[pn], writes=["L1" + nm])
                for nm, kk_ in (("w", 64), ("a", 64), ("g", 128)):
                    for blk in range(2):
                        bs_ = slice(blk * 512, (blk + 1) * 512)
                        pt, pn = nextps()
                        P.op("pe", lambda e, pt=pt, nm=nm, kk_=kk_, bs_=bs_: e.matmul(
                            pt[:, 0:512], lhsT=L1[nm][0:kk_, :], rhs=W2[nm][0:kk_, bs_], start=True, stop=True),
                            reads=["L1" + nm, "w2" + nm], writes=[pn])
                        if nm == "w":
                            P.op("dve", lambda e, pt=pt, bs_=bs_: e.tensor_tensor(out=Lt[:, bs_], in0=pt[:, 0:512],
                                                                                  in1=W0[:, bs_], op=ALU.add),
                                 reads=[pn, "BCt"], writes=["Lt"])
                        elif nm == "a":
                            P.op("dve", lambda e, pt=pt, bs_=bs_: e.tensor_tensor(out=At[:, bs_], in0=pt[:, 0:512],
                                                                                  in1=A0[:, bs_], op=ALU.add),
                                 reads=[pn, "BCt"], writes=["At"])
                        else:
                            P.op("act", lambda e, pt=pt, bs_=bs_: e.copy(out=OBt[:, 5 * D + bs_.start:5 * D + bs_.stop],
                                                                         in_=pt[:, 0:512]), reads=[pn], writes=["OBt"])
                P.op("act", lambda e: e.activation(out=Lt[:], in_=Lt[:], func=AF.Sigmoid), reads=["Lt"], writes=["Lt"])
                P.op("pool", lambda e: e.tensor_scalar(out=Lt[:], in0=Lt[:], scalar1=DECAY_C, scalar2=None, op0=ALU.mult),
                     reads=["Lt"], writes=["Lt"])
                P.dma(OLW[tt:tt + 128, :], Lt[:], reads=["Lt"])
                P.op("act", lambda e: e.activation(out=At[:], in_=At[:], func=AF.Sigmoid), reads=["At"], writes=["At"])
                P.op("dve", lambda e: e.tensor_tensor(out=t1[:], in0=Kt[:], in1=KK_, op=ALU.mult),
                     reads=["Kt", "BCt"], writes=["t1"])
                P.op("pool", lambda e: e.tensor_tensor(out=t2[:], in0=t1[:], in1=t1[:], op=ALU.mult),
                     reads=["t1"], writes=["t2"])
                P.op("dve", lambda e: e.tensor_reduce(out=sm[:, 0:16], in_=v3(t2[:]), axis=AX.X, op=ALU.add),
                     reads=["t2"], writes=["sm"])
                P.op("act", lambda e: e.sqrt(out=sm[:, 0:16], in_=sm[:, 0:16]), reads=["sm"], writes=["sm"])
                P.op("dve", lambda e: e.tensor_scalar(out=sm[:, 0:16], in0=sm[:, 0:16], scalar1=1e-12, scalar2=None,
                                                      op0=ALU.max), reads=["sm"], writes=["sm"])
                P.op("dve", lambda e: e.reciprocal(out=sm[:, 16:32], in_=sm[:, 0:16]), reads=["sm"], writes=["sm"])
                P.op("dve", lambda e: e.tensor_tensor(out=v3(t1[:]), in0=v3(t1[:]),
                                                      in1=sm[:, 16:32].unsqueeze(2).to_broadcast([128, 16, 64]),
                                                      op=ALU.mult), reads=["t1", "sm"], writes=["t1"])
                P.op("act", lambda e: e.copy(out=OBt[:, 3 * D:4 * D], in_=t1[:]), reads=["t1"], writes=["OBt"])
                P.op("dve", lambda e: e.tensor_tensor(out=OBt[:, 4 * D:5 * D], in0=t1[:], in1=At[:], op=ALU.mult),
                     reads=["t1", "At"], writes=["OBt"])
                P.op("dve", lambda e: e.scalar_tensor_tensor(out=t2[:], in0=At[:], scalar=-1.0, in1=KA_,
                                                             op0=ALU.add, op1=ALU.mult),
                     reads=["At", "BCt"], writes=["t2"])
                P.op("dve", lambda e: e.tensor_tensor(out=t2[:], in0=t2[:], in1=Kt[:], op=ALU.mult),
                     reads=["t2", "Kt"], writes=["t2"])
                P.op("dve", lambda e: e.tensor_tensor(out=t2[:], in0=t2[:], in1=Kt[:], op=ALU.add),
                     reads=["t2", "Kt"], writes=["t2"])
                P.op("act", lambda e: e.copy(out=OBt[:, 1 * D:2 * D], in_=t2[:]), reads=["t2"], writes=["OBt"])
                P.op("dve", lambda e: e.tensor_tensor(out=t2[:], in0=t2[:], in1=Rt[:], op=ALU.mult),
                     reads=["t2", "Rt"], writes=["t2"])
                P.op("pool", lambda e: e.tensor_tensor(out=t2[:], in0=t2[:], in1=RK_, op=ALU.mult),
                     reads=["t2", "BCt"], writes=["t2"])
                P.op("dve", lambda e: e.tensor_reduce(out=sm[:, 32:48], in_=v3(t2[:]), axis=AX.X, op=ALU.add),
                     reads=["t2"], writes=["sm"])
                P.dma(OBS[tt:tt + 128, :], sm[:, 32:48], reads=["sm"])
                P.op("act", lambda e: e.copy(out=OBt[:, 0:D], in_=Rt[:]), reads=["Rt"], writes=["OBt"])
                P.op("pool", lambda e: e.tensor_copy(out=OBt[:, 2 * D:3 * D], in_=Vt[:]), reads=["Vt"], writes=["OBt"])
                P.dma(OB[tt:tt + 128, :], OBt[:], reads=["OBt"], q="pool")
        P.emit()
    return nc


def rwkv_a_inputs(xT_halo, p, gnv):
    BC = np.concatenate([np.tile(p[k].reshape(1, D), (128, 1)) for k in ("w0", "a0", "k_k", "k_a", "r_k")], axis=1)
    mu = np.concatenate([pack_vec(p["mu"][j]) for j in (0, 1, 2, 3, 4, 5)], axis=1)
    return {"xT": xT_halo, "gn": pack_vec(gnv), "mu": np.ascontiguousarray(mu), "w_r": p["w_r"], "w_k": p["w_k"],
            "w_v": p["w_v"], "w_w1": p["w_w1"], "a_w1": p["a_w1"], "g_w1": p["g_w1"], "w_w2": p["w_w2"],
            "a_w2": p["a_w2"], "g_w2": p["g_w2"], "BC": np.ascontiguousarray(BC.astype(np.float32))}


def rwkv_consts():
    import ml_dtypes
    s = np.arange(128)[:, None]
    t = np.arange(128)[None, :]
    up = (t > s).astype(np.float32)
    upe = (t >= s).astype(np.float32)
    return {"TRI": upe.copy(), "TRIS": up.copy(),
            "MS1": np.concatenate([-up, -upe], 1), "MS2": np.concatenate([up, upe], 1),
            "MS3": -(s > t).astype(np.float32),
            "IDB": np.eye(128, dtype=np.float32).astype(ml_dtypes.bfloat16), "IDF": np.eye(128, dtype=np.float32)}


def build_rwkv_b(T):
    nc = bass.Bass("TRN2", target_bir_lowering=False)
    din = lambda name, shape, dt=F32: nc.dram_tensor(name, shape, dt, kind="ExternalInput").ap()
    FMd = {n: din(n, [128, 4, T], BF16) for n in ("RT", "KT", "KKT", "BT")}
    LWd = din("LW", [T, 512])
    Vd = din("V", [T, 512], BF16)
    Gd = din("G", [T, 512], BF16)
    BSd = din("BS", [T, 8])
    LNd = din("LN", [128, 1024])
    Cd = {n: din(n, [128, w], dt) for n, w, dt in (("TRI", 128, F32), ("TRIS", 128, F32), ("MS1", 256, F32),
                                                    ("MS2", 256, F32), ("MS3", 128, F32), ("IDB", 128, BF16),
                                                    ("IDF", 128, F32))}
    Od = nc.dram_tensor("O", [T, 512], BF16, kind="ExternalOutput").ap()
    P = Prog(nc)
    with contextlib.ExitStack() as es:
        Tn = lambda name, shape, dt: es.enter_context(nc.sbuf_tensor("sb_" + name, shape, dt))
        C = {}
        for n, (w, dt) in (("TRI", (128, F32)), ("TRIS", (128, F32)), ("MS1", (256, F32)), ("MS2", (256, F32)),
                           ("MS3", (128, F32)), ("IDB", (128, BF16)), ("IDF", (128, F32))):
            C[n] = Tn(n, [128, w], dt)
            P.dma(C[n][:], Cd[n], writes=[n])
        LN = Tn("LN", [128, 1024], F32)
        P.dma(LN[:], LNd, writes=["LN"])
        nps_ = make_ps(es, nc, 8)
        FM = {n: Tn(n, [128, 4, 128], BF16) for n in ("RT", "KT", "KKT", "BT")}
        LW = Tn("LW", [128, 512], F32)
        Vt = Tn("Vt", [128, 512], BF16)
        Gt = Tn("Gt", [128, 512], BF16)
        BS = Tn("BS", [128, 8], F32)
        eg = [Tn("eg%d" % i, [128, 128], F32) for i in range(4)]
        egx = [Tn("egx%d" % i, [128, 128], F32) for i in range(4)]
        egi = [Tn("egi%d" % i, [128, 128], F32) for i in range(4)]
        KR = [Tn("KR%d" % i, [128, 256], BF16) for i in range(4)]
        KTl = [Tn("KTl%d" % i, [128, 128], BF16) for i in range(4)]
        BTl = [Tn("BTl%d" % i, [128, 128], BF16) for i in range(4)]
        hat = [Tn("hat%d" % i, [128, 128], BF16) for i in range(8)]
        KH = [Tn("KH%d" % i, [128, 128], BF16) for i in range(4)]
        BH = [Tn("BH%d" % i, [128, 128], BF16) for i in range(4)]
        A = [[Tn("A%d_%d" % (h, i), [128, 128], F32) for i in range(2)] for h in range(8)]
        AT = [[Tn("AT%d_%d" % (h, i), [128, 128], F32) for i in range(2)] for h in range(8)]
        PT = [Tn("PT%d" % h, [128, 128], F32) for h in range(8)]
        MVK = [Tn("MVK%d" % h, [128, 256], BF16) for h in range(8)]
        NAUB = [Tn("NAUB%d" % h, [128, 128], BF16) for h in range(8)]
        RHS = [Tn("RHS%d" % h, [128, 64], F32) for h in range(8)]
        Ub = [Tn("Ub%d" % h, [128, 64], BF16) for h in range(8)]
        S32 = Tn("S32", [128, 8, 64], F32)
        S0b = Tn("S0b", [128, 8, 64], BF16)
        yc = [Tn("yc%d" % h, [128, 64], F32) for h in range(8)]
        ysq = [Tn("ysq%d" % h, [128, 64], F32) for h in range(8)]
        sm = [Tn("sm%d" % h, [128, 4], F32) for h in range(8)]
        OUT = Tn("OUT", [128, 512], BF16)
        P.op("pool", lambda e: e.memset(S32[:], 0.0), writes=["S32_%d" % h for h in range(8)])
        P.op("pool", lambda e: e.memset(S0b[:], 0.0), writes=["S0b_%d" % h for h in range(8)])

        def pair_chain(pair):
            pc = slice(pair * 128, pair * 128 + 128)
            egn, egxn, egin = "eg%d" % pair, "egx%d" % pair, "egi%d" % pair
            krn, ktn, btn = "KR%d" % pair, "KTl%d" % pair, "BTl%d" % pair
            pc_, pcn = nps_()
            P.op("pe", lambda e: e.matmul(pc_[:, 0:128], lhsT=LW[:, pc], rhs=C["TRI"][:], start=True, stop=True),
                 reads=["LW", "TRI"], writes=[pcn])
            px_, pxn = nps_()
            P.op("pe", lambda e: e.matmul(px_[:, 0:128], lhsT=LW[:, pc], rhs=C["TRIS"][:], start=True, stop=True),
                 reads=["LW", "TRIS"], writes=[pxn])
            yield
            P.op("act", lambda e: e.activation(out=eg[pair][:], in_=pc_[:, 0:128], func=AF.Exp), reads=[pcn], writes=[egn])
            P.op("act", lambda e: e.activation(out=egi[pair][:], in_=pc_[:, 0:128], func=AF.Exp, scale=-1.0),
                 reads=[pcn], writes=[egin])
            P.op("act", lambda e: e.activation(out=egx[pair][:], in_=px_[:, 0:128], func=AF.Exp), reads=[pxn], writes=[egxn])
            yield
            P.op("dve", lambda e: e.tensor_tensor(out=KR[pair][:, 0:128], in0=FM["KKT"][:, pair, :], in1=egx[pair][:],
                                                  op=ALU.mult), reads=["KKT", egxn], writes=[krn])
            P.op("dve", lambda e: e.tensor_tensor(out=KR[pair][:, 128:256], in0=FM["RT"][:, pair, :], in1=eg[pair][:],
                                                  op=ALU.mult), reads=["RT", egn], writes=[krn])
            P.op("dve", lambda e: e.tensor_tensor(out=KTl[pair][:], in0=FM["KT"][:, pair, :], in1=egi[pair][:],
                                                  op=ALU.mult), reads=["KT", egin], writes=[ktn])
            P.op("dve", lambda e: e.tensor_tensor(out=BTl[pair][:], in0=FM["BT"][:, pair, :], in1=egi[pair][:],
                                                  op=ALU.mult), reads=["BT", egin], writes=[btn])
            for qi, (src, srcn, dst, dstn, sc) in enumerate(((KTl[pair], ktn, KH[pair], "KH%d" % pair, 1.0),
                                                             (BTl[pair], btn, BH[pair], "BH%d" % pair, -1.0))):
                ht, htn = hat[pair * 2 + qi], "hat%d" % (pair * 2 + qi)
                P.op("dve", lambda e, src=src, ht=ht: e.tensor_scalar(out=ht[:], in0=src[:], scalar1=eg[pair][:, 127:128],
                                                                      scalar2=None, op0=ALU.mult),
                     reads=[srcn, egn], writes=[htn])
                yield
                ph, phn = nps_()
                P.op("pe", lambda e, ph=ph, ht=ht: e.matmul(ph[:, 0:128], lhsT=ht[:], rhs=C["IDB"][:], start=True, stop=True),
                     reads=[htn, "IDB"], writes=[phn])
                yield
                P.op("act", lambda e, ph=ph, dst=dst, sc=sc: e.activation(out=dst[:], in_=ph[:, 0:128], func=AF.Copy,
                                                                          scale=sc), reads=[phn], writes=[dstn])

        def head_chain(h):
            pair, hl = h // 2, h % 2
            pr = slice(64 * hl, 64 * hl + 64)
            Vh = Vt[:, h * 64:(h + 1) * 64]
            krn, ktn, btn = "KR%d" % pair, "KTl%d" % pair, "BTl%d" % pair
            KRp, KTp, BTp = KR[pair], KTl[pair], BTl[pair]
            An = ["A%d_%d" % (h, i) for i in range(2)]
            ATn = ["AT%d_%d" % (h, i) for i in range(2)]
            PTn, mvkn, naubn, rhsn, ubn = "PT%d" % h, "MVK%d" % h, "NAUB%d" % h, "RHS%d" % h, "Ub%d" % h
            s32n, s0bn, ycn, ysqn, smn = "S32_%d" % h, "S0b_%d" % h, "yc%d" % h, "ysq%d" % h, "sm%d" % h
            Ah, ATh, PTh, smh, ych, ysqh = A[h], AT[h], PT[h], sm[h], yc[h], ysq[h]
            p1, p1n = nps_()
            P.op("pe", lambda e: e.matmul(p1[:, 0:256], lhsT=BTp[pr, :], rhs=KRp[pr, :], start=True, stop=True),
                 reads=[btn, krn], writes=[p1n])
            yield
            P.op("dve", lambda e: e.tensor_tensor(out=ATh[0][:], in0=p1[:, 0:128], in1=C["MS1"][:, 0:128], op=ALU.mult),
                 reads=[p1n, "MS1"], writes=[ATn[0]])
            P.op("dve", lambda e: e.tensor_tensor(out=NAUB[h][:], in0=p1[:, 128:256], in1=C["MS1"][:, 128:256],
                                                  op=ALU.mult), reads=[p1n, "MS1"], writes=[naubn])
            p3, p3n = nps_()
            P.op("pe", lambda e: e.matmul(p3[:, 0:128], lhsT=KRp[pr, 0:128], rhs=BTp[pr, :], start=True, stop=True),
                 reads=[btn, krn], writes=[p3n])
            yield
            P.op("dve", lambda e: e.tensor_tensor(out=Ah[0][:], in0=p3[:, 0:128], in1=C["MS3"][:], op=ALU.mult),
                 reads=[p3n, "MS3"], writes=[An[0]])
            P.op("dve", lambda e: e.tensor_tensor(out=PTh[:], in0=ATh[0][:], in1=C["IDF"][:], op=ALU.add),
                 reads=[ATn[0], "IDF"], writes=[PTn])
            p2, p2n = nps_()
            P.op("pe", lambda e: e.matmul(p2[:, 0:256], lhsT=KTp[pr, :], rhs=KRp[pr, :], start=True, stop=True),
                 reads=[ktn, krn], writes=[p2n])
            yield
            P.op("dve", lambda e: e.tensor_tensor(out=MVK[h][:], in0=p2[:, 0:256], in1=C["MS2"][:], op=ALU.mult),
                 reads=[p2n, "MS2"], writes=[mvkn])
            for k in range(6):
                a, b = k % 2, (k + 1) % 2
                pa, pan = nps_()
                P.op("pe", lambda e, pa=pa, a=a: e.matmul(pa[:, 0:128], lhsT=ATh[a][:], rhs=Ah[a][:], start=True, stop=True),
                     reads=[ATn[a], An[a]], writes=[pan])
                if k < 5:
                    pb, pbn = nps_()
                    P.op("pe", lambda e, pb=pb, a=a: e.matmul(pb[:, 0:128], lhsT=Ah[a][:], rhs=ATh[a][:],
                                                             start=True, stop=True),
                         reads=[ATn[a], An[a]], writes=[pbn])
                yield
                P.op("act", lambda e, pa=pa, b=b: e.copy(out=Ah[b][:], in_=pa[:, 0:128]), reads=[pan], writes=[An[b]])
                if k < 5:
                    P.op("act", lambda e, pb=pb, b=b: e.copy(out=ATh[b][:], in_=pb[:, 0:128]), reads=[pbn], writes=[ATn[b]])
                yield
                pp, ppn = nps_()
                P.op("pe", lambda e, pp=pp, b=b: e.matmul(pp[:, 0:128], lhsT=Ah[b][:], rhs=PTh[:], start=True, stop=True),
                     reads=[An[b], PTn], writes=[ppn])
                yield
                P.op("dve", lambda e, pp=pp: e.tensor_tensor(out=PTh[:], in0=pp[:, 0:128], in1=PTh[:], op=ALU.add),
                     reads=[ppn, PTn], writes=[PTn])
            pr_, prn = nps_()
            P.op("pe", lambda e: e.matmul(pr_[:, 0:64], lhsT=KRp[pr, 0:128], rhs=S0b[pr, h, :], start=True, stop=False),
                 reads=[krn, s0bn], writes=[prn])
            P.op("pe", lambda e: e.matmul(pr_[:, 0:64], lhsT=MVK[h][:, 0:128], rhs=Vh, start=False, stop=True),
                 reads=[mvkn, "Vt"], writes=[prn])
            yield
            P.op("act", lambda e: e.copy(out=RHS[h][:], in_=pr_[:, 0:64]), reads=[prn], writes=[rhsn])
            yield
            pu, pun = nps_()
            P.op("pe", lambda e: e.matmul(pu[:, 0:64], lhsT=PTh[:], rhs=RHS[h][:], start=True, stop=True),
                 reads=[PTn, rhsn], writes=[pun])
            yield
            P.op("act", lambda e: e.copy(out=Ub[h][:], in_=pu[:, 0:64]), reads=[pun], writes=[ubn])
            yield
            py, pyn = nps_()
            P.op("pe", lambda e: e.matmul(py[:, 0:64], lhsT=KRp[pr, 128:256], rhs=S0b[pr, h, :], start=True, stop=False),
                 reads=[krn, s0bn], writes=[pyn])
            P.op("pe", lambda e: e.matmul(py[:, 0:64], lhsT=MVK[h][:, 128:256], rhs=Vh, start=False, stop=False),
                 reads=[mvkn, "Vt"], writes=[pyn])
            P.op("pe", lambda e: e.matmul(py[:, 0:64], lhsT=NAUB[h][:], rhs=Ub[h][:], start=False, stop=True),
                 reads=[naubn, ubn], writes=[pyn])
            pst, pstn = nps_()
            P.op("pe", lambda e: e.matmul(pst[:, 0:64], lhsT=KH[pair][:], rhs=Vh, start=True, stop=False),
                 reads=["KH%d" % pair, "Vt"], writes=[pstn])
            P.op("pe", lambda e: e.matmul(pst[:, 0:64], lhsT=BH[pair][:], rhs=Ub[h][:], start=False, stop=True),
                 reads=["BH%d" % pair, ubn], writes=[pstn])
            yield
            P.op("dve", lambda e: e.scalar_tensor_tensor(
                out=S32[pr, h, :], in0=S32[pr, h, :], scalar=eg[pair][pr, 127:128], in1=pst[pr, 0:64],
                op0=ALU.mult, op1=ALU.add), reads=[pstn, s32n, "eg%d" % pair], writes=[s32n])
            P.op("act", lambda e: e.copy(out=S0b[pr, h, :], in_=S32[pr, h, :]), reads=[s32n], writes=[s0bn])
            P.op("dve", lambda e: e.reduce_sum(out=smh[:, 0:1], in_=py[:, 0:64], axis=AX.X), reads=[pyn], writes=[smn])
            P.op("dve", lambda e: e.tensor_scalar(out=smh[:, 0:1], in0=smh[:, 0:1], scalar1=-1.0 / 64, scalar2=None,
                                                  op0=ALU.mult), reads=[smn], writes=[smn])
            P.op("dve", lambda e: e.tensor_scalar(out=ych[:], in0=py[:, 0:64], scalar1=smh[:, 0:1], scalar2=None,
                                                  op0=ALU.add), reads=[pyn, smn], writes=[ycn])
            yield
            P.op("dve", lambda e: e.tensor_tensor(out=ysqh[:], in0=ych[:], in1=ych[:], op=ALU.mult), reads=[ycn], writes=[ysqn])
            P.op("dve", lambda e: e.reduce_sum(out=smh[:, 1:2], in_=ysqh[:], axis=AX.X), reads=[ysqn], writes=[smn])
            P.op("dve", lambda e: e.tensor_scalar(out=smh[:, 1:2], in0=smh[:, 1:2], scalar1=1.0 / 64, scalar2=GN_EPS,
                                                  op0=ALU.mult, op1=ALU.add), reads=[smn], writes=[smn])
            yield
            P.op("act", lambda e: e.sqrt(out=smh[:, 1:2], in_=smh[:, 1:2]), reads=[smn], writes=[smn])
            yield
            P.op("dve", lambda e: e.reciprocal(out=smh[:, 2:3], in_=smh[:, 1:2]), reads=[smn], writes=[smn])
            P.op("dve", lambda e: e.scalar_tensor_tensor(
                out=ych[:], in0=ych[:], scalar=smh[:, 2:3], in1=LN[:, h * 64:(h + 1) * 64],
                op0=ALU.mult, op1=ALU.mult), reads=[ycn, smn, "LN"], writes=[ycn])
            P.op("dve", lambda e: e.tensor_tensor(out=ych[:], in0=ych[:], in1=LN[:, 512 + h * 64:512 + (h + 1) * 64],
                                                  op=ALU.add), reads=[ycn, "LN"], writes=[ycn])
            P.op("dve", lambda e: e.scalar_tensor_tensor(
                out=ych[:], in0=Vh, scalar=BS[:, h:h + 1], in1=ych[:], op0=ALU.mult, op1=ALU.add),
                reads=["Vt", "BS", ycn], writes=[ycn])
            P.op("dve", lambda e: e.tensor_tensor(out=OUT[:, h * 64:(h + 1) * 64], in0=ych[:],
                                                  in1=Gt[:, h * 64:(h + 1) * 64], op=ALU.mult),
                 reads=[ycn, "Gt"], writes=["OUT%d" % h])

        def round_robin(gens):
            gens = list(gens)
            while gens:
                for g in list(gens):
                    try:
                        next(g)
                    except StopIteration:
                        gens.remove(g)

        for t0 in range(0, T, 128):
            for qi, n in enumerate(("RT", "KT", "KKT", "BT")):
                P.dma(FM[n][:], FMd[n][:, :, t0:t0 + 128], writes=[n], q=("sp", "pool")[qi % 2])
            P.dma(LW[:], LWd[t0:t0 + 128, :], writes=["LW"])
            P.dma(Vt[:], Vd[t0:t0 + 128, :], writes=["Vt"], q="pool")
            P.dma(Gt[:], Gd[t0:t0 + 128, :], writes=["Gt"])
            P.dma(BS[:], BSd[t0:t0 + 128, :], writes=["BS"], q="pool")
            round_robin([pair_chain(p_) for p_ in range(4)])
            round_robin([head_chain(h) for h in range(0, 4)])
            round_robin([head_chain(h) for h in range(4, 8)])
            P.dma(Od[t0:t0 + 128, :], OUT[:], reads=["OUT%d" % h for h in range(8)])
        P.emit()
    return nc


def rwkv_b_inputs(OB, OLW, OBS, p, T, hh_, consts):
    cs = slice(512 * hh_, 512 * hh_ + 512)
    r, k, v, kk, b, g = (OB[:, i * D:(i + 1) * D][:, cs] for i in range(6))

    def fm(z):
        return np.ascontiguousarray(z.reshape(T, 4, 128).transpose(2, 1, 0))
    LN = np.concatenate([np.tile(p["ln_w"][cs][None], (128, 1)), np.tile(p["ln_b"][cs][None], (128, 1))], 1)
    d = {"RT": fm(r), "KT": fm(k), "KKT": fm(kk), "BT": fm(b), "LW": np.ascontiguousarray(OLW[:, cs]),
         "V": np.ascontiguousarray(v), "G": np.ascontiguousarray(g),
         "BS": np.ascontiguousarray(OBS[:, 8 * hh_:8 * hh_ + 8]), "LN": np.ascontiguousarray(LN.astype(np.float32))}
    d.update(consts)
    return d


_PROGS = {}
NCORES = 8


def _prog(key, fn):
    if key not in _PROGS:
        _PROGS[key] = fn()
    return _PROGS[key]


def _run(nc, in_maps):
    res = run_bass_kernel_spmd(nc, in_maps, core_ids=list(range(NCORES)))
    return res.results


def _halo_slice(xT, c, per, T, halo):
    t0 = c * per
    if halo == 0:
        return np.ascontiguousarray(xT[:, t0:t0 + per])
    out = np.zeros((xT.shape[0], per + halo), dtype=xT.dtype)
    out[:, halo:] = xT[:, t0:t0 + per]
    if t0 % T != 0:
        out[:, :halo] = xT[:, t0 - halo:t0]
    return out


def _nsa_layer(xT, inp, i, j, B, T):
    per = xT.shape[1] // NCORES
    cos, sin = rope_tables_np(T)
    p = {k[4:]: np.asarray(v[j]) for k, v in inp.items() if k.startswith("nsa_")}
    nca = _prog(("nsa_a", per), lambda: build_nsa_a(per))
    gn = pack_vec(np.asarray(inp["norm_mixer"][i]))
    bg = np.ascontiguousarray(np.tile(p["b_gate"][None], (128, 1)).astype(np.float32))
    ims = []
    for c in range(NCORES):
        pos0 = (c * per) % T
        ims.append({"xT": _halo_slice(xT, c, per, T, 0), "w_in": p["w_in"], "gn": gn,
                    "cos": np.ascontiguousarray(cos[pos0:pos0 + per]), "sin": np.ascontiguousarray(sin[pos0:pos0 + per]),
                    "bg": bg})
    ra = _run(nca, ims)
    pr = np.concatenate([r["pr"] for r in ra], axis=0).reshape(B, T, 3584)
    gate = np.concatenate([r["gate"] for r in ra], axis=0).reshape(B, T, 48)
    consts = nsa_consts(T)
    ncb = _prog(("nsa_b", T), lambda: build_nsa_b(T))
    ims = [nsa_b_inputs(pr[c // 2], gate[c // 2], p, T, c % 2, consts) for c in range(NCORES)]
    rb = _run(ncb, ims)
    o = np.concatenate([np.concatenate([rb[2 * b]["O"], rb[2 * b + 1]["O"]], axis=1) for b in range(B)], axis=0)
    return o, p["w_out"]


def _mlstm_layer(xT, inp, i, j, B, T):
    per = xT.shape[1] // NCORES
    p = {k[6:]: np.asarray(v[j]) for k, v in inp.items() if k.startswith("mlstm_")}
    nca = _prog(("proj_a", per), lambda: build_proj_a(per, 3088, f32_cols=(2048, 2176)))
    gn = pack_vec(np.asarray(inp["norm_mixer"][i]))
    ims = [{"xT": _halo_slice(xT, c, per, T, 0), "w_in": p["w_in"], "gn": gn} for c in range(NCORES)]
    ra = _run(nca, ims)
    pr = np.concatenate([r["pr"] for r in ra], axis=0).reshape(B, T, 3088)
    pf = np.concatenate([r["pf"] for r in ra], axis=0).reshape(B, T, 128)
    consts = mlstm_consts()
    ncb = _prog(("mlstm_b", T), lambda: build_mlstm_b(T))
    ims = [mlstm_b_inputs(pr[c // 2], pf[c // 2], p, T, c % 2, consts) for c in range(NCORES)]
    rb = _run(ncb, ims)
    o = np.concatenate([np.concatenate([rb[2 * b]["O"], rb[2 * b + 1]["O"]], axis=1) for b in range(B)], axis=0)
    return o, p["w_out"]


def _rwkv_layer(xT, inp, i, j, B, T):
    per = xT.shape[1] // NCORES
    p = {k[5:]: np.asarray(v[j]) for k, v in inp.items() if k.startswith("rwkv_")}
    p["r_k"] = p["r_k"].reshape(-1)
    nca = _prog(("rwkv_a", per), lambda: build_rwkv_a(per))
    gnv = np.asarray(inp["norm_mixer"][i])
    ims = [rwkv_a_inputs(_halo_slice(xT, c, per, T, 1), p, gnv) for c in range(NCORES)]
    ra = _run(nca, ims)
    OB = np.concatenate([r["OB"] for r in ra], axis=0).reshape(B, T, 6 * 1024)
    OLW = np.concatenate([r["OLW"] for r in ra], axis=0).reshape(B, T, 1024)
    OBS = np.concatenate([r["OBS"] for r in ra], axis=0).reshape(B, T, 16)
    consts = rwkv_consts()
    ncb = _prog(("rwkv_b", T), lambda: build_rwkv_b(T))
    ims = [rwkv_b_inputs(OB[c // 2], OLW[c // 2], OBS[c // 2], p, T, c % 2, consts) for c in range(NCORES)]
    rb = _run(ncb, ims)
    o = np.concatenate([np.concatenate([rb[2 * b]["O"], rb[2 * b + 1]["O"]], axis=1) for b in range(B)], axis=0)
    return o, p["w_o"]


def _ffn_layer(xT, o, w_out, inp, i, B, T, final):
    per = xT.shape[1] // NCORES
    ncf = _prog(("ffn", per, final), lambda: build_ffn(per, final_norm=final))
    oT = np.ascontiguousarray(o.T)
    cw = np.asarray(inp["ffn_conv_w"][i])
    small = {"gn": pack_vec(np.asarray(inp["norm_ffn"][i])), "gfin": pack_vec(np.asarray(inp["final_norm"])),
             "cw": np.ascontiguousarray(np.concatenate([pack_vec(cw[k]) for k in range(3)], axis=1)),
             "cb": pack_vec(np.asarray(inp["ffn_conv_b"][i]))}
    ims = []
    for c in range(NCORES):
        d = {"xT": _halo_slice(xT, c, per, T, 2), "oT": _halo_slice(oT, c, per, T, 2),
             "w_out": np.asarray(w_out), "w_up": np.asarray(inp["ffn_w_up"][i]),
             "w_down": np.asarray(inp["ffn_w_down"][i])}
        d.update(small)
        ims.append(d)
    rf = _run(ncf, ims)
    return np.concatenate([r["yT"] for r in rf], axis=1)


def kernel(**inputs):
    inp = {k: np.asarray(v) for k, v in inputs.items()}
    x = inp["x"]
    B, T, Dm = x.shape
    xT = np.ascontiguousarray(x.reshape(B * T, Dm).T)
    depth = inp["norm_mixer"].shape[0]
    for i in range(depth):
        kind, j = i % 3, i // 3
        if kind == 0:
            o, w_out = _nsa_layer(xT, inp, i, j, B, T)
        elif kind == 1:
            o, w_out = _mlstm_layer(xT, inp, i, j, B, T)
        else:
            o, w_out = _rwkv_layer(xT, inp, i, j, B, T)
        xT = _ffn_layer(xT, o, w_out, inp, i, B, T, final=(i == depth - 1))
    return np.ascontiguousarray(xT.T).reshape(B, T, Dm).astype(np.float32, copy=False)
```
